# Optimizing a Trainium2 kernel written in Bass

```python
import jax
import jax.numpy as jnp
from jax import lax
import numpy as np

D_MODEL = 4096
BATCH = 2
SEQ = 4096
DEPTH = 1

MIX_WIDTH = D_MODEL
MLSTM_WIDTH = MIX_WIDTH // 2
MLSTM_HEADS = 8
MLSTM_HEAD_DIM = MLSTM_WIDTH // MLSTM_HEADS
HGRN_WIDTH = MIX_WIDTH - MLSTM_WIDTH
HGRN_EXPAND = 128
HGRN_HEADS = HGRN_WIDTH // HGRN_EXPAND
CONV_W = 4
CHUNK = 64
N_GROUPS = 8
EXPERTS_PER_GROUP = 8
N_EXPERTS = N_GROUPS * EXPERTS_PER_GROUP
TOP_K = 2
D_EXPERT = (3 * D_MODEL) // 16
ROWS_PER_BLOCK = 128
EPS = 1e-6
STAB_INIT = -1e30
IN_SPLITS = (MLSTM_WIDTH,) * 4 + (MLSTM_HEADS,) * 2 + (HGRN_WIDTH,) * 4 + (D_MODEL,) * 2
C_IN = sum(IN_SPLITS)

kernel_name = 'hybrid_mlstm_hgrn2_hmoe_block'


def _rmsnorm(x, w):
    x32 = x.astype(jnp.float32)
    y = x32 * lax.rsqrt(jnp.mean(x32 * x32, axis=-1, keepdims=True) + EPS)
    return (y * w.astype(jnp.float32)).astype(x.dtype)


def _head_rmsnorm(t, w, out_dtype):
    y = t * lax.rsqrt(jnp.mean(t * t, axis=-1, keepdims=True) + EPS)
    y = y.reshape(t.shape[0], t.shape[1], -1) * w.astype(jnp.float32)
    return y.astype(out_dtype)


def _causal_conv(x, w, b):
    c = x.shape[-1]
    y = lax.conv_general_dilated(x, w[:, None, :], window_strides=(1,), padding=[(CONV_W - 1, 0)],
                                 dimension_numbers=('NWC', 'WIO', 'NWC'), feature_group_count=c)
    return y + b


def _heads(t, n_heads):
    b, s, _ = t.shape
    return t.reshape(b, s, n_heads, -1).transpose(0, 2, 1, 3).astype(jnp.float32)


def _to_chunks(t):
    t = t.reshape(t.shape[:2] + (t.shape[2] // CHUNK, CHUNK) + t.shape[3:])
    return jnp.moveaxis(t, 2, 0)


def _from_chunks(t):
    t = jnp.moveaxis(t, 0, 2)
    return t.reshape(t.shape[:2] + (t.shape[2] * t.shape[3],) + t.shape[4:])


def _mlstm_chunkwise(q, k, v, ig, logf):
    b_, h_, _, dk = q.shape
    dv = v.shape[-1]
    causal = jnp.tril(jnp.ones((CHUNK, CHUNK), dtype=bool))

    def step(carry, xs):
        c_st, n_st, m_st = carry
        qc, kc, vc, ic, fc = xs
        b = jnp.cumsum(fc, axis=-1)
        g = b[..., -1]
        dmat = jnp.where(causal, b[..., :, None] - b[..., None, :] + ic[..., None, :], -jnp.inf)
        m_inter = b + m_st[..., None]
        m_j = jnp.maximum(m_inter, jnp.max(dmat, axis=-1))
        s = jnp.einsum('bhjd,bhsd->bhjs', qc, kc) * jnp.exp(dmat - m_j[..., None])
        inter = jnp.exp(m_inter - m_j)
        num = jnp.einsum('bhjs,bhse->bhje', s, vc) + inter[..., None] * jnp.einsum('bhjd,bhde->bhje', qc, c_st)
        den = jnp.sum(s, axis=-1) + inter * jnp.einsum('bhjd,bhd->bhj', qc, n_st)
        h_out = num / jnp.maximum(jnp.abs(den), jnp.exp(-m_j))[..., None]
        log_w = g[..., None] - b + ic
        m_new = jnp.maximum(g + m_st, jnp.max(log_w, axis=-1))
        w = jnp.exp(log_w - m_new[..., None])
        decay = jnp.exp(g + m_st - m_new)
        c_st = decay[..., None, None] * c_st + jnp.einsum('bhsd,bhse->bhde', kc * w[..., None], vc)
        n_st = decay[..., None] * n_st + jnp.einsum('bhs,bhsd->bhd', w, kc)
        return (c_st, n_st, m_new), h_out

    init = (jnp.zeros((b_, h_, dk, dv), jnp.float32),
            jnp.zeros((b_, h_, dk), jnp.float32),
            jnp.full((b_, h_), STAB_INIT, jnp.float32))
    _, hs = lax.scan(step, init, (_to_chunks(q), _to_chunks(k), _to_chunks(v), _to_chunks(ig), _to_chunks(logf)))
    return _from_chunks(hs)


def _hgrn2_chunkwise(q, k, v, logf):
    b_, h_, _, dk = q.shape
    dv = v.shape[-1]
    causal = jnp.tril(jnp.ones((CHUNK, CHUNK), dtype=bool))

    def step(s_st, xs):
        qc, kc, vc, fc = xs
        b = jnp.cumsum(fc, axis=2)
        g = b[:, :, -1]
        diff = jnp.where(causal[..., None], b[:, :, :, None, :] - b[:, :, None, :, :], -jnp.inf)
        a = jnp.einsum('bhjc,bhsc,bhjsc->bhjs', qc, kc, jnp.exp(diff))
        o = jnp.einsum('bhjs,bhse->bhje', a, vc) + jnp.einsum('bhjc,bhce->bhje', qc * jnp.exp(b), s_st)
        s_st = jnp.exp(g)[..., None] * s_st + jnp.einsum('bhsc,bhse->bhce', kc * jnp.exp(g[:, :, None, :] - b), vc)
        return s_st, o

    init = jnp.zeros((b_, h_, dk, dv), jnp.float32)
    _, os_ = lax.scan(step, init, (_to_chunks(q), _to_chunks(k), _to_chunks(v), _to_chunks(logf)))
    return _from_chunks(os_)


def _hybrid_mixer(h, w_in, conv_w, conv_b, gate_b, lb, m_norm_w, h_norm_w, w_bm, w_bh, w_out):
    bsz, s, _ = h.shape
    u = h @ w_in
    offsets = np.cumsum(IN_SPLITS)[:-1].tolist()
    q_m, k_m, v_m, o_m, i_pre, f_pre, q_h, f_h, i_h, g_h, gate_m, gate_h = jnp.split(u, offsets, axis=-1)

    qk = jax.nn.silu(_causal_conv(jnp.concatenate([q_m, k_m], axis=-1), conv_w, conv_b))
    q_m, k_m = jnp.split(qk, 2, axis=-1)
    qm = _heads(q_m, MLSTM_HEADS)
    km = _heads(k_m, MLSTM_HEADS) * (MLSTM_HEAD_DIM ** -0.5)
    vm = _heads(v_m, MLSTM_HEADS)
    gb = gate_b.astype(jnp.float32)
    ig = (i_pre.astype(jnp.float32) + gb[:MLSTM_HEADS]).transpose(0, 2, 1)
    logf_m = jax.nn.log_sigmoid(f_pre.astype(jnp.float32) + gb[MLSTM_HEADS:]).transpose(0, 2, 1)
    hm = _mlstm_chunkwise(qm, km, vm, ig, logf_m).transpose(0, 2, 1, 3)
    hm = _head_rmsnorm(hm, m_norm_w, h.dtype) * jax.nn.sigmoid(o_m)
    y_m = hm @ w_bm

    f = lb + (1.0 - lb) * jax.nn.sigmoid(f_h.astype(jnp.float32))
    qh = _heads(jax.nn.silu(q_h), HGRN_HEADS)
    kh = _heads(1.0 - f, HGRN_HEADS)
    logf_h = _heads(jnp.log(f), HGRN_HEADS)
    vh = _heads(i_h, HGRN_HEADS)
    oh = _hgrn2_chunkwise(qh, kh, vh, logf_h).transpose(0, 2, 1, 3)
    oh = _head_rmsnorm(oh, h_norm_w, h.dtype) * jax.nn.sigmoid(g_h)
    y_h = oh @ w_bh

    y = jax.nn.sigmoid(gate_m) * y_m + jax.nn.sigmoid(gate_h) * y_h
    return y @ w_out


def _hier_moe(h, rg_w, rg_b, re_w, re_b, w_gate, w_up, w_down):
    n, d = h.shape
    g_logits = (h @ rg_w + rg_b).astype(jnp.float32)
    g_sel = jnp.argmax(g_logits, axis=-1).astype(jnp.int32)
    p_group = jnp.take_along_axis(jax.nn.softmax(g_logits, axis=-1), g_sel[:, None], axis=-1)
    e_logits = (h @ re_w + re_b).astype(jnp.float32).reshape(n, N_GROUPS, EXPERTS_PER_GROUP)
    e_logits = jnp.take_along_axis(e_logits, g_sel[:, None, None], axis=1)[:, 0]
    top_val, top_idx = lax.top_k(e_logits, TOP_K)
    weights = jax.nn.softmax(top_val, axis=-1) * p_group
    experts = g_sel[:, None] * EXPERTS_PER_GROUP + top_idx.astype(jnp.int32)

    nk = n * TOP_K
    e_flat = experts.reshape(nk)
    tok_flat = jnp.repeat(jnp.arange(n, dtype=jnp.int32), TOP_K)
    w_flat = weights.reshape(nk)
    order = jnp.argsort(e_flat)
    e_sorted = e_flat[order]
    counts = jnp.bincount(e_flat, length=N_EXPERTS)
    start = jnp.cumsum(counts) - counts
    padded = (counts + ROWS_PER_BLOCK - 1) // ROWS_PER_BLOCK * ROWS_PER_BLOCK
    pad_end = jnp.cumsum(padded)
    dest = pad_end[e_sorted] - padded[e_sorted] + jnp.arange(nk, dtype=jnp.int32) - start[e_sorted]
    n_blocks = -(-nk // ROWS_PER_BLOCK) + N_EXPERTS
    n_rows = n_blocks * ROWS_PER_BLOCK
    row_tok = jnp.zeros((n_rows,), jnp.int32).at[dest].set(tok_flat[order])
    row_w = jnp.zeros((n_rows,), h.dtype).at[dest].set(w_flat[order].astype(h.dtype))
    block_expert = jnp.minimum(
        jnp.searchsorted(pad_end, jnp.arange(n_blocks, dtype=pad_end.dtype) * ROWS_PER_BLOCK, side='right'),
        N_EXPERTS - 1)

    def expert_block(args):
        tok, wt, e = args
        xb = h[tok]
        a = jax.nn.silu(xb @ w_gate[e]) * (xb @ w_up[e])
        return (a @ w_down[e]) * wt[:, None]

    ys = lax.map(expert_block, (row_tok.reshape(n_blocks, ROWS_PER_BLOCK),
                                row_w.reshape(n_blocks, ROWS_PER_BLOCK), block_expert))
    return jax.ops.segment_sum(ys.reshape(n_rows, d), row_tok, num_segments=n)


def setup_inputs(seed: int = 0) -> dict:
    key = jax.random.key(seed)
    ks = jax.random.split(key, 24)

    def nrm(k, shape, scale):
        return jax.random.normal(k, shape, jnp.float32) * scale

    x = nrm(ks[0], (BATCH, SEQ, D_MODEL), 1.0)
    norm_mix_w = 1.0 + nrm(ks[1], (DEPTH, D_MODEL), 0.02)
    w_in = nrm(ks[2], (DEPTH, D_MODEL, C_IN), D_MODEL ** -0.5)
    conv_qk_w = nrm(ks[3], (DEPTH, CONV_W, 2 * MLSTM_WIDTH), CONV_W ** -0.5)
    conv_qk_b = nrm(ks[4], (DEPTH, 2 * MLSTM_WIDTH), 0.01)
    i_bias = nrm(ks[5], (DEPTH, MLSTM_HEADS), 0.1)
    f_bias = jnp.linspace(3.0, 6.0, MLSTM_HEADS, dtype=jnp.float32)[None, :] + nrm(ks[6], (DEPTH, MLSTM_HEADS), 0.1)
    gate_if_b = jnp.concatenate([i_bias, f_bias], axis=-1)
    hgrn_lb = nrm(ks[7], (DEPTH + 1, HGRN_WIDTH), 0.5)
    mlstm_norm_w = 1.0 + nrm(ks[8], (DEPTH, MLSTM_WIDTH), 0.02)
    hgrn_norm_w = 1.0 + nrm(ks[9], (DEPTH, HGRN_WIDTH), 0.02)
    w_branch_m = nrm(ks[10], (DEPTH, MLSTM_WIDTH, D_MODEL), MLSTM_WIDTH ** -0.5)
    w_branch_h = nrm(ks[11], (DEPTH, HGRN_WIDTH, D_MODEL), HGRN_WIDTH ** -0.5)
    w_out = nrm(ks[12], (DEPTH, D_MODEL, D_MODEL), D_MODEL ** -0.5)
    norm_ffn_w = 1.0 + nrm(ks[13], (DEPTH, D_MODEL), 0.02)
    router_group_w = nrm(ks[14], (DEPTH, D_MODEL, N_GROUPS), D_MODEL ** -0.5)
    router_group_b = nrm(ks[15], (DEPTH, N_GROUPS), 0.01)
    router_expert_w = nrm(ks[16], (DEPTH, D_MODEL, N_EXPERTS), D_MODEL ** -0.5)
    router_expert_b = nrm(ks[17], (DEPTH, N_EXPERTS), 0.01)
    w_gate = nrm(ks[18], (DEPTH, N_EXPERTS, D_MODEL, D_EXPERT), D_MODEL ** -0.5)
    w_up = nrm(ks[19], (DEPTH, N_EXPERTS, D_MODEL, D_EXPERT), D_MODEL ** -0.5)
    w_down = nrm(ks[20], (DEPTH, N_EXPERTS, D_EXPERT, D_MODEL), D_EXPERT ** -0.5)
    norm_final_w = 1.0 + nrm(ks[21], (D_MODEL,), 0.02)
    return {'x': x, 'norm_mix_w': norm_mix_w, 'w_in': w_in, 'conv_qk_w': conv_qk_w, 'conv_qk_b': conv_qk_b,
            'gate_if_b': gate_if_b, 'hgrn_lb': hgrn_lb, 'mlstm_norm_w': mlstm_norm_w, 'hgrn_norm_w': hgrn_norm_w,
            'w_branch_m': w_branch_m, 'w_branch_h': w_branch_h, 'w_out': w_out, 'norm_ffn_w': norm_ffn_w,
            'router_group_w': router_group_w, 'router_group_b': router_group_b,
            'router_expert_w': router_expert_w, 'router_expert_b': router_expert_b,
            'w_gate': w_gate, 'w_up': w_up, 'w_down': w_down, 'norm_final_w': norm_final_w}


def reference(x, norm_mix_w, w_in, conv_qk_w, conv_qk_b, gate_if_b, hgrn_lb, mlstm_norm_w, hgrn_norm_w,
              w_branch_m, w_branch_h, w_out, norm_ffn_w, router_group_w, router_group_b,
              router_expert_w, router_expert_b, w_gate, w_up, w_down, norm_final_w):
    bsz, s, d = x.shape
    lb_all = jnp.cumsum(jax.nn.softmax(hgrn_lb.astype(jnp.float32), axis=0), axis=0)
    for l in range(DEPTH):
        h = _rmsnorm(x, norm_mix_w[l])
        x = x + _hybrid_mixer(h, w_in[l], conv_qk_w[l], conv_qk_b[l], gate_if_b[l], lb_all[l],
                              mlstm_norm_w[l], hgrn_norm_w[l], w_branch_m[l], w_branch_h[l], w_out[l])
        h = _rmsnorm(x, norm_ffn_w[l])
        y = _hier_moe(h.reshape(bsz * s, d), router_group_w[l], router_group_b[l], router_expert_w[l],
                      router_expert_b[l], w_gate[l], w_up[l], w_down[l])
        x = x + y.reshape(bsz, s, d)
    return _rmsnorm(x, norm_final_w)
```

```python
import contextlib
import numpy as np
import concourse.bass as bass
import concourse.mybir as mybir
from concourse.bass_utils import run_bass_kernel_spmd

F32, BF16, I32 = mybir.dt.float32, mybir.dt.bfloat16, mybir.dt.int32
AF = mybir.ActivationFunctionType
ALU = mybir.AluOpType
AX = mybir.AxisListType

D = 4096
S = 4096
NCORE = 8
EPS = 1e-6
CAP = 1536
XW = D + 16
ENGS = ("pe", "act", "dve", "pool", "sp")
ROT = 30000
NDMA = 6


class Buf:
    __slots__ = ("name", "w", "r")

    def __init__(self, name=""):
        self.name = name
        self.w = None
        self.r = {}


class Sched:
    def __init__(self, nc, stack):
        self.nc = nc
        self.stack = stack
        self.streams = {e: [] for e in ENGS}
        self.cnt = {e: 0 for e in ENGS}
        self.prog = {e: [] for e in ENGS}
        self.seen = {e: {} for e in ENGS}
        self.dma_sems = {e: [] for e in ENGS}
        self.dma_val = {}
        self.dma_rr = {e: 0 for e in ENGS}
        self.nsem = 0

    def new_sem(self, name):
        self.nsem += 1
        return self.stack.enter_context(self.nc.semaphore(name))

    def _prog_event(self, e):
        idx = self.cnt[e]
        self.cnt[e] += 1
        r = idx // ROT
        while len(self.prog[e]) <= r:
            self.prog[e].append(self.new_sem("p_%s_%d" % (e, len(self.prog[e]))))
        return (self.prog[e][r], idx % ROT + 1, e)

    def _waits(self, e, deps):
        best = {}
        for (sem, val, src) in deps:
            if src == "pe" and e == "pe":
                continue
            if self.seen[e].get(sem, 0) >= val:
                continue
            if sem not in best or best[sem] < val:
                best[sem] = val
        out = []
        for sem, val in best.items():
            self.seen[e][sem] = val
            out.append((sem, val))
        return out

    def _deps(self, reads, writes):
        deps = []
        for b in reads:
            if b.w is not None:
                deps.append(b.w)
        for b in writes:
            if b.w is not None:
                deps.append(b.w)
            deps.extend(b.r.values())
        return deps

    def _mark(self, ev, reads, writes):
        for b in reads:
            old = b.r.get(ev[0])
            if old is None or old[1] < ev[1]:
                b.r[ev[0]] = ev
        for b in writes:
            b.w = ev
            b.r = {}

    def op(self, e, fn, reads=(), writes=()):
        waits = self._waits(e, self._deps(reads, writes))
        ev = self._prog_event(e)
        self.streams[e].append((waits, fn, ev[0], 1))
        self._mark(ev, reads, writes)
        return ev

    def dma(self, q, fn, reads=(), writes=(), inc=16):
        deps = self._deps(reads, writes)
        sems = self.dma_sems[q]
        i = self.dma_rr[q] % NDMA
        self.dma_rr[q] += 1
        if len(sems) <= i:
            s = self.new_sem("d_%s_%d" % (q, i))
            sems.append(s)
            self.dma_val[s] = 0
        sem = sems[i]
        prev = self.dma_val[sem]
        if prev > 0:
            deps.append((sem, prev, "dma"))
        val = prev + inc
        self.dma_val[sem] = val
        waits = self._waits(q, deps)
        ev = (sem, val, "dma")
        self.streams[q].append((waits, fn, sem, inc))
        self._mark(ev, reads, writes)
        return ev

    def barrier(self):
        evs = []
        for e in ENGS:
            c = self.cnt[e]
            if c > 0:
                r = (c - 1) // ROT
                evs.append((self.prog[e][r], (c - 1) % ROT + 1, "x"))
        for s, v in self.dma_val.items():
            if v > 0:
                evs.append((s, v, "dma"))
        for e in ENGS:
            waits = self._waits(e, evs)
            if waits:
                self.streams[e].append((waits, None, None, 0))

    def emit(self):
        nc = self.nc
        streams = self.streams
        self.streams = {e: [] for e in ENGS}

        def run(eng, lst):
            for (waits, fn, sem, inc) in lst:
                for (s, v) in waits:
                    eng.wait_ge(s, v)
                if fn is not None:
                    fn(eng).then_inc(sem, inc)

        with nc.Block() as block:
            block.tensor(lambda eng: run(eng, streams["pe"]))
            block.scalar(lambda eng: run(eng, streams["act"]))
            block.vector(lambda eng: run(eng, streams["dve"]))
            block.gpsimd(lambda eng: run(eng, streams["pool"]))
            block.sync(lambda eng: run(eng, streams["sp"]))


class Ctx:
    pass


def _psum_pool(nc, stack, S_):
    banks = []
    for i in range(6):
        t = stack.enter_context(nc.psum_tensor("psf%d" % i, [128, 512], F32))
        banks.append((t, Buf("psf%d" % i)))
    bb = []
    for i in range(2):
        t = stack.enter_context(nc.psum_tensor("psb%d" % i, [128, 1024], BF16))
        bb.append((t, Buf("psb%d" % i)))
    st = {"f": 0, "b": 0}

    def getf():
        st["f"] += 1
        return banks[st["f"] % 6]

    def getb():
        st["b"] += 1
        return bb[st["b"] % 2]

    return getf, getb


_SBN = [0]


def _sb(nc, stack, name, shape, dt):
    _SBN[0] += 1
    return stack.enter_context(nc.sbuf_tensor("s%d_%s" % (_SBN[0], name), list(shape), dt))


def stage_norm_T(nc, S_, C, src, nw_dram, dst, ntok):
    with contextlib.ExitStack() as st:
        getf, getb = C.getf, C.getb
        xs = [_sb(nc, st, "n_xs%d" % i, [128, D], F32) for i in range(2)]
        xsb = [Buf() for _ in range(2)]
        xn = [_sb(nc, st, "n_xn%d" % i, [128, D], BF16) for i in range(2)]
        xnb = [Buf() for _ in range(2)]
        nw = _sb(nc, st, "n_nw", [128, D], F32)
        nwb = Buf()
        hT = [_sb(nc, st, "n_hT%d" % i, [128, 32, 512], BF16) for i in range(2)]
        hTb = [Buf() for _ in range(2)]
        ss = _sb(nc, st, "n_ss", [128, 8], F32)
        ssb = [Buf() for _ in range(4)]
        S_.barrier()
        S_.dma("sp", lambda e: e.dma_start(out=nw[:], in_=nw_dram), writes=[nwb])
        for ti in range(ntok // 128):
            tt, ts = ti // 4, ti % 4
            k = ti % 2
            S_.dma("sp", lambda e, k=k, ti=ti: e.dma_start(out=xs[k][:], in_=src[ti * 128:(ti + 1) * 128, :]),
                   writes=[xsb[k]])
            sb_ = ssb[ti % 4]
            c0 = (ti % 4) * 2
            S_.op("act", lambda e, k=k, c0=c0: e.activation(out=xn[k][:], in_=xs[k][:], func=AF.Square,
                                                            accum_out=ss[:, c0:c0 + 1]),
                  reads=[xsb[k]], writes=[xnb[k], sb_])
            S_.op("act", lambda e, c0=c0: e.activation(out=ss[:, c0 + 1:c0 + 2], in_=ss[:, c0:c0 + 1], func=AF.Ln,
                                                       bias=C.epsc[:, 0:1], scale=1.0 / D), reads=[sb_, C.identb], writes=[sb_])
            S_.op("act", lambda e, c0=c0: e.activation(out=ss[:, c0 + 1:c0 + 2], in_=ss[:, c0 + 1:c0 + 2], func=AF.Exp,
                                                       scale=-0.5), reads=[sb_], writes=[sb_])
            S_.op("dve", lambda e, k=k, c0=c0: e.scalar_tensor_tensor(out=xn[k][:], in0=xs[k][:],
                                                                      scalar=ss[:, c0 + 1:c0 + 2], in1=nw[:],
                                                                      op0=ALU.mult, op1=ALU.mult),
                  reads=[xsb[k], sb_, nwb], writes=[xnb[k]])
            hb = (tt % 2)
            for g in range(4):
                pt, pb = getb()
                for q in range(8):
                    kc = g * 8 + q
                    S_.op("pe", lambda e, pt=pt, q=q, kc=kc, k=k: e.transpose(
                        out=pt[:, q * 128:(q + 1) * 128], in_=xn[k][:, kc * 128:(kc + 1) * 128], identity=C.ident[:]),
                        reads=[xnb[k], C.identb], writes=[pb])
                eng = "act" if g % 2 == 0 else "dve"
                if eng == "act":
                    S_.op("act", lambda e, pt=pt, g=g, hb=hb, ts=ts: e.copy(
                        out=hT[hb][:, g * 8:(g + 1) * 8, ts * 128:(ts + 1) * 128],
                        in_=pt[:, :].rearrange("p (a b) -> p a b", b=128)), reads=[pb], writes=[hTb[hb]])
                else:
                    S_.op("dve", lambda e, pt=pt, g=g, hb=hb, ts=ts: e.tensor_copy(
                        out=hT[hb][:, g * 8:(g + 1) * 8, ts * 128:(ts + 1) * 128],
                        in_=pt[:, :].rearrange("p (a b) -> p a b", b=128)), reads=[pb], writes=[hTb[hb]])
            if ts == 3:
                S_.dma("sp", lambda e, hb=hb, tt=tt: e.dma_start(
                    out=dst[tt, :, :], in_=hT[hb][:, :, :].rearrange("p a b -> p (a b)")),
                    reads=[hTb[hb]], writes=[C.dramb])
        S_.barrier()
        S_.emit()


def stage_mixer(nc, S_, C, I, ntiles=8, dbg=None):
    with contextlib.ExitStack() as st:
        getf, getb = C.getf, C.getb
        sb = lambda name, shape, dt: _sb(nc, st, "m_" + name, shape, dt)
        hT = sb("hT", [128, 32, 512], BF16); hTb = Buf()
        W = [sb("W%d" % i, [128, 32, 256], BF16) for i in range(2)]; Wb = [Buf(), Buf()]
        wg = sb("wg", [128, 32, 4], BF16); wgb = Buf()
        FA = [sb("FA%d" % i, [128, 2, 512], BF16) for i in range(2)]; FAb = [Buf(), Buf()]
        FB = [sb("FB%d" % i, [128, 2, 512], BF16) for i in range(2)]; FBb = [Buf(), Buf()]
        FBf = [sb("FBf%d" % i, [128, 2, 512], F32) for i in range(2)]
        TA = [sb("TA%d" % i, [128, 4, 258], BF16) for i in range(2)]; TAb = [Buf(), Buf()]
        TB = [sb("TB%d" % i, [128, 4, 256], BF16) for i in range(2)]; TBb = [Buf(), Buf()]
        hist = sb("hist", [128, 8, 4], F32); histb = [Buf() for _ in range(8)]
        pre = [sb("pre%d" % i, [128, 516], F32) for i in range(2)]; preb = [Buf(), Buf()]
        acc = [sb("acc%d" % i, [128, 512], F32) for i in range(2)]; accb = [Buf(), Buf()]
        sg = [sb("sg%d" % i, [128, 512], F32) for i in range(2)]; sgb = [Buf(), Buf()]
        ex = [sb("ex%d" % i, [128, 512], F32) for i in range(3)]; exb = [Buf() for _ in range(3)]
        eg = sb("eg", [128, 2, 8], F32); egb = Buf()
        qe0 = sb("qe0", [128, 2, 512], BF16); qe1 = sb("qe1", [128, 2, 512], BF16)
        ke = sb("ke", [128, 2, 512], BF16); kg = sb("kg", [128, 2, 512], BF16)
        qeb, keb, kgb = Buf(), Buf(), Buf()
        kgT = sb("kgT", [128, 4, 256], BF16); kgTb = Buf()
        AT = [sb("AT%d" % i, [128, 128], BF16) for i in range(2)]; ATb = [Buf(), Buf()]
        Cst = sb("Cst", [128, 8, 257], F32)
        Cb = sb("Cb", [128, 8, 2, 258], BF16)
        Cstb = [Buf() for _ in range(8)]; Cbb = [[Buf(), Buf()] for _ in range(8)]
        numS = [sb("numS%d" % i, [128, 257], F32) for i in range(2)]; numSb = [Buf(), Buf()]
        ytmp = [sb("ytmp%d" % i, [128, 256], F32) for i in range(2)]; ytmpb = [Buf(), Buf()]
        sm = sb("sm", [128, 16], F32); smb = [Buf(), Buf()]
        outst = [sb("outst%d" % i, [128, 4, 256], BF16) for i in range(2)]; outstb = [Buf(), Buf()]
        maskUT = sb("maskUT", [128, 128], F32); rst = sb("rst", [128, 512], F32)
        mnw = sb("mnw", [128, 512], F32); hnw = sb("hnw", [128, 512], F32)
        cw = sb("cw", [128, 8, 4], F32); cbias = sb("cbias", [128, 8], F32)
        lbt = sb("lbt", [128, 4, 4], F32)
        gbt = sb("gbt", [1, 8], F32)
        rows = sb("rows", [1, 6, 512], F32); rowsb = [Buf() for _ in range(6)]
        ones1 = sb("ones1", [1, 128], F32)
        constb = Buf()

        S_.barrier()
        S_.dma("sp", lambda e: e.dma_start(out=maskUT[:], in_=I["maskUT"]), writes=[constb])
        S_.dma("sp", lambda e: e.dma_start(out=rst[:], in_=I["rst"]), writes=[constb])
        S_.dma("sp", lambda e: e.dma_start(out=mnw[:], in_=I["mnw"]), writes=[constb])
        S_.dma("sp", lambda e: e.dma_start(out=hnw[:], in_=I["hnw"]), writes=[constb])
        S_.dma("sp", lambda e: e.dma_start(out=cw[:], in_=I["cw"]), writes=[constb])
        S_.dma("sp", lambda e: e.dma_start(out=cbias[:], in_=I["cb"]), writes=[constb])
        S_.dma("sp", lambda e: e.dma_start(out=lbt[:, :, 0:2], in_=I["lb"]), writes=[constb])
        S_.dma("sp", lambda e: e.dma_start(out=gbt[:], in_=I["gb"]), writes=[constb])
        S_.dma("pool", lambda e: e.dma_start(out=wg[:], in_=I["wg"]), writes=[wgb])
        S_.op("dve", lambda e: e.memset(ones1[:], 1.0), writes=[constb])
        S_.op("dve", lambda e: e.memset(hist[:], 0.0), writes=histb)
        S_.op("dve", lambda e: e.memset(Cst[:], 0.0), writes=Cstb)
        S_.op("dve", lambda e: e.memset(Cb[:], 0.0), writes=[b for p in Cbb for b in p])
        S_.op("dve", lambda e: e.memset(qe0[:], 0.0), writes=[qeb])
        S_.op("dve", lambda e: e.memset(qe1[:], 0.0), writes=[qeb])
        for i in range(2):
            S_.op("dve", lambda e, i=i: e.memset(TA[i][:], 1.0), writes=[TAb[i]])
        S_.op("dve", lambda e: e.tensor_tensor(out=lbt[:, :, 2], in0=lbt[:, :, 0], in1=lbt[:, :, 1], op=ALU.subtract),
              reads=[constb], writes=[constb])
        S_.op("act", lambda e: e.activation(out=lbt[:, :, 2], in_=lbt[:, :, 2], func=AF.Sigmoid),
              reads=[constb], writes=[constb])
        S_.op("dve", lambda e: e.tensor_scalar(out=lbt[:, :, 3], in0=lbt[:, :, 2], scalar1=-1.0, scalar2=1.0,
                                               op0=ALU.mult, op1=ALU.add), reads=[constb], writes=[constb])
        S_.op("dve", lambda e: e.tensor_scalar(out=gbt[:, 4:6], in0=gbt[:, 2:4], scalar1=-1.0, scalar2=None,
                                               op0=ALU.mult), reads=[constb], writes=[constb])

        wcount = [0]

        def load_w(t):
            k = wcount[0] % 2
            wcount[0] += 1
            S_.dma("pool", lambda e, k=k, t=t: e.dma_start(out=W[k][:], in_=I["w1"][t]), writes=[Wb[k]])
            return k

        cnt = {"acc": 0, "ex": 0, "num": 0, "AT": 0}

        def gla(tt, u, FAq, FBk, FBk_b, TAv, TAv_b, nd, dv, hsel, sidx, ebuf, eamb, egba, egap, post):
            qv = lambda t_: t_.rearrange("p (a c l) -> p a c l", c=2, l=64)
            for dc in range(nd):
                S_.op("dve", lambda e, dc=dc: e.tensor_tensor(out=qv(qe0[:, dc, :])[:, :, 0, :], in0=qv(FAq[:, dc, :])[:, :, 0, :],
                                                             in1=qv(ebuf)[:, :, 0, :], op=ALU.mult),
                      reads=[FAb[u], exb[0]], writes=[qeb])
                S_.op("dve", lambda e, dc=dc: e.tensor_tensor(out=qv(qe1[:, dc, :])[:, :, 1, :], in0=qv(FAq[:, dc, :])[:, :, 1, :],
                                                             in1=qv(ebuf)[:, :, 1, :], op=ALU.mult),
                      reads=[FAb[u], exb[0]], writes=[qeb])
                S_.op("pool", lambda e, dc=dc: e.tensor_tensor(out=ke[:, dc, :], in0=FBk[:, dc, :], in1=eamb, op=ALU.mult),
                      reads=[FBk_b, exb[1]], writes=[keb])
                S_.op("pool", lambda e, dc=dc: e.tensor_tensor(out=kg[:, dc, :], in0=FBk[:, dc, :], in1=egba, op=ALU.mult),
                      reads=[FBk_b, exb[2]], writes=[kgb])
            for ts in range(4):
                pt, pb = getb()
                for dc in range(nd):
                    S_.op("pe", lambda e, pt=pt, dc=dc, ts=ts: e.transpose(out=pt[:, dc * 128:(dc + 1) * 128],
                                                                          in_=kg[:, dc, ts * 128:(ts + 1) * 128],
                                                                          identity=C.ident[:]),
                          reads=[kgb, C.identb], writes=[pb])
                S_.op("act", lambda e, pt=pt, ts=ts: e.copy(out=kgT[:, ts, 0:nd * 128], in_=pt[:, 0:nd * 128]),
                      reads=[pb], writes=[kgTb])
            for ts in range(4):
                tok = slice(ts * 128, (ts + 1) * 128)
                pa, pab = getf()
                n_mm = 2 * nd
                i_mm = 0
                for dc in range(nd):
                    for qq in (qe0, qe1):
                        S_.op("pe", lambda e, pa=pa, dc=dc, qq=qq, i_mm=i_mm, tok=tok: e.matmul(
                            pa[:, 0:128], lhsT=ke[:, dc, tok], rhs=qq[:, dc, tok], start=(i_mm == 0), stop=(i_mm == n_mm - 1)),
                            reads=[keb, qeb], writes=[pab])
                        i_mm += 1
                ai = cnt["AT"] % 2
                cnt["AT"] += 1
                S_.op("dve", lambda e, pa=pa, ai=ai: e.tensor_tensor(out=AT[ai][:], in0=pa[:, 0:128], in1=maskUT[:], op=ALU.mult),
                      reads=[pab, constb], writes=[ATb[ai]])
                v = TAv(ts)
                for c in range(2):
                    rowsl = slice(c * 64, (c + 1) * 64)
                    if c == 1:
                        po, pob = getf()
                        S_.op("pe", lambda e, po=po, ai=ai, v=v: e.matmul(po[:, 0:dv], lhsT=AT[ai][:], rhs=v, start=True, stop=False),
                              reads=[ATb[ai], TAv_b], writes=[pob])
                        for dc in range(nd):
                            si = sidx[dc]
                            S_.op("pe", lambda e, po=po, dc=dc, si=si, tok=tok: e.matmul(po[:, 0:dv], lhsT=qe0[:, dc, tok], rhs=Cb[:, si, 0, 0:dv],
                                                                               start=False, stop=False),
                                  reads=[qeb, Cbb[si][0]], writes=[pob])
                            S_.op("pe", lambda e, po=po, dc=dc, si=si, tok=tok: e.matmul(po[:, 0:dv], lhsT=qe1[:, dc, tok], rhs=Cb[:, si, 1, 0:dv],
                                                                               start=False, stop=(dc == nd - 1)),
                                  reads=[qeb, Cbb[si][1]], writes=[pob])
                        post(ts, po, pob)
                    for dc in range(nd):
                        si = sidx[dc]
                        pu, pub = getf()
                        S_.op("pe", lambda e, pu=pu, dc=dc, rowsl=rowsl, v=v, ts=ts: e.matmul(
                            pu[:, 0:dv], lhsT=kgT[rowsl, ts, dc * 128:(dc + 1) * 128], rhs=v[rowsl, :], start=True, stop=True),
                            reads=[kgTb, TAv_b], writes=[pub])
                        ch = ts * 2 + c
                        S_.op("dve", lambda e, pu=pu, si=si, ch=ch, dc=dc: e.scalar_tensor_tensor(
                            out=Cst[:, si, 0:dv], in0=Cst[:, si, 0:dv], scalar=egap(dc)[:, ch:ch + 1], in1=pu[:, 0:dv],
                            op0=ALU.mult, op1=ALU.add), reads=[pub, egb, Cstb[si]], writes=[Cstb[si]])
                        tgt = 1 if c == 0 else 0
                        S_.op("act", lambda e, si=si, tgt=tgt: e.copy(out=Cb[:, si, tgt, 0:dv], in_=Cst[:, si, 0:dv]),
                              reads=[Cstb[si]], writes=[Cbb[si][tgt]])

        def small(i):
            return sm[:, i:i + 1]

        for tt in range(ntiles):
            S_.dma("sp", lambda e, tt=tt: e.dma_start(out=hT[:, :, :].rearrange("p a b -> p (a b)"), in_=C.hT1[tt, :, :]),
                   reads=[C.dramb], writes=[hTb])
            for gi in range(4):
                pg, pgb = getf()
                for kc in range(32):
                    S_.op("pe", lambda e, pg=pg, gi=gi, kc=kc: e.matmul(pg[0:1, :], lhsT=wg[:, kc, gi:gi + 1], rhs=hT[:, kc, :],
                                                                       start=(kc == 0), stop=(kc == 31)),
                          reads=[wgb, hTb], writes=[pgb])
                h = gi % 2
                if gi < 2:
                    S_.op("act", lambda e, pg=pg, h=h: e.activation(out=rows[:, h, :], in_=pg[0:1, :], func=AF.Identity,
                                                                    bias=gbt[:, h:h + 1], scale=1.0),
                          reads=[pgb, constb], writes=[rowsb[h]])
                else:
                    S_.op("act", lambda e, pg=pg, h=h: e.activation(out=rows[:, 4, :], in_=pg[0:1, :], func=AF.Exp,
                                                                    bias=gbt[:, 4 + h:5 + h], scale=-1.0),
                          reads=[pgb, constb], writes=[rowsb[4]])
                    S_.op("act", lambda e: e.activation(out=rows[:, 4, :], in_=rows[:, 4, :], func=AF.Ln, bias=1.0, scale=1.0),
                          reads=[rowsb[4]], writes=[rowsb[4]])
                    S_.op("dve", lambda e, h=h: e.tensor_tensor_scan(out=rows[:, 2 + h, :], data0=rst[0:1, :], data1=rows[:, 4, :],
                                                                     initial=0.0, op0=ALU.mult, op1=ALU.subtract),
                          reads=[rowsb[4], constb], writes=[rowsb[2 + h]])
            for u in range(4):
                ub = u % 2
                mls = u < 2
                for part in range(4):
                    k = load_w(u * 4 + part)
                    if part < 2:
                        for cc in range(2):
                            pp, ppb = getf()
                            for kc in range(32):
                                S_.op("pe", lambda e, pp=pp, k=k, cc=cc, kc=kc: e.matmul(
                                    pp[:, :], lhsT=W[k][:, kc, cc * 128:(cc + 1) * 128], rhs=hT[:, kc, :],
                                    start=(kc == 0), stop=(kc == 31)), reads=[Wb[k], hTb], writes=[ppb])
                            if mls:
                                ci = u * 4 + part * 2 + cc
                                cwi = part * 4 + u * 2 + cc
                                pk = cnt["acc"] % 2
                                cnt["acc"] += 1
                                S_.op("act", lambda e, pp=pp, pk=pk: e.copy(out=pre[pk][:, 3:515], in_=pp[:, :]),
                                      reads=[ppb], writes=[preb[pk]])
                                S_.op("pool", lambda e, pk=pk, cwi=cwi: e.tensor_copy(out=pre[pk][:, 0:3], in_=hist[:, cwi, 0:3]),
                                      reads=[histb[cwi]], writes=[preb[pk]])
                                S_.op("dve", lambda e, pk=pk, cwi=cwi: e.tensor_scalar(
                                    out=acc[pk][:], in0=pre[pk][:, 3:515], scalar1=cw[:, cwi, 3:4], scalar2=cbias[:, cwi:cwi + 1],
                                    op0=ALU.mult, op1=ALU.add), reads=[preb[pk], constb], writes=[accb[pk]])
                                for tap in range(3):
                                    S_.op("dve", lambda e, pk=pk, cwi=cwi, tap=tap: e.scalar_tensor_tensor(
                                        out=acc[pk][:], in0=pre[pk][:, tap:tap + 512], scalar=cw[:, cwi, tap:tap + 1], in1=acc[pk][:],
                                        op0=ALU.mult, op1=ALU.add), reads=[preb[pk], constb, accb[pk]], writes=[accb[pk]])
                                S_.op("pool", lambda e, pk=pk, cwi=cwi: e.tensor_copy(out=hist[:, cwi, 0:3], in_=pre[pk][:, 512:515]),
                                      reads=[preb[pk]], writes=[histb[cwi]])
                                S_.op("act", lambda e, pk=pk: e.activation(out=sg[pk][:], in_=acc[pk][:], func=AF.Sigmoid),
                                      reads=[accb[pk]], writes=[sgb[pk]])
                                dstt = FA[ub] if part == 0 else FB[ub]
                                dstb = FAb[ub] if part == 0 else FBb[ub]
                                cmul = 1.0 if part == 0 else 1.0 / 16.0
                                S_.op("dve", lambda e, pk=pk, dstt=dstt, cc=cc, cmul=cmul: e.scalar_tensor_tensor(
                                    out=dstt[:, cc, :], in0=acc[pk][:], scalar=cmul, in1=sg[pk][:], op0=ALU.mult, op1=ALU.mult),
                                    reads=[accb[pk], sgb[pk]], writes=[dstb])
                            else:
                                hh = (u - 2) * 2 + cc
                                pk = cnt["acc"] % 2
                                cnt["acc"] += 1
                                if part == 0:
                                    S_.op("act", lambda e, pp=pp, pk=pk: e.activation(out=sg[pk][:], in_=pp[:, :], func=AF.Sigmoid),
                                          reads=[ppb], writes=[sgb[pk]])
                                    S_.op("dve", lambda e, pp=pp, pk=pk, cc=cc, ub=ub: e.tensor_tensor(
                                        out=FA[ub][:, cc, :], in0=pp[:, :], in1=sg[pk][:], op=ALU.mult),
                                        reads=[ppb, sgb[pk]], writes=[FAb[ub]])
                                else:
                                    S_.op("act", lambda e, pp=pp, pk=pk: e.activation(out=sg[pk][:], in_=pp[:, :], func=AF.Sigmoid),
                                          reads=[ppb], writes=[sgb[pk]])
                                    S_.op("dve", lambda e, pk=pk, hh=hh: e.tensor_scalar(
                                        out=acc[pk][:], in0=sg[pk][:], scalar1=lbt[:, hh, 3:4], scalar2=lbt[:, hh, 2:3],
                                        op0=ALU.mult, op1=ALU.add), reads=[sgb[pk], constb], writes=[accb[pk]])
                                    S_.op("pool", lambda e, pk=pk, cc=cc, ub=ub: e.tensor_scalar(
                                        out=FB[ub][:, cc, :], in0=acc[pk][:], scalar1=-1.0, scalar2=1.0, op0=ALU.mult, op1=ALU.add),
                                        reads=[accb[pk]], writes=[FBb[ub]])
                                    S_.op("act", lambda e, pk=pk: e.activation(out=sg[pk][:], in_=acc[pk][:], func=AF.Ln),
                                          reads=[accb[pk]], writes=[sgb[pk]])
                                    S_.op("dve", lambda e, pk=pk, cc=cc, ub=ub: e.tensor_tensor_scan(
                                        out=FBf[ub][:, cc, :], data0=rst[:], data1=sg[pk][:], initial=0.0, op0=ALU.mult, op1=ALU.add),
                                        reads=[sgb[pk], constb], writes=[FBb[ub]])
                    else:
                        for ts in range(4):
                            pp, ppb = getf()
                            for kc in range(32):
                                S_.op("pe", lambda e, pp=pp, k=k, ts=ts, kc=kc: e.matmul(
                                    pp[:, 0:256], lhsT=hT[:, kc, ts * 128:(ts + 1) * 128], rhs=W[k][:, kc, :],
                                    start=(kc == 0), stop=(kc == 31)), reads=[Wb[k], hTb], writes=[ppb])
                            if part == 2:
                                S_.op("act", lambda e, pp=pp, ts=ts, ub=ub: e.copy(out=TA[ub][:, ts, 0:256], in_=pp[:, 0:256]),
                                      reads=[ppb], writes=[TAb[ub]])
                            else:
                                S_.op("act", lambda e, pp=pp, ts=ts, ub=ub: e.activation(out=TB[ub][:, ts, :], in_=pp[:, 0:256], func=AF.Sigmoid),
                                      reads=[ppb], writes=[TBb[ub]])
                ob = outstb[ub]
                if mls:
                    h = u
                    S_.op("dve", lambda e, h=h: e.tensor_tensor(out=rows[:, 5, :], in0=rows[:, h, :], in1=rows[:, 2 + h, :], op=ALU.subtract),
                          reads=[rowsb[h], rowsb[2 + h]], writes=[rowsb[5]])
                    r3 = lambda ap: ap.rearrange("p (c l) -> p c l", l=64)
                    S_.op("dve", lambda e, h=h: e.tensor_tensor(
                        out=r3(rows[:, 4, :]), in0=r3(rows[:, 5, :]), in1=r3(rows[:, 2 + h, :])[:, :, 63:64].to_broadcast([1, 8, 64]),
                        op=ALU.add), reads=[rowsb[5], rowsb[2 + h]], writes=[rowsb[4]])
                    for (ri, xi, rb_) in ((2 + h, 0, rowsb[2 + h]), (5, 1, rowsb[5]), (4, 2, rowsb[4])):
                        pbc, pbcb = getf()
                        S_.op("pe", lambda e, pbc=pbc, ri=ri: e.matmul(pbc[:, :], lhsT=ones1[:, :], rhs=rows[:, ri, :], start=True, stop=True),
                              reads=[rb_, constb], writes=[pbcb])
                        S_.op("act", lambda e, pbc=pbc, xi=xi: e.activation(out=ex[xi][:], in_=pbc[:, :], func=AF.Exp),
                              reads=[pbcb], writes=[exb[xi]])
                    S_.op("pool", lambda e: e.tensor_copy(out=eg[:, 0, :], in_=ex[0][:].rearrange("p (c l) -> p c l", l=64)[:, :, 63]),
                          reads=[exb[0]], writes=[egb])

                    def post_m(ts, po, pob, h=h, ub=ub, ob=ob):
                        ni = cnt["num"] % 2
                        cnt["num"] += 1
                        o0 = ni * 8
                        S_.op("act", lambda e: e.copy(out=numS[ni][:], in_=po[:, 0:257]), reads=[pob], writes=[numSb[ni]])
                        S_.op("act", lambda e: e.activation(out=small(o0), in_=numS[ni][:, 256:257], func=AF.Abs),
                              reads=[numSb[ni]], writes=[smb[ni]])
                        S_.op("dve", lambda e: e.tensor_scalar(out=small(o0), in0=small(o0), scalar1=1.0, scalar2=None,
                                                               op0=ALU.max), reads=[smb[ni]], writes=[smb[ni]])
                        S_.op("dve", lambda e: e.reciprocal(out=small(o0 + 1), in_=small(o0)), reads=[smb[ni]], writes=[smb[ni]])
                        S_.op("act", lambda e: e.activation(out=ytmp[ni][:], in_=numS[ni][:, 0:256], func=AF.Square,
                                                            accum_out=small(o0 + 2)), reads=[numSb[ni]], writes=[ytmpb[ni], smb[ni]])
                        S_.op("dve", lambda e: e.tensor_tensor(out=small(o0 + 3), in0=small(o0 + 1), in1=small(o0 + 1), op=ALU.mult),
                              reads=[smb[ni]], writes=[smb[ni]])
                        S_.op("dve", lambda e: e.tensor_tensor(out=small(o0 + 3), in0=small(o0 + 3), in1=small(o0 + 2), op=ALU.mult),
                              reads=[smb[ni]], writes=[smb[ni]])
                        S_.op("act", lambda e: e.activation(out=small(o0 + 3), in_=small(o0 + 3), func=AF.Ln, bias=C.epsc[:, 0:1],
                                                            scale=1.0 / 256), reads=[smb[ni], C.identb], writes=[smb[ni]])
                        S_.op("act", lambda e: e.activation(out=small(o0 + 3), in_=small(o0 + 3), func=AF.Exp, scale=-0.5),
                              reads=[smb[ni]], writes=[smb[ni]])
                        S_.op("dve", lambda e: e.tensor_tensor(out=small(o0 + 3), in0=small(o0 + 3), in1=small(o0 + 1), op=ALU.mult),
                              reads=[smb[ni]], writes=[smb[ni]])
                        S_.op("dve", lambda e: e.scalar_tensor_tensor(out=ytmp[ni][:], in0=numS[ni][:, 0:256], scalar=small(o0 + 3),
                                                                      in1=mnw[:, h * 256:(h + 1) * 256], op0=ALU.mult, op1=ALU.mult),
                              reads=[numSb[ni], smb[ni], constb], writes=[ytmpb[ni]])
                        S_.op("pool", lambda e: e.tensor_tensor(out=outst[ub][:, ts, :], in0=ytmp[ni][:], in1=TB[ub][:, ts, :], op=ALU.mult),
                              reads=[ytmpb[ni], TBb[ub]], writes=[ob])

                    gla(tt, ub, FA[ub], FB[ub], FBb[ub], lambda ts: TA[ub][:, ts, 0:257], TAb[ub], 2, 257, h, [h * 2, h * 2 + 1],
                        ex[0][:], ex[1][:], ex[2][:], lambda dc: eg[:, 0, :], post_m)
                    col0 = h * 256
                else:
                    for cc in range(2):
                        hh = (u - 2) * 2 + cc
                        bsrc = FBf[ub][:, cc, :]
                        S_.op("act", lambda e, bsrc=bsrc: e.activation(out=ex[0][:], in_=bsrc, func=AF.Exp), reads=[FBb[ub]], writes=[exb[0]])
                        S_.op("dve", lambda e, bsrc=bsrc: e.tensor_scalar(out=ex[1][:], in0=bsrc, scalar1=-1.0, scalar2=80.0,
                                                                          op0=ALU.mult, op1=ALU.min), reads=[FBb[ub]], writes=[exb[1]])
                        S_.op("act", lambda e: e.activation(out=ex[1][:], in_=ex[1][:], func=AF.Exp), reads=[exb[1]], writes=[exb[1]])
                        r3 = lambda ap: ap.rearrange("p (c l) -> p c l", l=64)
                        S_.op("dve", lambda e, bsrc=bsrc: e.tensor_tensor(
                            out=r3(ex[2][:]), in0=r3(bsrc)[:, :, 63:64].to_broadcast([128, 8, 64]), in1=r3(bsrc), op=ALU.subtract),
                            reads=[FBb[ub]], writes=[exb[2]])
                        S_.op("act", lambda e: e.activation(out=ex[2][:], in_=ex[2][:], func=AF.Exp), reads=[exb[2]], writes=[exb[2]])
                        S_.op("pool", lambda e, cc=cc: e.tensor_copy(out=eg[:, cc, :], in_=r3(ex[0][:])[:, :, 63]),
                              reads=[exb[0]], writes=[egb])

                        def post_h(ts, po, pob, hh=hh, cc=cc, ub=ub, ob=ob):
                            ni = cnt["num"] % 2
                            cnt["num"] += 1
                            o0 = ni * 8
                            S_.op("act", lambda e: e.activation(out=ytmp[ni][:, 0:128], in_=po[:, 0:128], func=AF.Square,
                                                                accum_out=small(o0 + 2)), reads=[pob], writes=[ytmpb[ni], smb[ni]])
                            S_.op("act", lambda e: e.activation(out=small(o0 + 3), in_=small(o0 + 2), func=AF.Ln, bias=C.epsc[:, 0:1],
                                                                scale=1.0 / 128), reads=[smb[ni], C.identb], writes=[smb[ni]])
                            S_.op("act", lambda e: e.activation(out=small(o0 + 3), in_=small(o0 + 3), func=AF.Exp, scale=-0.5),
                                  reads=[smb[ni]], writes=[smb[ni]])
                            S_.op("dve", lambda e: e.scalar_tensor_tensor(out=ytmp[ni][:, 0:128], in0=po[:, 0:128], scalar=small(o0 + 3),
                                                                          in1=hnw[:, hh * 128:(hh + 1) * 128], op0=ALU.mult, op1=ALU.mult),
                                  reads=[pob, smb[ni], constb], writes=[ytmpb[ni]])
                            S_.op("pool", lambda e: e.tensor_tensor(out=outst[ub][:, ts, cc * 128:(cc + 1) * 128], in0=ytmp[ni][:, 0:128],
                                                                    in1=TB[ub][:, ts, cc * 128:(cc + 1) * 128], op=ALU.mult),
                                  reads=[ytmpb[ni], TBb[ub]], writes=[ob])

                        gla(tt, ub, FA[ub][:, cc:cc + 1, :], FB[ub][:, cc:cc + 1, :], FBb[ub],
                            lambda ts, cc=cc: TA[ub][:, ts, cc * 128:(cc + 1) * 128], TAb[ub], 1, 128, hh, [4 + hh],
                            ex[0][:], ex[1][:], ex[2][:], lambda dc, cc=cc: eg[:, cc, :], post_h)
                    col0 = 512 + (u - 2) * 256
                S_.dma("sp", lambda e, ub=ub, tt=tt, col0=col0: e.dma_start(
                    out=C.cc1_in[tt * 512:(tt + 1) * 512, col0:col0 + 256].rearrange("(a p) c -> p a c", p=128), in_=outst[ub][:]),
                    reads=[ob], writes=[C.dramb])
        if dbg is not None:
            S_.barrier()
            for nm, t in (("FA1", FA[1]), ("FB1", FB[1]), ("FBf1", FBf[1]), ("TA1", TA[1]), ("TB1", TB[1]), ("ex0", ex[0]), ("ex1", ex[1]),
                          ("ex2", ex[2]), ("kgT", kgT), ("Cst", Cst), ("qe0", qe0), ("qe1", qe1), ("ke", ke), ("kg", kg), ("hT", hT), ("W0", W[0]), ("W1", W[1])):
                shp = list(t.shape)
                d_ = nc.dram_tensor("dbg_" + nm, shp, t.dtype, kind="ExternalOutput").ap()
                S_.dma("sp", lambda e, d_=d_, t=t: e.dma_start(out=d_, in_=t[:]))
        S_.barrier()
        S_.emit()


def build(stages=("p1",), debug=False, ntiles=8):
    nc = bass.Bass("TRN2", target_bir_lowering=False)
    I = {}

    def inp(name, shape, dt=F32):
        I[name] = nc.dram_tensor(name, list(shape), dt, kind="ExternalInput").ap()

    inp("ident", [128, 128]); inp("maskUT", [128, 128]); inp("rst", [128, 512])
    inp("xb", [S, D]); inp("nw1", [128, D]); inp("w1", [16, 128, 32, 256]); inp("wg", [128, 32, 4])
    inp("cw", [128, 8, 4]); inp("cb", [128, 8]); inp("lb", [128, 4, 2]); inp("gb", [1, 8])
    inp("mnw", [128, 512]); inp("hnw", [128, 512])
    C = Ctx()
    dbg_kind = "ExternalOutput" if debug else "Internal"
    C.hT1_t = nc.dram_tensor("hT1", [8, 128, 32 * 512], BF16, kind=dbg_kind)
    C.hT1 = C.hT1_t.ap()
    C.cc1_t = nc.dram_tensor("cc1_in", [S, 1024], BF16, kind=dbg_kind)
    C.cc1_in = C.cc1_t.ap()
    C.dramb = Buf("dram")
    with contextlib.ExitStack() as stack:
        S_ = Sched(nc, stack)
        C.getf, C.getb = _psum_pool(nc, stack, S_)
        C.ident = _sb(nc, stack, "ident", [128, 128], BF16)
        C.identb = Buf("ident")
        S_.dma("pool", lambda e: e.dma_start(out=C.ident[:], in_=I["ident"]), writes=[C.identb])
        C.epsc = _sb(nc, stack, "epsc", [128, 2], F32)
        S_.op("dve", lambda e: e.memset(C.epsc[:], EPS), writes=[C.identb])
        stage_norm_T(nc, S_, C, I["xb"], I["nw1"], C.hT1, ntiles * 512)
        stage_mixer(nc, S_, C, I, ntiles, True if debug else None)
        if debug:
            fin = _sb(nc, stack, "fin", [128, 8], F32)
            S_.barrier()
            S_.emit()
    return nc


def _prep_common():
    ident = np.eye(128, dtype=np.float32)
    s = np.arange(128)
    maskUT = ((s[:, None] // 64 == s[None, :] // 64) & (s[:, None] <= s[None, :])).astype(np.float32)
    rst = np.ones((128, 512), np.float32)
    rst[:, ::64] = 0.0
    return {"ident": ident, "maskUT": maskUT, "rst": rst}


def _wtile(wcols):
    return np.ascontiguousarray(wcols.reshape(32, 128, wcols.shape[1]).transpose(1, 0, 2))


def prep_p1(c, inputs):
    b, j = c // 4, c % 4
    w_in = inputs["w_in"][0]
    MW = 2048
    off = {"qm": 0, "km": MW, "vm": 2 * MW, "om": 3 * MW, "ipre": 4 * MW, "fpre": 4 * MW + 8,
           "qh": 4 * MW + 16, "fh": 5 * MW + 16, "ih": 6 * MW + 16, "gh": 7 * MW + 16,
           "gm": 8 * MW + 16, "ghh": 8 * MW + 16 + D}
    tiles = []
    for u in range(2):
        hd = 2 * j + u
        for nm in ("qm", "km", "vm", "om"):
            tiles.append(_wtile(w_in[:, off[nm] + hd * 256: off[nm] + (hd + 1) * 256]))
    for u in range(2):
        h0 = 4 * j + 2 * u
        for nm in ("qh", "fh", "ih", "gh"):
            tiles.append(_wtile(w_in[:, off[nm] + h0 * 128: off[nm] + (h0 + 2) * 128]))
    w1 = np.stack(tiles, 0)
    gcols = np.stack([w_in[:, off["ipre"] + 2 * j], w_in[:, off["ipre"] + 2 * j + 1],
                      w_in[:, off["fpre"] + 2 * j], w_in[:, off["fpre"] + 2 * j + 1]], 1)
    wg = _wtile(gcols)
    conv_w = inputs["conv_qk_w"][0]
    conv_b = inputs["conv_qk_b"][0]
    cw = np.zeros((128, 8, 4), np.float32)
    cb = np.zeros((128, 8), np.float32)
    for part in range(2):
        for u in range(2):
            for cc in range(2):
                ch0 = part * 2048 + (2 * j + u) * 256 + cc * 128
                idx = part * 4 + u * 2 + cc
                cw[:, idx, :] = conv_w[:, ch0:ch0 + 128].T
                cb[:, idx] = conv_b[ch0:ch0 + 128]
    lbr = inputs["hgrn_lb"]
    lb = np.zeros((128, 4, 2), np.float32)
    for hh in range(4):
        ch0 = (4 * j + hh) * 128
        lb[:, hh, 0] = lbr[0, ch0:ch0 + 128]
        lb[:, hh, 1] = lbr[1, ch0:ch0 + 128]
    gbv = inputs["gate_if_b"][0]
    gb = np.zeros((1, 8), np.float32)
    gb[0, 0:2] = gbv[2 * j:2 * j + 2]
    gb[0, 2:4] = gbv[8 + 2 * j:8 + 2 * j + 2]
    mnw = np.broadcast_to(inputs["mlstm_norm_w"][0][j * 512:(j + 1) * 512], (128, 512))
    hnw = np.broadcast_to(inputs["hgrn_norm_w"][0][j * 512:(j + 1) * 512], (128, 512))
    d = {"xb": inputs["x"][b], "nw1": np.broadcast_to(inputs["norm_mix_w"][0], (128, D)), "w1": w1, "wg": wg,
         "cw": cw, "cb": cb, "lb": lb, "gb": gb, "mnw": mnw, "hnw": hnw}
    return {k: np.ascontiguousarray(v, dtype=np.float32) for k, v in d.items()}


def x_mm(S_, out, lhsT, rhs, start, stop, reads, writes):
    S_.op("pe", lambda e: e.matmul(out, lhsT=lhsT, rhs=rhs, start=start, stop=stop), reads, writes)


def x_tr(S_, C, out, in_, reads, writes):
    S_.op("pe", lambda e: e.transpose(out=out, in_=in_, identity=C.ident[:]), list(reads) + [C.identb], writes)


def x_act(S_, out, in_, func, reads, writes, **kw):
    S_.op("act", lambda e: e.activation(out=out, in_=in_, func=func, **kw), reads, writes)


def x_cp(S_, eng, out, in_, reads, writes):
    if eng == "act":
        S_.op("act", lambda e: e.copy(out=out, in_=in_), reads, writes)
    else:
        S_.op(eng, lambda e: e.tensor_copy(out=out, in_=in_), reads, writes)


def x_tt(S_, eng, out, in0, in1, op, reads, writes):
    S_.op(eng, lambda e: e.tensor_tensor(out=out, in0=in0, in1=in1, op=op), reads, writes)


def x_ts(S_, eng, out, in0, s1, s2, op0, op1, reads, writes):
    if op1 is None:
        S_.op(eng, lambda e: e.tensor_scalar(out=out, in0=in0, scalar1=s1, scalar2=None, op0=op0), reads, writes)
    else:
        S_.op(eng, lambda e: e.tensor_scalar(out=out, in0=in0, scalar1=s1, scalar2=s2, op0=op0, op1=op1), reads, writes)


def x_stt(S_, eng, out, in0, scalar, in1, op0, op1, reads, writes):
    S_.op(eng, lambda e: e.scalar_tensor_tensor(out=out, in0=in0, scalar=scalar, in1=in1, op0=op0, op1=op1), reads, writes)


def x_red(S_, out, in_, op, reads, writes):
    S_.op("dve", lambda e: e.tensor_reduce(out=out, in_=in_, axis=AX.X, op=op), reads, writes)


def x_dma(S_, q, out, in_, reads, writes):
    return S_.dma(q, lambda e: e.dma_start(out=out, in_=in_), reads, writes)


def x_rstd(S_, C, out, in_, n, reads, writes):
    x_act(S_, out, in_, AF.Ln, list(reads) + [C.identb], writes, bias=C.epsc[:, 0:1], scale=1.0 / n)
    x_act(S_, out, out, AF.Exp, writes, writes, scale=-0.5)


def coll(S_, nc, kind, groups, src, dst, reads, writes):
    sem = S_.new_sem("cc%d" % S_.nsem)
    deps = S_._deps(reads, writes)
    waits = S_._waits("pool", deps)
    ev = (sem, 1, "dma")
    S_.streams["pool"].append((waits, lambda e: e.collective_compute(kind, ALU.bypass, replica_groups=groups, ins=[src], outs=[dst]), sem, 1))
    S_._mark(ev, reads, writes)


def stage_branch(nc, S_, C, I):
    with contextlib.ExitStack() as st:
        getf, getb = C.getf, C.getb
        sb = lambda name, shape, dt: _sb(nc, st, "b_" + name, shape, dt)
        hoT = sb("hoT", [128, 32, 512], BF16); hoTb = Buf()
        hT2 = sb("hT2", [128, 32, 512], BF16); hT2b = Buf()
        yT = sb("yT", [128, 32, 512], BF16); yTb = Buf()
        W = [sb("W%d" % i, [128, 96, 128], BF16) for i in range(2)]; Wb = [Buf(), Buf()]
        gt = [sb("gt%d" % i, [128, 1024], BF16) for i in range(4)]; gtb = [Buf() for _ in range(4)]
        gidx = sb("gidx", [128, 8, 4], I32); gidxb = Buf()
        sg = [sb("sg%d" % i, [128, 512], F32) for i in range(4)]; sgb = [Buf() for _ in range(4)]
        S_.barrier()
        x_dma(S_, "sp", gidx[:], I["gidx"], [], [gidxb])
        wc = 0
        for tt in range(2):
            x_dma(S_, "sp", hT2[:, :, :].rearrange("p a b -> p (a b)"), C.hT2[tt, :, :], [C.dramb], [hT2b])
            for ts in range(4):
                sub = tt * 4 + ts
                for jp in range(4):
                    g = gt[jp]
                    S_.dma("pool", lambda e, g=g, sub=sub, jp=jp: e.indirect_dma_start(
                        out=g[:, :], out_offset=None, in_=C.cc1_out[:, :],
                        in_offset=bass.IndirectOffsetOnAxis(ap=gidx[:, sub, jp:jp + 1], axis=0),
                        bounds_check=4 * S - 1, oob_is_err=False), reads=[gidxb, C.dramb], writes=[gtb[jp]])
                    pt, pb = getb()
                    for cbk in range(8):
                        x_tr(S_, C, pt[:, cbk * 128:(cbk + 1) * 128], g[:, cbk * 128:(cbk + 1) * 128], [gtb[jp]], [pb])
                    x_cp(S_, "act", hoT[:, jp * 4:jp * 4 + 4, ts * 128:(ts + 1) * 128],
                         pt[:, 0:512].rearrange("p (a b) -> p a b", b=128), [pb], [hoTb])
                    x_cp(S_, "dve", hoT[:, 16 + jp * 4:16 + jp * 4 + 4, ts * 128:(ts + 1) * 128],
                         pt[:, 512:1024].rearrange("p (a b) -> p a b", b=128), [pb], [hoTb])
            for dcn in range(32):
                k = wc % 2
                wc += 1
                x_dma(S_, "pool", W[k][:], I["w2"][dcn], [], [Wb[k]])
                ps = []
                for (i0, n, src, srcb) in ((0, 16, hoT, hoTb), (16, 16, hoT, hoTb), (32, 32, hT2, hT2b), (64, 32, hT2, hT2b)):
                    pp, ppb = getf()
                    for q in range(n):
                        kk = (i0 + q) if i0 < 32 else q
                        x_mm(S_, pp[:, :], W[k][:, i0 + q, :], src[:, kk, :], q == 0, q == n - 1, [Wb[k], srcb], [ppb])
                    ps.append((pp, ppb))
                (pym, pymb), (pyh, pyhb), (pgm, pgmb), (pgh, pghb) = ps
                x_act(S_, sg[0][:], pgm[:, :], AF.Sigmoid, [pgmb], [sgb[0]])
                x_act(S_, sg[1][:], pgh[:, :], AF.Sigmoid, [pghb], [sgb[1]])
                x_tt(S_, "dve", sg[2][:], pym[:, :], sg[0][:], ALU.mult, [pymb, sgb[0]], [sgb[2]])
                x_tt(S_, "dve", sg[3][:], pyh[:, :], sg[1][:], ALU.mult, [pyhb, sgb[1]], [sgb[3]])
                x_tt(S_, "pool", yT[:, dcn, :], sg[2][:], sg[3][:], ALU.add, [sgb[2], sgb[3]], [yTb])
            x_dma(S_, "sp", C.yT[tt, :, :], yT[:, :, :].rearrange("p a b -> p (a b)"), [yTb], [C.dramb])
        S_.barrier()
        S_.emit()


def stage_outproj(nc, S_, C, I):
    with contextlib.ExitStack() as st:
        getf, getb = C.getf, C.getb
        sb = lambda name, shape, dt: _sb(nc, st, "o_" + name, shape, dt)
        yT = sb("yT", [128, 32, 512], BF16); yTb = Buf()
        x2 = sb("x2", [128, 4, D], F32); x2b = [Buf() for _ in range(4)]
        W = [sb("W%d" % i, [128, 32, 256], BF16) for i in range(2)]; Wb = [Buf(), Buf()]
        xn = sb("xn", [128, D], BF16); xnb = Buf()
        h2T = sb("h2T", [128, 32, 128], BF16); h2Tb = Buf()
        nw = sb("nw", [128, D], F32); nwb = Buf()
        wr = sb("wr", [128, 32, 72], BF16); wrb = Buf()
        rb = sb("rb", [128, 72], F32); io8 = sb("io8", [128, 8], F32)
        lg = sb("lg", [128, 72], F32); lgb = Buf()
        t64 = sb("t64", [128, 64], F32); t8 = sb("t8", [128, 6, 8], F32); sm = sb("sm", [128, 16], F32)
        info = sb("info", [128, 16], F32); infob = Buf()
        cb_ = Buf()
        S_.barrier()
        x_dma(S_, "sp", nw[:], I["nw2"], [], [nwb])
        x_dma(S_, "pool", wr[:], I["wr"], [], [wrb])
        x_dma(S_, "sp", rb[:], I["rb"], [], [cb_])
        x_dma(S_, "sp", io8[:], I["io8"], [], [cb_])
        S_.op("dve", lambda e: e.memset(info[:], 0.0), writes=[infob])
        wc = 0
        s_ = lambda i: sm[:, i:i + 1]
        for tt in range(2):
            x_dma(S_, "sp", yT[:, :, :].rearrange("p a b -> p (a b)"), C.yT[tt, :, :], [C.dramb], [yTb])
            for ts in range(4):
                r0 = (tt * 4 + ts) * 128
                x_dma(S_, "sp", x2[:, ts, :], I["xt"][r0:r0 + 128, :], [], [x2b[ts]])
            for ct in range(16):
                k = wc % 2
                wc += 1
                x_dma(S_, "pool", W[k][:], I["w3"][ct], [], [Wb[k]])
                for ts in range(4):
                    pp, ppb = getf()
                    for kc in range(32):
                        x_mm(S_, pp[:, 0:256], yT[:, kc, ts * 128:(ts + 1) * 128], W[k][:, kc, :], kc == 0, kc == 31, [yTb, Wb[k]], [ppb])
                    x_tt(S_, "dve", x2[:, ts, ct * 256:(ct + 1) * 256], x2[:, ts, ct * 256:(ct + 1) * 256], pp[:, 0:256], ALU.add,
                         [ppb, x2b[ts]], [x2b[ts]])
            for ts in range(4):
                r0 = (tt * 4 + ts) * 128
                x_dma(S_, "sp", C.cc2_in[r0:r0 + 128, 0:D], x2[:, ts, :], [x2b[ts]], [C.dramb])
                x_act(S_, xn[:], x2[:, ts, :], AF.Square, [x2b[ts]], [xnb, cb_], accum_out=s_(0))
                x_rstd(S_, C, s_(1), s_(0), D, [cb_], [cb_])
                x_stt(S_, "dve", xn[:], x2[:, ts, :], s_(1), nw[:], ALU.mult, ALU.mult, [x2b[ts], cb_, nwb], [xnb])
                for g in range(4):
                    pt, pb = getb()
                    for q in range(8):
                        kc = g * 8 + q
                        x_tr(S_, C, pt[:, q * 128:(q + 1) * 128], xn[:, kc * 128:(kc + 1) * 128], [xnb], [pb])
                    x_cp(S_, "act" if g % 2 == 0 else "dve", h2T[:, g * 8:(g + 1) * 8, :], pt[:, :].rearrange("p (a b) -> p a b", b=128), [pb], [h2Tb])
                pr, prb = getf()
                for kc in range(32):
                    x_mm(S_, pr[:, 0:72], h2T[:, kc, :], wr[:, kc, :], kc == 0, kc == 31, [h2Tb, wrb], [prb])
                R_, W_ = [lgb, cb_], [cb_]
                x_tt(S_, "dve", lg[:], pr[:, 0:72], rb[:], ALU.add, [prb, cb_], [lgb])
                x_red(S_, s_(2), lg[:, 0:8], ALU.max, R_, W_)
                x_ts(S_, "dve", t8[:, 0, :], lg[:, 0:8], s_(2), None, ALU.is_equal, None, R_, W_)
                x_tt(S_, "dve", t8[:, 1, :], t8[:, 0, :], io8[:], ALU.mult, R_, W_)
                x_red(S_, info[:, 0:1], t8[:, 1, :], ALU.add, R_, [infob, cb_])
                x_ts(S_, "dve", s_(3), s_(2), -1.0, None, ALU.mult, None, R_, W_)
                x_act(S_, t8[:, 1, :], lg[:, 0:8], AF.Exp, R_, W_, bias=s_(3), scale=1.0, accum_out=s_(4))
                S_.op("dve", lambda e: e.reciprocal(out=sm[:, 5:6], in_=sm[:, 4:5]), R_, W_)
                el3 = lg[:, 8:72].rearrange("p (g e) -> p g e", e=8)
                oh3 = t8[:, 0, :].rearrange("p (g o) -> p g o", o=1).to_broadcast([128, 8, 8])
                x_tt(S_, "dve", t64[:].rearrange("p (g e) -> p g e", e=8), el3, oh3, ALU.mult, R_, W_)
                x_red(S_, t8[:, 2, :], t64[:].rearrange("p (g e) -> p e g", e=8), ALU.add, R_, W_)
                x_red(S_, s_(6), t8[:, 2, :], ALU.max, R_, W_)
                x_ts(S_, "dve", t8[:, 3, :], t8[:, 2, :], s_(6), None, ALU.is_equal, None, R_, W_)
                x_stt(S_, "dve", t8[:, 4, :], t8[:, 3, :], -1e30, t8[:, 2, :], ALU.mult, ALU.add, R_, W_)
                x_red(S_, s_(7), t8[:, 4, :], ALU.max, R_, W_)
                x_ts(S_, "dve", t8[:, 5, :], t8[:, 4, :], s_(7), None, ALU.is_equal, None, R_, W_)
                x_tt(S_, "dve", s_(8), s_(7), s_(6), ALU.subtract, R_, W_)
                x_act(S_, s_(9), s_(8), AF.Exp, R_, W_)
                x_ts(S_, "dve", s_(10), s_(9), 1.0, None, ALU.add, None, R_, W_)
                S_.op("dve", lambda e: e.reciprocal(out=sm[:, 10:11], in_=sm[:, 10:11]), R_, W_)
                x_tt(S_, "dve", s_(11), s_(10), s_(5), ALU.mult, R_, W_)
                x_tt(S_, "dve", s_(12), s_(11), s_(9), ALU.mult, R_, W_)
                x_ts(S_, "dve", t8[:, 1, :], t8[:, 3, :], s_(11), None, ALU.mult, None, R_, W_)
                x_stt(S_, "dve", info[:, 1:9], t8[:, 5, :], s_(12), t8[:, 1, :], ALU.mult, ALU.add, R_, [infob, cb_])
                x_dma(S_, "sp", C.cc2_in[r0:r0 + 128, D:D + 16], info[:], [infob], [C.dramb, infob])
        S_.barrier()
        S_.emit()


def stage_moe(nc, S_, C, I):
    NB = CAP // 128
    with contextlib.ExitStack() as st:
        getf, getb = C.getf, C.getb
        sb = lambda name, shape, dt: _sb(nc, st, "e_" + name, shape, dt)
        acc = sb("acc", [128, 4, XW], F32); accb = [Buf() for _ in range(4)]
        XgT = sb("XgT", [128, 32, 512], BF16); XgTb = Buf()
        xn = sb("xn", [128, D], BF16); xnb = Buf()
        aT = sb("aT", [128, 6, 512], BF16); aTb = Buf()
        Wt = [sb("Wt%d" % i, [128, 64, 128], BF16) for i in range(2)]; Wtb = [Buf(), Buf()]
        Wd = [sb("Wd%d" % i, [128, 6, 512], BF16) for i in range(2)]; Wdb = [Buf(), Buf()]
        nw = sb("nw", [128, D], F32); nwb = Buf()
        sg = [sb("sg%d" % i, [128, 512], F32) for i in range(2)]; sgb = [Buf(), Buf()]
        inf = sb("inf", [128, 64, 16], F32); cb_ = Buf()
        wk = sb("wk", [128, 6, 64], F32)
        idx = sb("idx", [128, NB], I32); idxb = Buf()
        gid = sb("gid", [128, 2], F32); Ls = sb("Ls", [128, 128], F32)
        sm = sb("sm", [128, 8], F32)
        s_ = lambda i: sm[:, i:i + 1]
        S_.barrier()
        iotar = sb("iotar", [128, CAP], F32)
        tokhl = sb("tokhl", [128, 64, 4], BF16)
        oh = [sb("oh%d" % i, [128, CAP], BF16) for i in range(2)]; ohb = [Buf(), Buf()]
        res = sb("res", [128, NB, 4], F32)
        x_dma(S_, "sp", inf[:], C.cc2_out[:, D:D + 16].rearrange("(p i) c -> p i c", i=64), [C.dramb], [cb_])
        x_dma(S_, "sp", gid[:, 0:1], I["gid"], [], [cb_])
        x_dma(S_, "sp", Ls[:], I["Ls"], [], [cb_])
        x_dma(S_, "sp", iotar[:], I["iotar"], [], [cb_])
        x_dma(S_, "pool", tokhl[:], I["tokhl"], [], [cb_])
        S_.op("dve", lambda e: e.memset(wk[:, 0, :], 1.0), writes=[cb_])
        S_.op("dve", lambda e: e.memset(acc[:], 0.0), writes=accb)
        R_, W_ = [cb_], [cb_]
        x_ts(S_, "dve", wk[:, 1, :], inf[:, :, 0], gid[:, 0:1], None, ALU.is_equal, None, R_, W_)
        S_.op("dve", lambda e: e.tensor_tensor_scan(out=wk[:, 2, :], data0=wk[:, 0, :], data1=wk[:, 1, :], initial=0.0,
                                                    op0=ALU.mult, op1=ALU.add), R_, W_)
        pp, ppb = getf()
        x_mm(S_, pp[:, 0:1], Ls[:], wk[:, 2, 63:64], True, True, R_, [ppb])
        x_cp(S_, "act", s_(0), pp[:, 0:1], [ppb], W_)
        x_ts(S_, "dve", wk[:, 3, :], wk[:, 2, :], s_(0), None, ALU.add, None, R_, W_)
        x_tt(S_, "dve", wk[:, 3, :], wk[:, 3, :], wk[:, 1, :], ALU.mult, R_, W_)
        x_ts(S_, "dve", wk[:, 3, :], wk[:, 3, :], -1.0, None, ALU.add, None, R_, W_)
        identF = sb("identF", [128, 128], F32)
        rowsT = sb("rowsT", [4, CAP], F32)
        x_dma(S_, "sp", identF[:], I["ident"], [], [cb_])
        pcs = [getf() for _ in range(CAP // 512)]
        for i in range(64):
            k = i % 2
            x_ts(S_, "dve", oh[k][:], iotar[:], wk[:, 3, i:i + 1], None, ALU.is_equal, None, R_, [ohb[k]])
            for c3, (pc, pcb) in enumerate(pcs):
                x_mm(S_, pc[0:4, :], tokhl[:, i, :], oh[k][:, c3 * 512:(c3 + 1) * 512], i == 0, i == 63, [ohb[k], cb_], [pcb])
        for c3, (pc, pcb) in enumerate(pcs):
            x_cp(S_, "act", rowsT[:, c3 * 512:(c3 + 1) * 512], pc[0:4, :], [pcb], W_)
        pt2, pt2b = getf()
        for rb_ in range(NB):
            S_.op("pe", lambda e, rb_=rb_: e.transpose(out=pt2[:, rb_ * 4:rb_ * 4 + 4], in_=rowsT[0:4, rb_ * 128:(rb_ + 1) * 128],
                                                       identity=identF[0:4, 0:4]), R_, [pt2b])
        x_cp(S_, "act", res[:], pt2[:, 0:NB * 4].rearrange("p (a b) -> p a b", b=4), [pt2b], W_)
        x_stt(S_, "dve", res[:, :, 3], res[:, :, 0], 64.0, res[:, :, 1], ALU.mult, ALU.add, R_, W_)
        x_ts(S_, "dve", res[:, :, 3], res[:, :, 3], -100000.0, None, ALU.add, None, R_, W_)
        x_tt(S_, "dve", res[:, :, 3], res[:, :, 3], res[:, :, 2], ALU.mult, R_, W_)
        x_ts(S_, "dve", res[:, :, 3], res[:, :, 3], 100000.0, None, ALU.add, None, R_, W_)
        x_cp(S_, "dve", idx[:], res[:, :, 3], R_, [idxb])
        x_dma(S_, "sp", C.oidx, idx[:], [idxb], [C.dramb])
        wtc = 0
        wdc = 0
        for tt in range(CAP // 512):
            x_dma(S_, "sp", nw[:], I["nw2"], [], [nwb])
            for ts in range(4):
                blk = tt * 4 + ts
                S_.dma("pool", lambda e, ts=ts, blk=blk: e.indirect_dma_start(
                    out=acc[:, ts, :], out_offset=None, in_=C.cc2_out[:, :],
                    in_offset=bass.IndirectOffsetOnAxis(ap=idx[:, blk:blk + 1], axis=0),
                    bounds_check=NCORE * 1024 - 1, oob_is_err=False), reads=[idxb, C.dramb], writes=[accb[ts]])
                x_act(S_, xn[:], acc[:, ts, 0:D], AF.Square, [accb[ts]], [xnb, cb_], accum_out=s_(1))
                x_rstd(S_, C, s_(2), s_(1), D, [cb_], [cb_])
                x_stt(S_, "dve", xn[:], acc[:, ts, 0:D], s_(2), nw[:], ALU.mult, ALU.mult, [accb[ts], cb_, nwb], [xnb])
                for g in range(4):
                    pt, pb = getb()
                    for q in range(8):
                        kc = g * 8 + q
                        x_tr(S_, C, pt[:, q * 128:(q + 1) * 128], xn[:, kc * 128:(kc + 1) * 128], [xnb], [pb])
                    x_cp(S_, "act" if g % 2 == 0 else "dve", XgT[:, g * 8:(g + 1) * 8, ts * 128:(ts + 1) * 128],
                         pt[:, :].rearrange("p (a b) -> p a b", b=128), [pb], [XgTb])
            for ex in range(8):
                for fc in range(6):
                    k = wtc % 2
                    wtc += 1
                    x_dma(S_, "pool", Wt[k][:], I["wgu"][ex * 6 + fc], [], [Wtb[k]])
                    pg, pgb = getf()
                    for kc in range(32):
                        x_mm(S_, pg[:, :], Wt[k][:, kc, :], XgT[:, kc, :], kc == 0, kc == 31, [Wtb[k], XgTb], [pgb])
                    pu, pub = getf()
                    for kc in range(32):
                        x_mm(S_, pu[:, :], Wt[k][:, 32 + kc, :], XgT[:, kc, :], kc == 0, kc == 31, [Wtb[k], XgTb], [pub])
                    x_act(S_, sg[0][:], pg[:, :], AF.Sigmoid, [pgb], [sgb[0]])
                    x_tt(S_, "dve", sg[1][:], pg[:, :], sg[0][:], ALU.mult, [pgb, sgb[0]], [sgb[1]])
                    x_tt(S_, "dve", aT[:, fc, :], pu[:, :], sg[1][:], ALU.mult, [pub, sgb[1]], [aTb])
                for ct in range(8):
                    k = wdc % 2
                    wdc += 1
                    x_dma(S_, "pool", Wd[k][:], I["wdn"][ex * 8 + ct], [], [Wdb[k]])
                    for ts in range(4):
                        po, pob = getf()
                        for fc in range(6):
                            x_mm(S_, po[:, :], aT[:, fc, ts * 128:(ts + 1) * 128], Wd[k][:, fc, :], fc == 0, fc == 5, [aTb, Wdb[k]], [pob])
                        x_stt(S_, "dve", acc[:, ts, ct * 512:(ct + 1) * 512], po[:, :], acc[:, ts, D + 1 + ex:D + 2 + ex],
                              acc[:, ts, ct * 512:(ct + 1) * 512], ALU.mult, ALU.add, [pob, accb[ts]], [accb[ts]])
            x_dma(S_, "sp", nw[:], I["nwf"], [], [nwb])
            for ts in range(4):
                blk = tt * 4 + ts
                x_act(S_, xn[:], acc[:, ts, 0:D], AF.Square, [accb[ts]], [xnb, cb_], accum_out=s_(1))
                x_rstd(S_, C, s_(2), s_(1), D, [cb_], [cb_])
                x_stt(S_, "dve", acc[:, ts, 0:D], acc[:, ts, 0:D], s_(2), nw[:], ALU.mult, ALU.mult, [accb[ts], cb_, nwb], [accb[ts]])
                x_dma(S_, "sp", C.orow[blk * 128:(blk + 1) * 128, :], acc[:, ts, 0:D], [accb[ts]], [C.dramb])
        S_.barrier()
        S_.emit()


def build_full(upto=3, debug=False, mode="fused"):
    nc = bass.Bass("TRN2", target_bir_lowering=False)
    I = {}

    def inp(name, shape, dt=F32):
        I[name] = nc.dram_tensor(name, list(shape), dt, kind="ExternalInput").ap()

    fused = mode == "fused"
    do1 = fused or mode == "L1"
    do2 = (fused and upto >= 2) or mode == "L2"
    do3 = (fused and upto >= 3) or mode == "L3"
    inp("ident", [128, 128])
    if do1:
        inp("maskUT", [128, 128]); inp("rst", [128, 512])
        inp("xb", [S, D]); inp("w1", [16, 128, 32, 256]); inp("wg", [128, 32, 4])
        inp("cw", [128, 8, 4]); inp("cb", [128, 8]); inp("lb", [128, 4, 2]); inp("gb", [1, 8])
        inp("mnw", [128, 512]); inp("hnw", [128, 512])
    if do1 or do2:
        inp("nw1", [128, D])
    if do2:
        inp("xt", [1024, D]); inp("gidx", [128, 8, 4], I32); inp("w2", [32, 128, 96, 128]); inp("w3", [16, 128, 32, 256])
        inp("wr", [128, 32, 72]); inp("rb", [128, 72]); inp("io8", [128, 8])
    if do2 or do3:
        inp("nw2", [128, D])
    if do3:
        inp("gid", [128, 1]); inp("Ls", [128, 128]); inp("iotar", [128, CAP]); inp("tokhl", [128, 64, 4])
        inp("wgu", [48, 128, 64, 128]); inp("wdn", [64, 128, 6, 512]); inp("nwf", [128, D])
    C = Ctx()
    dk = lambda lvl: "ExternalOutput" if (debug and upto == lvl) else "Internal"
    C.dramb = Buf("dram")
    if do1:
        C.hT1 = nc.dram_tensor("hT1", [8, 128, 32 * 512], BF16).ap()
        C.cc1_in = nc.dram_tensor("cc1_in", [S, 1024], BF16, kind="ExternalOutput" if mode == "L1" else dk(1)).ap()
    if do2:
        if fused:
            C.cc1_out = nc.dram_tensor("cc1_out", [4 * S, 1024], BF16).ap()
        else:
            C.cc1_out = nc.dram_tensor("cc1_out", [4 * S, 1024], BF16, kind="ExternalInput").ap()
        C.hT2 = nc.dram_tensor("hT2", [2, 128, 32 * 512], BF16).ap()
        C.yT = nc.dram_tensor("yT", [2, 128, 32 * 512], BF16).ap()
        C.cc2_in = nc.dram_tensor("cc2_in", [1024, XW], F32, kind="ExternalOutput" if mode == "L2" else dk(2)).ap()
    if do3:
        if fused:
            C.cc2_out = nc.dram_tensor("cc2_out", [NCORE * 1024, XW], F32).ap()
        else:
            C.cc2_out = nc.dram_tensor("cc2_out", [NCORE * 1024, XW], F32, kind="ExternalInput").ap()
        C.oidx = nc.dram_tensor("oidx", [128, CAP // 128], I32, kind="ExternalOutput").ap()
        C.orow = nc.dram_tensor("orow", [CAP, D], F32, kind="ExternalOutput").ap()
    with contextlib.ExitStack() as stack:
        S_ = Sched(nc, stack)
        C.getf, C.getb = _psum_pool(nc, stack, S_)
        C.ident = _sb(nc, stack, "ident", [128, 128], BF16)
        C.identb = Buf("ident")
        S_.dma("pool", lambda e: e.dma_start(out=C.ident[:], in_=I["ident"]), writes=[C.identb])
        C.epsc = _sb(nc, stack, "epsc", [128, 2], F32)
        S_.op("dve", lambda e: e.memset(C.epsc[:], EPS), writes=[C.identb])
        if do1:
            stage_norm_T(nc, S_, C, I["xb"], I["nw1"], C.hT1, S)
            stage_mixer(nc, S_, C, I)
        if do2:
            if fused:
                S_.barrier()
                coll(S_, nc, "AllGather", [[0, 1, 2, 3], [4, 5, 6, 7]], C.cc1_in, C.cc1_out, [C.dramb], [C.dramb])
            stage_norm_T(nc, S_, C, I["xt"], I["nw1"], C.hT2, 1024)
            import os as _os
            lim = int(_os.environ.get("K_LIM", "9"))
            if lim >= 1:
                stage_branch(nc, S_, C, I)
            if lim >= 2:
                stage_outproj(nc, S_, C, I)
        if do3:
            if fused:
                S_.barrier()
                coll(S_, nc, "AllGather", [list(range(8))], C.cc2_in, C.cc2_out, [C.dramb], [C.dramb])
            stage_moe(nc, S_, C, I)
        S_.barrier()
        S_.emit()
    return nc


def prep_p2(c, inputs, shared):
    seg = c % 4
    gidx = np.zeros((128, 8, 4), np.int32)
    for sub in range(8):
        for jp in range(4):
            gidx[:, sub, jp] = jp * S + seg * 1024 + sub * 128 + np.arange(128)
    xt = inputs["x"].reshape(-1, D)[c * 1024:(c + 1) * 1024]
    d = {"xt": np.ascontiguousarray(xt, dtype=np.float32), "gidx": gidx}
    d.update(shared)
    return d


def prep_p2_shared(inputs):
    w_in = inputs["w_in"][0]
    g0 = 8 * 2048 + 16
    wbm = inputs["w_branch_m"][0]
    wbh = inputs["w_branch_h"][0]
    w2 = np.empty((32, 128, 96, 128), np.float32)
    for dcn in range(32):
        cs = slice(dcn * 128, (dcn + 1) * 128)
        w2[dcn, :, 0:16, :] = wbm[:, cs].reshape(16, 128, 128).transpose(1, 0, 2)
        w2[dcn, :, 16:32, :] = wbh[:, cs].reshape(16, 128, 128).transpose(1, 0, 2)
        w2[dcn, :, 32:64, :] = w_in[:, g0 + dcn * 128:g0 + (dcn + 1) * 128].reshape(32, 128, 128).transpose(1, 0, 2)
        w2[dcn, :, 64:96, :] = w_in[:, g0 + D + dcn * 128:g0 + D + (dcn + 1) * 128].reshape(32, 128, 128).transpose(1, 0, 2)
    wo = inputs["w_out"][0]
    w3 = np.ascontiguousarray(wo.reshape(32, 128, 16, 256).transpose(2, 1, 0, 3))
    wrc = np.concatenate([inputs["router_group_w"][0], inputs["router_expert_w"][0]], 1)
    wr = _wtile(wrc)
    rbv = np.concatenate([inputs["router_group_b"][0], inputs["router_expert_b"][0]])
    return {"w2": w2, "w3": w3, "nw2": np.ascontiguousarray(np.broadcast_to(inputs["norm_ffn_w"][0], (128, D)), dtype=np.float32),
            "wr": np.ascontiguousarray(wr, dtype=np.float32), "rb": np.ascontiguousarray(np.broadcast_to(rbv, (128, 72)), dtype=np.float32),
            "io8": np.ascontiguousarray(np.broadcast_to(np.arange(8, dtype=np.float32), (128, 8)))}


def _tokhl():
    t = np.zeros((128, 64, 4), np.float32)
    t[:, :, 0] = np.arange(128)[:, None]
    t[:, :, 1] = np.arange(64)[None, :]
    t[:, :, 2] = 1.0
    return t


def prep_p3(c, inputs):
    wg_, wu_, wd_ = inputs["w_gate"][0], inputs["w_up"][0], inputs["w_down"][0]
    wgu = np.empty((48, 128, 64, 128), np.float32)
    wdn = np.empty((64, 128, 6, 512), np.float32)
    for ex in range(8):
        e = c * 8 + ex
        a = np.asarray(wg_[e]).reshape(32, 128, 6, 128)
        b = np.asarray(wu_[e]).reshape(32, 128, 6, 128)
        wgu[ex * 6:(ex + 1) * 6, :, 0:32, :] = a.transpose(2, 1, 0, 3)
        wgu[ex * 6:(ex + 1) * 6, :, 32:64, :] = b.transpose(2, 1, 0, 3)
        dd = np.asarray(wd_[e]).reshape(6, 128, 8, 512)
        wdn[ex * 8:(ex + 1) * 8] = dd.transpose(2, 1, 0, 3)
    s = np.arange(128)
    return {"gid": np.full((128, 1), float(c), np.float32), "Ls": (s[:, None] < s[None, :]).astype(np.float32),
            "iotar": np.ascontiguousarray(np.broadcast_to(np.arange(CAP, dtype=np.float32), (128, CAP))), "tokhl": _tokhl(), "wgu": wgu, "wdn": wdn,
            "nwf": np.ascontiguousarray(np.broadcast_to(inputs["norm_final_w"], (128, D)), dtype=np.float32)}


_NC_CACHE = {}


def _get_nc(mode):
    if mode not in _NC_CACHE:
        _NC_CACHE[mode] = build_full(3, mode=mode)
    return _NC_CACHE[mode]


def _pick(d, nc_inputs):
    return {k: d[k] for k in nc_inputs}


def kernel(**inputs):
    inputs = {k: np.asarray(v) for k, v in inputs.items()}
    com = _prep_common()
    cores = list(range(NCORE))
    names1 = ["ident", "maskUT", "rst", "xb", "w1", "wg", "cw", "cb", "lb", "gb", "mnw", "hnw", "nw1"]
    maps = []
    for c in cores:
        d = prep_p1(c, inputs)
        d.update(com)
        maps.append(_pick(d, names1))
    r1 = run_bass_kernel_spmd(_get_nc("L1"), maps, core_ids=cores).results
    ho = [np.asarray(r["cc1_in"]) for r in r1]
    del maps
    shared = prep_p2_shared(inputs)
    nw1 = np.ascontiguousarray(np.broadcast_to(inputs["norm_mix_w"][0], (128, D)), dtype=np.float32)
    names2 = ["ident", "nw1", "xt", "gidx", "w2", "w3", "wr", "rb", "io8", "nw2", "cc1_out"]
    gath = [np.concatenate(ho[0:4], 0), np.concatenate(ho[4:8], 0)]
    maps = []
    for c in cores:
        d = prep_p2(c, inputs, shared)
        d.update({"ident": com["ident"], "nw1": nw1, "cc1_out": gath[c // 4]})
        maps.append(_pick(d, names2))
    r2 = run_bass_kernel_spmd(_get_nc("L2"), maps, core_ids=cores).results
    x2all = np.concatenate([np.asarray(r["cc2_in"]) for r in r2], 0)
    del maps
    names3 = ["ident", "nw2", "gid", "Ls", "iotar", "tokhl", "wgu", "wdn", "nwf", "cc2_out"]
    maps = []
    for c in cores:
        d = prep_p3(c, inputs)
        d.update({"ident": com["ident"], "nw2": shared["nw2"], "cc2_out": x2all})
        maps.append(_pick(d, names3))
    r3 = run_bass_kernel_spmd(_get_nc("L3"), maps, core_ids=cores).results
    out = np.zeros((NCORE * 1024, D), np.float32)
    for c in cores:
        idx = np.asarray(r3[c]["oidx"]).T.reshape(-1)
        rows = np.asarray(r3[c]["orow"])
        ok = (idx >= 0) & (idx < NCORE * 1024)
        out[idx[ok]] = rows[ok]
    return out.reshape(2, S, D)
```

```python
import contextlib
import numpy as np
import concourse.bass as bass
import concourse.mybir as mybir
from concourse.bass_utils import run_bass_kernel_spmd

F32, BF16, I32 = mybir.dt.float32, mybir.dt.bfloat16, mybir.dt.int32
AF = mybir.ActivationFunctionType
ALU = mybir.AluOpType
AX = mybir.AxisListType

D = 4096
S = 4096
NCORE = 8
EPS = 1e-6
CAP = 1536
XW = D + 16
ENGS = ("pe", "act", "dve", "pool", "sp")
ROT = 30000
NDMA = 6
CH1 = 256
CH2 = 32


class Buf:
    __slots__ = ("name", "w", "r")

    def __init__(self, name=""):
        self.name = name
        self.w = None
        self.r = {}


class Sched:
    def __init__(self, nc, stack):
        self.nc = nc
        self.stack = stack
        self.streams = {e: [] for e in ENGS}
        self.cnt = {e: 0 for e in ENGS}
        self.prog = {e: [] for e in ENGS}
        self.seen = {e: {} for e in ENGS}
        self.dma_sems = {e: [] for e in ENGS}
        self.dma_val = {}
        self.dma_rr = {e: 0 for e in ENGS}
        self.nsem = 0

    def new_sem(self, name):
        self.nsem += 1
        return self.stack.enter_context(self.nc.semaphore(name))

    def _prog_event(self, e):
        idx = self.cnt[e]
        self.cnt[e] += 1
        r = idx // ROT
        while len(self.prog[e]) <= r:
            self.prog[e].append(self.new_sem("p_%s_%d" % (e, len(self.prog[e]))))
        return (self.prog[e][r], idx % ROT + 1, e)

    def _waits(self, e, deps):
        best = {}
        for (sem, val, src) in deps:
            if src == "pe" and e == "pe":
                continue
            if self.seen[e].get(sem, 0) >= val:
                continue
            if sem not in best or best[sem] < val:
                best[sem] = val
        out = []
        for sem, val in best.items():
            self.seen[e][sem] = val
            out.append((sem, val))
        return out

    def _deps(self, reads, writes):
        deps = []
        for b in reads:
            if b.w is not None:
                deps.append(b.w)
        for b in writes:
            if b.w is not None:
                deps.append(b.w)
            deps.extend(b.r.values())
        return deps

    def _mark(self, ev, reads, writes):
        for b in reads:
            old = b.r.get(ev[0])
            if old is None or old[1] < ev[1]:
                b.r[ev[0]] = ev
        for b in writes:
            b.w = ev
            b.r = {}

    def op(self, e, fn, reads=(), writes=()):
        waits = self._waits(e, self._deps(reads, writes))
        ev = self._prog_event(e)
        self.streams[e].append((waits, fn, ev[0], 1))
        self._mark(ev, reads, writes)
        return ev

    def dma(self, q, fn, reads=(), writes=(), inc=16):
        deps = self._deps(reads, writes)
        sems = self.dma_sems[q]
        i = self.dma_rr[q] % NDMA
        self.dma_rr[q] += 1
        if len(sems) <= i:
            s = self.new_sem("d_%s_%d" % (q, i))
            sems.append(s)
            self.dma_val[s] = 0
        sem = sems[i]
        prev = self.dma_val[sem]
        if prev > 0:
            deps.append((sem, prev, "dma"))
        val = prev + inc
        self.dma_val[sem] = val
        waits = self._waits(q, deps)
        ev = (sem, val, "dma")
        self.streams[q].append((waits, fn, sem, inc))
        self._mark(ev, reads, writes)
        return ev

    def barrier(self):
        evs = []
        for e in ENGS:
            c = self.cnt[e]
            if c > 0:
                r = (c - 1) // ROT
                evs.append((self.prog[e][r], (c - 1) % ROT + 1, "x"))
        for s, v in self.dma_val.items():
            if v > 0:
                evs.append((s, v, "dma"))
        if getattr(self, "cc_val", 0) > 0:
            evs.append((self.cc_sem, self.cc_val, "dma"))
        for e in ENGS:
            waits = self._waits(e, evs)
            if waits:
                self.streams[e].append((waits, None, None, 0))

    def emit(self):
        nc = self.nc
        streams = self.streams
        self.streams = {e: [] for e in ENGS}

        def run(eng, lst):
            for (waits, fn, sem, inc) in lst:
                for (s, v) in waits:
                    eng.wait_ge(s, v)
                if fn is not None:
                    fn(eng).then_inc(sem, inc)

        with nc.Block() as block:
            block.tensor(lambda eng: run(eng, streams["pe"]))
            block.scalar(lambda eng: run(eng, streams["act"]))
            block.vector(lambda eng: run(eng, streams["dve"]))
            block.gpsimd(lambda eng: run(eng, streams["pool"]))
            block.sync(lambda eng: run(eng, streams["sp"]))


class Ctx:
    pass


def _psum_pool(nc, stack, S_):
    banks = []
    for i in range(6):
        t = stack.enter_context(nc.psum_tensor("psf%d" % i, [128, 512], F32))
        banks.append((t, Buf("psf%d" % i)))
    bb = []
    for i in range(2):
        t = stack.enter_context(nc.psum_tensor("psb%d" % i, [128, 1024], BF16))
        bb.append((t, Buf("psb%d" % i)))
    st = {"f": 0, "b": 0}

    def getf():
        st["f"] += 1
        return banks[st["f"] % 6]

    def getb():
        st["b"] += 1
        return bb[st["b"] % 2]

    return getf, getb


_SBN = [0]


def _sb(nc, stack, name, shape, dt):
    _SBN[0] += 1
    return stack.enter_context(nc.sbuf_tensor("s%d_%s" % (_SBN[0], name), list(shape), dt))


def stage_norm_T(nc, S_, C, src, nw_dram, dst, ntok):
    with contextlib.ExitStack() as st:
        getf, getb = C.getf, C.getb
        xs = [_sb(nc, st, "n_xs%d" % i, [128, D], F32) for i in range(2)]
        xsb = [Buf() for _ in range(2)]
        xn = [_sb(nc, st, "n_xn%d" % i, [128, D], BF16) for i in range(2)]
        xnb = [Buf() for _ in range(2)]
        nw = _sb(nc, st, "n_nw", [128, D], F32)
        nwb = Buf()
        hT = [_sb(nc, st, "n_hT%d" % i, [128, 32, 512], BF16) for i in range(2)]
        hTb = [Buf() for _ in range(2)]
        ss = _sb(nc, st, "n_ss", [128, 8], F32)
        ssb = [Buf() for _ in range(4)]
        S_.barrier()
        S_.dma("sp", lambda e: e.dma_start(out=nw[:], in_=nw_dram), writes=[nwb])
        for ti in range(ntok // 128):
            tt, ts = ti // 4, ti % 4
            k = ti % 2
            S_.dma("sp", lambda e, k=k, ti=ti: e.dma_start(out=xs[k][:], in_=src[ti * 128:(ti + 1) * 128, :]),
                   writes=[xsb[k]])
            sb_ = ssb[ti % 4]
            c0 = (ti % 4) * 2
            S_.op("act", lambda e, k=k, c0=c0: e.activation(out=xn[k][:], in_=xs[k][:], func=AF.Square,
                                                            accum_out=ss[:, c0:c0 + 1]),
                  reads=[xsb[k]], writes=[xnb[k], sb_])
            S_.op("act", lambda e, c0=c0: e.activation(out=ss[:, c0 + 1:c0 + 2], in_=ss[:, c0:c0 + 1], func=AF.Ln,
                                                       bias=C.epsc[:, 0:1], scale=1.0 / D), reads=[sb_, C.identb], writes=[sb_])
            S_.op("act", lambda e, c0=c0: e.activation(out=ss[:, c0 + 1:c0 + 2], in_=ss[:, c0 + 1:c0 + 2], func=AF.Exp,
                                                       scale=-0.5), reads=[sb_], writes=[sb_])
            S_.op("dve", lambda e, k=k, c0=c0: e.scalar_tensor_tensor(out=xn[k][:], in0=xs[k][:],
                                                                      scalar=ss[:, c0 + 1:c0 + 2], in1=nw[:],
                                                                      op0=ALU.mult, op1=ALU.mult),
                  reads=[xsb[k], sb_, nwb], writes=[xnb[k]])
            hb = (tt % 2)
            for g in range(4):
                pt, pb = getb()
                for q in range(8):
                    kc = g * 8 + q
                    S_.op("pe", lambda e, pt=pt, q=q, kc=kc, k=k: e.transpose(
                        out=pt[:, q * 128:(q + 1) * 128], in_=xn[k][:, kc * 128:(kc + 1) * 128], identity=C.ident[:]),
                        reads=[xnb[k], C.identb], writes=[pb])
                eng = "act" if g % 2 == 0 else "dve"
                if eng == "act":
                    S_.op("act", lambda e, pt=pt, g=g, hb=hb, ts=ts: e.copy(
                        out=hT[hb][:, g * 8:(g + 1) * 8, ts * 128:(ts + 1) * 128],
                        in_=pt[:, :].rearrange("p (a b) -> p a b", b=128)), reads=[pb], writes=[hTb[hb]])
                else:
                    S_.op("dve", lambda e, pt=pt, g=g, hb=hb, ts=ts: e.tensor_copy(
                        out=hT[hb][:, g * 8:(g + 1) * 8, ts * 128:(ts + 1) * 128],
                        in_=pt[:, :].rearrange("p (a b) -> p a b", b=128)), reads=[pb], writes=[hTb[hb]])
            if ts == 3:
                S_.dma("sp", lambda e, hb=hb, tt=tt: e.dma_start(
                    out=dst[tt, :, :], in_=hT[hb][:, :, :].rearrange("p a b -> p (a b)")),
                    reads=[hTb[hb]], writes=[C.dramb])
        S_.barrier()
        S_.emit()


def stage_mixer(nc, S_, C, I, ntiles=8, dbg=None):
    with contextlib.ExitStack() as st:
        getf, getb = C.getf, C.getb
        sb = lambda name, shape, dt: _sb(nc, st, "m_" + name, shape, dt)
        hT = sb("hT", [128, 32, 512], BF16); hTb = Buf()
        W = [sb("W%d" % i, [128, 32, 256], BF16) for i in range(3)]; Wb = [Buf(), Buf(), Buf()]
        wg = sb("wg", [128, 32, 4], BF16); wgb = Buf()
        FA = [sb("FA%d" % i, [128, 2, 512], BF16) for i in range(2)]; FAb = [Buf(), Buf()]
        FB = [sb("FB%d" % i, [128, 2, 512], BF16) for i in range(2)]; FBb = [Buf(), Buf()]
        FBf = [sb("FBf%d" % i, [128, 2, 512], F32) for i in range(2)]
        TA = [sb("TA%d" % i, [128, 4, 258], BF16) for i in range(2)]; TAb = [Buf(), Buf()]
        TB = [sb("TB%d" % i, [128, 4, 256], BF16) for i in range(2)]; TBb = [Buf(), Buf()]
        hist = sb("hist", [128, 8, 4], F32); histb = [Buf() for _ in range(8)]
        pre = [sb("pre%d" % i, [128, 516], F32) for i in range(2)]; preb = [Buf(), Buf()]
        acc = [sb("acc%d" % i, [128, 512], F32) for i in range(2)]; accb = [Buf(), Buf()]
        sg = [sb("sg%d" % i, [128, 512], F32) for i in range(2)]; sgb = [Buf(), Buf()]
        ex = [sb("ex%d" % i, [128, 512], F32) for i in range(3)]; exb = [Buf() for _ in range(3)]
        eg = sb("eg", [128, 2, 8], F32); egb = Buf()
        qe0 = sb("qe0", [128, 2, 512], BF16); qe1 = sb("qe1", [128, 2, 512], BF16)
        ke = sb("ke", [128, 2, 512], BF16); kg = sb("kg", [128, 2, 512], BF16)
        qeb, keb, kgb = Buf(), Buf(), Buf()
        kgT = sb("kgT", [128, 4, 256], BF16); kgTb = Buf()
        AT = [sb("AT%d" % i, [128, 128], BF16) for i in range(2)]; ATb = [Buf(), Buf()]
        Cst = sb("Cst", [128, 8, 257], F32)
        Cb = sb("Cb", [128, 8, 2, 258], BF16)
        Cstb = [Buf() for _ in range(8)]; Cbb = [[Buf(), Buf()] for _ in range(8)]
        numS = [sb("numS%d" % i, [128, 257], F32) for i in range(2)]; numSb = [Buf(), Buf()]
        ytmp = [sb("ytmp%d" % i, [128, 256], F32) for i in range(2)]; ytmpb = [Buf(), Buf()]
        sm = sb("sm", [128, 16], F32); smb = [Buf(), Buf()]
        outst = [sb("outst%d" % i, [128, 4, 256], BF16) for i in range(2)]; outstb = [Buf(), Buf()]
        maskUT = sb("maskUT", [128, 128], F32); rst = sb("rst", [128, 512], F32)
        mnw = sb("mnw", [128, 512], F32); hnw = sb("hnw", [128, 512], F32)
        cw = sb("cw", [128, 8, 4], F32); cbias = sb("cbias", [128, 8], F32)
        lbt = sb("lbt", [128, 4, 4], F32)
        gbt = sb("gbt", [1, 8], F32)
        rows = sb("rows", [1, 6, 512], F32); rowsb = [Buf() for _ in range(6)]
        ones1 = sb("ones1", [1, 128], F32)
        constb = Buf()

        S_.barrier()
        S_.dma("sp", lambda e: e.dma_start(out=maskUT[:], in_=I["maskUT"]), writes=[constb])
        S_.dma("sp", lambda e: e.dma_start(out=rst[:], in_=I["rst"]), writes=[constb])
        S_.dma("sp", lambda e: e.dma_start(out=mnw[:], in_=I["mnw"]), writes=[constb])
        S_.dma("sp", lambda e: e.dma_start(out=hnw[:], in_=I["hnw"]), writes=[constb])
        S_.dma("sp", lambda e: e.dma_start(out=cw[:], in_=I["cw"]), writes=[constb])
        S_.dma("sp", lambda e: e.dma_start(out=cbias[:], in_=I["cb"]), writes=[constb])
        S_.dma("sp", lambda e: e.dma_start(out=lbt[:, :, 0:2], in_=I["lb"]), writes=[constb])
        S_.dma("sp", lambda e: e.dma_start(out=gbt[:], in_=I["gb"]), writes=[constb])
        S_.dma("pool", lambda e: e.dma_start(out=wg[:], in_=I["wg"]), writes=[wgb])
        S_.op("dve", lambda e: e.memset(ones1[:], 1.0), writes=[constb])
        S_.op("dve", lambda e: e.memset(hist[:], 0.0), writes=histb)
        S_.op("dve", lambda e: e.memset(Cst[:], 0.0), writes=Cstb)
        S_.op("dve", lambda e: e.memset(Cb[:], 0.0), writes=[b for p in Cbb for b in p])
        S_.op("dve", lambda e: e.memset(qe0[:], 0.0), writes=[qeb])
        S_.op("dve", lambda e: e.memset(qe1[:], 0.0), writes=[qeb])
        for i in range(2):
            S_.op("dve", lambda e, i=i: e.memset(TA[i][:], 1.0), writes=[TAb[i]])
        S_.op("dve", lambda e: e.tensor_tensor(out=lbt[:, :, 2], in0=lbt[:, :, 0], in1=lbt[:, :, 1], op=ALU.subtract),
              reads=[constb], writes=[constb])
        S_.op("act", lambda e: e.activation(out=lbt[:, :, 2], in_=lbt[:, :, 2], func=AF.Sigmoid),
              reads=[constb], writes=[constb])
        S_.op("dve", lambda e: e.tensor_scalar(out=lbt[:, :, 3], in0=lbt[:, :, 2], scalar1=-1.0, scalar2=1.0,
                                               op0=ALU.mult, op1=ALU.add), reads=[constb], writes=[constb])
        S_.op("dve", lambda e: e.tensor_scalar(out=gbt[:, 4:6], in0=gbt[:, 2:4], scalar1=-1.0, scalar2=None,
                                               op0=ALU.mult), reads=[constb], writes=[constb])

        wcount = [0]

        def load_w(t):
            k = wcount[0] % 3
            wcount[0] += 1
            S_.dma("pool", lambda e, k=k, t=t: e.dma_start(out=W[k][:], in_=I["w1"][t]), writes=[Wb[k]])
            return k

        cnt = {"acc": 0, "ex": 0, "num": 0, "AT": 0}

        def gla(tt, u, FAq, FBk, FBk_b, TAv, TAv_b, nd, dv, hsel, sidx, ebuf, eamb, egba, egap, post):
            qv = lambda t_: t_.rearrange("p (a c l) -> p a c l", c=2, l=64)
            for dc in range(nd):
                S_.op("dve", lambda e, dc=dc: e.tensor_tensor(out=qv(qe0[:, dc, :])[:, :, 0, :], in0=qv(FAq[:, dc, :])[:, :, 0, :],
                                                             in1=qv(ebuf)[:, :, 0, :], op=ALU.mult),
                      reads=[FAb[u], exb[0]], writes=[qeb])
                S_.op("dve", lambda e, dc=dc: e.tensor_tensor(out=qv(qe1[:, dc, :])[:, :, 1, :], in0=qv(FAq[:, dc, :])[:, :, 1, :],
                                                             in1=qv(ebuf)[:, :, 1, :], op=ALU.mult),
                      reads=[FAb[u], exb[0]], writes=[qeb])
                S_.op("dve", lambda e, dc=dc: e.tensor_tensor(out=ke[:, dc, :], in0=FBk[:, dc, :], in1=eamb, op=ALU.mult),
                      reads=[FBk_b, exb[1]], writes=[keb])
                S_.op("dve", lambda e, dc=dc: e.tensor_tensor(out=kg[:, dc, :], in0=FBk[:, dc, :], in1=egba, op=ALU.mult),
                      reads=[FBk_b, exb[2]], writes=[kgb])
            for ts in range(4):
                pt, pb = getb()
                for dc in range(nd):
                    S_.op("pe", lambda e, pt=pt, dc=dc, ts=ts: e.transpose(out=pt[:, dc * 128:(dc + 1) * 128],
                                                                          in_=kg[:, dc, ts * 128:(ts + 1) * 128],
                                                                          identity=C.ident[:]),
                          reads=[kgb, C.identb], writes=[pb])
                S_.op("act", lambda e, pt=pt, ts=ts: e.copy(out=kgT[:, ts, 0:nd * 128], in_=pt[:, 0:nd * 128]),
                      reads=[pb], writes=[kgTb])
            for ts in range(4):
                tok = slice(ts * 128, (ts + 1) * 128)
                pa, pab = getf()
                n_mm = 2 * nd
                i_mm = 0
                for dc in range(nd):
                    for qq in (qe0, qe1):
                        S_.op("pe", lambda e, pa=pa, dc=dc, qq=qq, i_mm=i_mm, tok=tok: e.matmul(
                            pa[:, 0:128], lhsT=ke[:, dc, tok], rhs=qq[:, dc, tok], start=(i_mm == 0), stop=(i_mm == n_mm - 1)),
                            reads=[keb, qeb], writes=[pab])
                        i_mm += 1
                ai = cnt["AT"] % 2
                cnt["AT"] += 1
                S_.op("dve", lambda e, pa=pa, ai=ai: e.tensor_tensor(out=AT[ai][:], in0=pa[:, 0:128], in1=maskUT[:], op=ALU.mult),
                      reads=[pab, constb], writes=[ATb[ai]])
                v = TAv(ts)
                for c in range(2):
                    rowsl = slice(c * 64, (c + 1) * 64)
                    if c == 1:
                        po, pob = getf()
                        S_.op("pe", lambda e, po=po, ai=ai, v=v: e.matmul(po[:, 0:dv], lhsT=AT[ai][:], rhs=v, start=True, stop=False),
                              reads=[ATb[ai], TAv_b], writes=[pob])
                        for dc in range(nd):
                            si = sidx[dc]
                            S_.op("pe", lambda e, po=po, dc=dc, si=si, tok=tok: e.matmul(po[:, 0:dv], lhsT=qe0[:, dc, tok], rhs=Cb[:, si, 0, 0:dv],
                                                                               start=False, stop=False),
                                  reads=[qeb, Cbb[si][0]], writes=[pob])
                            S_.op("pe", lambda e, po=po, dc=dc, si=si, tok=tok: e.matmul(po[:, 0:dv], lhsT=qe1[:, dc, tok], rhs=Cb[:, si, 1, 0:dv],
                                                                               start=False, stop=(dc == nd - 1)),
                                  reads=[qeb, Cbb[si][1]], writes=[pob])
                        post(ts, po, pob)
                    for dc in range(nd):
                        si = sidx[dc]
                        pu, pub = getf()
                        S_.op("pe", lambda e, pu=pu, dc=dc, rowsl=rowsl, v=v, ts=ts: e.matmul(
                            pu[:, 0:dv], lhsT=kgT[rowsl, ts, dc * 128:(dc + 1) * 128], rhs=v[rowsl, :], start=True, stop=True),
                            reads=[kgTb, TAv_b], writes=[pub])
                        ch = ts * 2 + c
                        S_.op("dve", lambda e, pu=pu, si=si, ch=ch, dc=dc: e.scalar_tensor_tensor(
                            out=Cst[:, si, 0:dv], in0=Cst[:, si, 0:dv], scalar=egap(dc)[:, ch:ch + 1], in1=pu[:, 0:dv],
                            op0=ALU.mult, op1=ALU.add), reads=[pub, egb, Cstb[si]], writes=[Cstb[si]])
                        tgt = 1 if c == 0 else 0
                        S_.op("act", lambda e, si=si, tgt=tgt: e.copy(out=Cb[:, si, tgt, 0:dv], in_=Cst[:, si, 0:dv]),
                              reads=[Cstb[si]], writes=[Cbb[si][tgt]])

        def small(i):
            return sm[:, i:i + 1]

        for tt in range(ntiles):
            S_.dma("sp", lambda e, tt=tt: e.dma_start(out=hT[:, :, :].rearrange("p a b -> p (a b)"), in_=C.hT1[tt, :, :]),
                   reads=[C.dramb], writes=[hTb])
            for gi in range(4):
                pg, pgb = getf()
                for kc in range(32):
                    S_.op("pe", lambda e, pg=pg, gi=gi, kc=kc: e.matmul(pg[0:1, :], lhsT=wg[:, kc, gi:gi + 1], rhs=hT[:, kc, :],
                                                                       start=(kc == 0), stop=(kc == 31)),
                          reads=[wgb, hTb], writes=[pgb])
                h = gi % 2
                if gi < 2:
                    S_.op("act", lambda e, pg=pg, h=h: e.activation(out=rows[:, h, :], in_=pg[0:1, :], func=AF.Identity,
                                                                    bias=gbt[:, h:h + 1], scale=1.0),
                          reads=[pgb, constb], writes=[rowsb[h]])
                else:
                    S_.op("act", lambda e, pg=pg, h=h: e.activation(out=rows[:, 4, :], in_=pg[0:1, :], func=AF.Exp,
                                                                    bias=gbt[:, 4 + h:5 + h], scale=-1.0),
                          reads=[pgb, constb], writes=[rowsb[4]])
                    S_.op("act", lambda e: e.activation(out=rows[:, 4, :], in_=rows[:, 4, :], func=AF.Ln, bias=1.0, scale=1.0),
                          reads=[rowsb[4]], writes=[rowsb[4]])
                    S_.op("dve", lambda e, h=h: e.tensor_tensor_scan(out=rows[:, 2 + h, :], data0=rst[0:1, :], data1=rows[:, 4, :],
                                                                     initial=0.0, op0=ALU.mult, op1=ALU.subtract),
                          reads=[rowsb[4], constb], writes=[rowsb[2 + h]])
            def inproj(u):
                ub = u % 2
                mls = u < 2
                for part in range(4):
                    k = load_w(u * 4 + part)
                    if part < 2:
                        for cc in range(2):
                            pp, ppb = getf()
                            for kc in range(32):
                                S_.op("pe", lambda e, pp=pp, k=k, cc=cc, kc=kc: e.matmul(
                                    pp[:, :], lhsT=W[k][:, kc, cc * 128:(cc + 1) * 128], rhs=hT[:, kc, :],
                                    start=(kc == 0), stop=(kc == 31)), reads=[Wb[k], hTb], writes=[ppb])
                            if mls:
                                ci = u * 4 + part * 2 + cc
                                cwi = part * 4 + u * 2 + cc
                                pk = cnt["acc"] % 2
                                cnt["acc"] += 1
                                S_.op("act", lambda e, pp=pp, pk=pk: e.copy(out=pre[pk][:, 3:515], in_=pp[:, :]),
                                      reads=[ppb], writes=[preb[pk]])
                                S_.op("dve", lambda e, pk=pk, cwi=cwi: e.tensor_copy(out=pre[pk][:, 0:3], in_=hist[:, cwi, 0:3]),
                                      reads=[histb[cwi]], writes=[preb[pk]])
                                S_.op("dve", lambda e, pk=pk, cwi=cwi: e.tensor_scalar(
                                    out=acc[pk][:], in0=pre[pk][:, 3:515], scalar1=cw[:, cwi, 3:4], scalar2=cbias[:, cwi:cwi + 1],
                                    op0=ALU.mult, op1=ALU.add), reads=[preb[pk], constb], writes=[accb[pk]])
                                for tap in range(3):
                                    S_.op("dve", lambda e, pk=pk, cwi=cwi, tap=tap: e.scalar_tensor_tensor(
                                        out=acc[pk][:], in0=pre[pk][:, tap:tap + 512], scalar=cw[:, cwi, tap:tap + 1], in1=acc[pk][:],
                                        op0=ALU.mult, op1=ALU.add), reads=[preb[pk], constb, accb[pk]], writes=[accb[pk]])
                                S_.op("dve", lambda e, pk=pk, cwi=cwi: e.tensor_copy(out=hist[:, cwi, 0:3], in_=pre[pk][:, 512:515]),
                                      reads=[preb[pk]], writes=[histb[cwi]])
                                S_.op("act", lambda e, pk=pk: e.activation(out=sg[pk][:], in_=acc[pk][:], func=AF.Sigmoid),
                                      reads=[accb[pk]], writes=[sgb[pk]])
                                dstt = FA[ub] if part == 0 else FB[ub]
                                dstb = FAb[ub] if part == 0 else FBb[ub]
                                cmul = 1.0 if part == 0 else 1.0 / 16.0
                                S_.op("dve", lambda e, pk=pk, dstt=dstt, cc=cc, cmul=cmul: e.scalar_tensor_tensor(
                                    out=dstt[:, cc, :], in0=acc[pk][:], scalar=cmul, in1=sg[pk][:], op0=ALU.mult, op1=ALU.mult),
                                    reads=[accb[pk], sgb[pk]], writes=[dstb])
                            else:
                                hh = (u - 2) * 2 + cc
                                pk = cnt["acc"] % 2
                                cnt["acc"] += 1
                                if part == 0:
                                    S_.op("act", lambda e, pp=pp, pk=pk: e.activation(out=sg[pk][:], in_=pp[:, :], func=AF.Sigmoid),
                                          reads=[ppb], writes=[sgb[pk]])
                                    S_.op("dve", lambda e, pp=pp, pk=pk, cc=cc, ub=ub: e.tensor_tensor(
                                        out=FA[ub][:, cc, :], in0=pp[:, :], in1=sg[pk][:], op=ALU.mult),
                                        reads=[ppb, sgb[pk]], writes=[FAb[ub]])
                                else:
                                    S_.op("act", lambda e, pp=pp, pk=pk: e.activation(out=sg[pk][:], in_=pp[:, :], func=AF.Sigmoid),
                                          reads=[ppb], writes=[sgb[pk]])
                                    S_.op("dve", lambda e, pk=pk, hh=hh: e.tensor_scalar(
                                        out=acc[pk][:], in0=sg[pk][:], scalar1=lbt[:, hh, 3:4], scalar2=lbt[:, hh, 2:3],
                                        op0=ALU.mult, op1=ALU.add), reads=[sgb[pk], constb], writes=[accb[pk]])
                                    S_.op("dve", lambda e, pk=pk, cc=cc, ub=ub: e.tensor_scalar(
                                        out=FB[ub][:, cc, :], in0=acc[pk][:], scalar1=-1.0, scalar2=1.0, op0=ALU.mult, op1=ALU.add),
                                        reads=[accb[pk]], writes=[FBb[ub]])
                                    S_.op("act", lambda e, pk=pk: e.activation(out=sg[pk][:], in_=acc[pk][:], func=AF.Ln),
                                          reads=[accb[pk]], writes=[sgb[pk]])
                                    S_.op("dve", lambda e, pk=pk, cc=cc, ub=ub: e.tensor_tensor_scan(
                                        out=FBf[ub][:, cc, :], data0=rst[:], data1=sg[pk][:], initial=0.0, op0=ALU.mult, op1=ALU.add),
                                        reads=[sgb[pk], constb], writes=[FBb[ub]])
                    else:
                        for ts in range(4):
                            pp, ppb = getf()
                            for kc in range(32):
                                S_.op("pe", lambda e, pp=pp, k=k, ts=ts, kc=kc: e.matmul(
                                    pp[:, 0:256], lhsT=hT[:, kc, ts * 128:(ts + 1) * 128], rhs=W[k][:, kc, :],
                                    start=(kc == 0), stop=(kc == 31)), reads=[Wb[k], hTb], writes=[ppb])
                            if part == 2:
                                S_.op("act", lambda e, pp=pp, ts=ts, ub=ub: e.copy(out=TA[ub][:, ts, 0:256], in_=pp[:, 0:256]),
                                      reads=[ppb], writes=[TAb[ub]])
                            else:
                                S_.op("act", lambda e, pp=pp, ts=ts, ub=ub: e.activation(out=TB[ub][:, ts, :], in_=pp[:, 0:256], func=AF.Sigmoid),
                                      reads=[ppb], writes=[TBb[ub]])
            def recur(u):
                ub = u % 2
                mls = u < 2
                ob = outstb[ub]
                if mls:
                    h = u
                    S_.op("dve", lambda e, h=h: e.tensor_tensor(out=rows[:, 5, :], in0=rows[:, h, :], in1=rows[:, 2 + h, :], op=ALU.subtract),
                          reads=[rowsb[h], rowsb[2 + h]], writes=[rowsb[5]])
                    r3 = lambda ap: ap.rearrange("p (c l) -> p c l", l=64)
                    S_.op("dve", lambda e, h=h: e.tensor_tensor(
                        out=r3(rows[:, 4, :]), in0=r3(rows[:, 5, :]), in1=r3(rows[:, 2 + h, :])[:, :, 63:64].to_broadcast([1, 8, 64]),
                        op=ALU.add), reads=[rowsb[5], rowsb[2 + h]], writes=[rowsb[4]])
                    for (ri, xi, rb_) in ((2 + h, 0, rowsb[2 + h]), (5, 1, rowsb[5]), (4, 2, rowsb[4])):
                        pbc, pbcb = getf()
                        S_.op("pe", lambda e, pbc=pbc, ri=ri: e.matmul(pbc[:, :], lhsT=ones1[:, :], rhs=rows[:, ri, :], start=True, stop=True),
                              reads=[rb_, constb], writes=[pbcb])
                        S_.op("act", lambda e, pbc=pbc, xi=xi: e.activation(out=ex[xi][:], in_=pbc[:, :], func=AF.Exp),
                              reads=[pbcb], writes=[exb[xi]])
                    S_.op("dve", lambda e: e.tensor_copy(out=eg[:, 0, :], in_=ex[0][:].rearrange("p (c l) -> p c l", l=64)[:, :, 63]),
                          reads=[exb[0]], writes=[egb])

                    def post_m(ts, po, pob, h=h, ub=ub, ob=ob):
                        ni = cnt["num"] % 2
                        cnt["num"] += 1
                        o0 = ni * 8
                        S_.op("act", lambda e: e.copy(out=numS[ni][:], in_=po[:, 0:257]), reads=[pob], writes=[numSb[ni]])
                        S_.op("act", lambda e: e.activation(out=small(o0), in_=numS[ni][:, 256:257], func=AF.Abs),
                              reads=[numSb[ni]], writes=[smb[ni]])
                        S_.op("dve", lambda e: e.tensor_scalar(out=small(o0), in0=small(o0), scalar1=1.0, scalar2=None,
                                                               op0=ALU.max), reads=[smb[ni]], writes=[smb[ni]])
                        S_.op("dve", lambda e: e.reciprocal(out=small(o0 + 1), in_=small(o0)), reads=[smb[ni]], writes=[smb[ni]])
                        S_.op("act", lambda e: e.activation(out=ytmp[ni][:], in_=numS[ni][:, 0:256], func=AF.Square,
                                                            accum_out=small(o0 + 2)), reads=[numSb[ni]], writes=[ytmpb[ni], smb[ni]])
                        S_.op("dve", lambda e: e.tensor_tensor(out=small(o0 + 3), in0=small(o0 + 1), in1=small(o0 + 1), op=ALU.mult),
                              reads=[smb[ni]], writes=[smb[ni]])
                        S_.op("dve", lambda e: e.tensor_tensor(out=small(o0 + 3), in0=small(o0 + 3), in1=small(o0 + 2), op=ALU.mult),
                              reads=[smb[ni]], writes=[smb[ni]])
                        S_.op("act", lambda e: e.activation(out=small(o0 + 3), in_=small(o0 + 3), func=AF.Ln, bias=C.epsc[:, 0:1],
                                                            scale=1.0 / 256), reads=[smb[ni], C.identb], writes=[smb[ni]])
                        S_.op("act", lambda e: e.activation(out=small(o0 + 3), in_=small(o0 + 3), func=AF.Exp, scale=-0.5),
                              reads=[smb[ni]], writes=[smb[ni]])
                        S_.op("dve", lambda e: e.tensor_tensor(out=small(o0 + 3), in0=small(o0 + 3), in1=small(o0 + 1), op=ALU.mult),
                              reads=[smb[ni]], writes=[smb[ni]])
                        S_.op("dve", lambda e: e.scalar_tensor_tensor(out=ytmp[ni][:], in0=numS[ni][:, 0:256], scalar=small(o0 + 3),
                                                                      in1=mnw[:, h * 256:(h + 1) * 256], op0=ALU.mult, op1=ALU.mult),
                              reads=[numSb[ni], smb[ni], constb], writes=[ytmpb[ni]])
                        S_.op("dve", lambda e: e.tensor_tensor(out=outst[ub][:, ts, :], in0=ytmp[ni][:], in1=TB[ub][:, ts, :], op=ALU.mult),
                              reads=[ytmpb[ni], TBb[ub]], writes=[ob])

                    gla(tt, ub, FA[ub], FB[ub], FBb[ub], lambda ts: TA[ub][:, ts, 0:257], TAb[ub], 2, 257, h, [h * 2, h * 2 + 1],
                        ex[0][:], ex[1][:], ex[2][:], lambda dc: eg[:, 0, :], post_m)
                    col0 = h * 256
                else:
                    for cc in range(2):
                        hh = (u - 2) * 2 + cc
                        bsrc = FBf[ub][:, cc, :]
                        S_.op("act", lambda e, bsrc=bsrc: e.activation(out=ex[0][:], in_=bsrc, func=AF.Exp), reads=[FBb[ub]], writes=[exb[0]])
                        S_.op("dve", lambda e, bsrc=bsrc: e.tensor_scalar(out=ex[1][:], in0=bsrc, scalar1=-1.0, scalar2=80.0,
                                                                          op0=ALU.mult, op1=ALU.min), reads=[FBb[ub]], writes=[exb[1]])
                        S_.op("act", lambda e: e.activation(out=ex[1][:], in_=ex[1][:], func=AF.Exp), reads=[exb[1]], writes=[exb[1]])
                        r3 = lambda ap: ap.rearrange("p (c l) -> p c l", l=64)
                        S_.op("dve", lambda e, bsrc=bsrc: e.tensor_tensor(
                            out=r3(ex[2][:]), in0=r3(bsrc)[:, :, 63:64].to_broadcast([128, 8, 64]), in1=r3(bsrc), op=ALU.subtract),
                            reads=[FBb[ub]], writes=[exb[2]])
                        S_.op("act", lambda e: e.activation(out=ex[2][:], in_=ex[2][:], func=AF.Exp), reads=[exb[2]], writes=[exb[2]])
                        S_.op("dve", lambda e, cc=cc: e.tensor_copy(out=eg[:, cc, :], in_=r3(ex[0][:])[:, :, 63]),
                              reads=[exb[0]], writes=[egb])

                        def post_h(ts, po, pob, hh=hh, cc=cc, ub=ub, ob=ob):
                            ni = cnt["num"] % 2
                            cnt["num"] += 1
                            o0 = ni * 8
                            S_.op("act", lambda e: e.activation(out=ytmp[ni][:, 0:128], in_=po[:, 0:128], func=AF.Square,
                                                                accum_out=small(o0 + 2)), reads=[pob], writes=[ytmpb[ni], smb[ni]])
                            S_.op("act", lambda e: e.activation(out=small(o0 + 3), in_=small(o0 + 2), func=AF.Ln, bias=C.epsc[:, 0:1],
                                                                scale=1.0 / 128), reads=[smb[ni], C.identb], writes=[smb[ni]])
                            S_.op("act", lambda e: e.activation(out=small(o0 + 3), in_=small(o0 + 3), func=AF.Exp, scale=-0.5),
                                  reads=[smb[ni]], writes=[smb[ni]])
                            S_.op("dve", lambda e: e.scalar_tensor_tensor(out=ytmp[ni][:, 0:128], in0=po[:, 0:128], scalar=small(o0 + 3),
                                                                          in1=hnw[:, hh * 128:(hh + 1) * 128], op0=ALU.mult, op1=ALU.mult),
                                  reads=[pob, smb[ni], constb], writes=[ytmpb[ni]])
                            S_.op("dve", lambda e: e.tensor_tensor(out=outst[ub][:, ts, cc * 128:(cc + 1) * 128], in0=ytmp[ni][:, 0:128],
                                                                    in1=TB[ub][:, ts, cc * 128:(cc + 1) * 128], op=ALU.mult),
                                  reads=[ytmpb[ni], TBb[ub]], writes=[ob])

                        gla(tt, ub, FA[ub][:, cc:cc + 1, :], FB[ub][:, cc:cc + 1, :], FBb[ub],
                            lambda ts, cc=cc: TA[ub][:, ts, cc * 128:(cc + 1) * 128], TAb[ub], 1, 128, hh, [4 + hh],
                            ex[0][:], ex[1][:], ex[2][:], lambda dc, cc=cc: eg[:, cc, :], post_h)
                    col0 = 512 + (u - 2) * 256
                S_.dma("sp", lambda e, ub=ub, tt=tt, col0=col0: e.dma_start(
                    out=C.cc1_in[tt * 512:(tt + 1) * 512, col0:col0 + 256].rearrange("(a p) c -> p a c", p=128), in_=outst[ub][:]),
                    reads=[ob], writes=[C.dramb])

            inproj(0)
            for u in range(4):
                if u < 3:
                    inproj(u + 1)
                recur(u)
        if dbg is not None:
            S_.barrier()
            for nm, t in (("FA1", FA[1]), ("FB1", FB[1]), ("FBf1", FBf[1]), ("TA1", TA[1]), ("TB1", TB[1]), ("ex0", ex[0]), ("ex1", ex[1]),
                          ("ex2", ex[2]), ("kgT", kgT), ("Cst", Cst), ("qe0", qe0), ("qe1", qe1), ("ke", ke), ("kg", kg), ("hT", hT), ("W0", W[0]), ("W1", W[1])):
                shp = list(t.shape)
                d_ = nc.dram_tensor("dbg_" + nm, shp, t.dtype, kind="ExternalOutput").ap()
                S_.dma("sp", lambda e, d_=d_, t=t: e.dma_start(out=d_, in_=t[:]))
        S_.barrier()
        S_.emit()


def build(stages=("p1",), debug=False, ntiles=8):
    nc = bass.Bass("TRN2", target_bir_lowering=False)
    I = {}

    def inp(name, shape, dt=F32):
        I[name] = nc.dram_tensor(name, list(shape), dt, kind="ExternalInput").ap()

    inp("ident", [128, 128]); inp("maskUT", [128, 128]); inp("rst", [128, 512])
    inp("xb", [S, D]); inp("nw1", [128, D]); inp("w1", [16, 128, 32, 256]); inp("wg", [128, 32, 4])
    inp("cw", [128, 8, 4]); inp("cb", [128, 8]); inp("lb", [128, 4, 2]); inp("gb", [1, 8])
    inp("mnw", [128, 512]); inp("hnw", [128, 512])
    C = Ctx()
    dbg_kind = "ExternalOutput" if debug else "Internal"
    C.hT1_t = nc.dram_tensor("hT1", [8, 128, 32 * 512], BF16, kind=dbg_kind)
    C.hT1 = C.hT1_t.ap()
    C.cc1_t = nc.dram_tensor("cc1_in", [S, 1024], BF16, kind=dbg_kind)
    C.cc1_in = C.cc1_t.ap()
    C.dramb = Buf("dram")
    with contextlib.ExitStack() as stack:
        S_ = Sched(nc, stack)
        C.getf, C.getb = _psum_pool(nc, stack, S_)
        C.ident = _sb(nc, stack, "ident", [128, 128], BF16)
        C.identb = Buf("ident")
        S_.dma("pool", lambda e: e.dma_start(out=C.ident[:], in_=I["ident"]), writes=[C.identb])
        C.epsc = _sb(nc, stack, "epsc", [128, 2], F32)
        S_.op("dve", lambda e: e.memset(C.epsc[:], EPS), writes=[C.identb])
        stage_norm_T(nc, S_, C, I["xb"], I["nw1"], C.hT1, ntiles * 512)
        stage_mixer(nc, S_, C, I, ntiles, True if debug else None)
        if debug:
            fin = _sb(nc, stack, "fin", [128, 8], F32)
            S_.barrier()
            S_.emit()
    return nc


def _prep_common():
    ident = np.eye(128, dtype=np.float32)
    s = np.arange(128)
    maskUT = ((s[:, None] // 64 == s[None, :] // 64) & (s[:, None] <= s[None, :])).astype(np.float32)
    rst = np.ones((128, 512), np.float32)
    rst[:, ::64] = 0.0
    return {"ident": ident, "maskUT": maskUT, "rst": rst}


def _wtile(wcols):
    return np.ascontiguousarray(wcols.reshape(32, 128, wcols.shape[1]).transpose(1, 0, 2))


def prep_p1(c, inputs):
    b, j = c // 4, c % 4
    w_in = inputs["w_in"][0]
    MW = 2048
    off = {"qm": 0, "km": MW, "vm": 2 * MW, "om": 3 * MW, "ipre": 4 * MW, "fpre": 4 * MW + 8,
           "qh": 4 * MW + 16, "fh": 5 * MW + 16, "ih": 6 * MW + 16, "gh": 7 * MW + 16,
           "gm": 8 * MW + 16, "ghh": 8 * MW + 16 + D}
    tiles = []
    for u in range(2):
        hd = 2 * j + u
        for nm in ("qm", "km", "vm", "om"):
            tiles.append(_wtile(w_in[:, off[nm] + hd * 256: off[nm] + (hd + 1) * 256]))
    for u in range(2):
        h0 = 4 * j + 2 * u
        for nm in ("qh", "fh", "ih", "gh"):
            tiles.append(_wtile(w_in[:, off[nm] + h0 * 128: off[nm] + (h0 + 2) * 128]))
    w1 = np.stack(tiles, 0)
    gcols = np.stack([w_in[:, off["ipre"] + 2 * j], w_in[:, off["ipre"] + 2 * j + 1],
                      w_in[:, off["fpre"] + 2 * j], w_in[:, off["fpre"] + 2 * j + 1]], 1)
    wg = _wtile(gcols)
    conv_w = inputs["conv_qk_w"][0]
    conv_b = inputs["conv_qk_b"][0]
    cw = np.zeros((128, 8, 4), np.float32)
    cb = np.zeros((128, 8), np.float32)
    for part in range(2):
        for u in range(2):
            for cc in range(2):
                ch0 = part * 2048 + (2 * j + u) * 256 + cc * 128
                idx = part * 4 + u * 2 + cc
                cw[:, idx, :] = conv_w[:, ch0:ch0 + 128].T
                cb[:, idx] = conv_b[ch0:ch0 + 128]
    lbr = inputs["hgrn_lb"]
    lb = np.zeros((128, 4, 2), np.float32)
    for hh in range(4):
        ch0 = (4 * j + hh) * 128
        lb[:, hh, 0] = lbr[0, ch0:ch0 + 128]
        lb[:, hh, 1] = lbr[1, ch0:ch0 + 128]
    gbv = inputs["gate_if_b"][0]
    gb = np.zeros((1, 8), np.float32)
    gb[0, 0:2] = gbv[2 * j:2 * j + 2]
    gb[0, 2:4] = gbv[8 + 2 * j:8 + 2 * j + 2]
    mnw = np.broadcast_to(inputs["mlstm_norm_w"][0][j * 512:(j + 1) * 512], (128, 512))
    hnw = np.broadcast_to(inputs["hgrn_norm_w"][0][j * 512:(j + 1) * 512], (128, 512))
    d = {"xb": inputs["x"][b], "nw1": np.broadcast_to(inputs["norm_mix_w"][0], (128, D)), "w1": w1, "wg": wg,
         "cw": cw, "cb": cb, "lb": lb, "gb": gb, "mnw": mnw, "hnw": hnw}
    return {k: np.ascontiguousarray(v, dtype=np.float32) for k, v in d.items()}


def x_mm(S_, out, lhsT, rhs, start, stop, reads, writes):
    S_.op("pe", lambda e: e.matmul(out, lhsT=lhsT, rhs=rhs, start=start, stop=stop), reads, writes)


def x_tr(S_, C, out, in_, reads, writes):
    S_.op("pe", lambda e: e.transpose(out=out, in_=in_, identity=C.ident[:]), list(reads) + [C.identb], writes)


def x_act(S_, out, in_, func, reads, writes, **kw):
    S_.op("act", lambda e: e.activation(out=out, in_=in_, func=func, **kw), reads, writes)


def x_cp(S_, eng, out, in_, reads, writes):
    if eng == "act":
        S_.op("act", lambda e: e.copy(out=out, in_=in_), reads, writes)
    else:
        S_.op(eng, lambda e: e.tensor_copy(out=out, in_=in_), reads, writes)


def x_tt(S_, eng, out, in0, in1, op, reads, writes):
    S_.op(eng, lambda e: e.tensor_tensor(out=out, in0=in0, in1=in1, op=op), reads, writes)


def x_ts(S_, eng, out, in0, s1, s2, op0, op1, reads, writes):
    if op1 is None:
        S_.op(eng, lambda e: e.tensor_scalar(out=out, in0=in0, scalar1=s1, scalar2=None, op0=op0), reads, writes)
    else:
        S_.op(eng, lambda e: e.tensor_scalar(out=out, in0=in0, scalar1=s1, scalar2=s2, op0=op0, op1=op1), reads, writes)


def x_stt(S_, eng, out, in0, scalar, in1, op0, op1, reads, writes):
    S_.op(eng, lambda e: e.scalar_tensor_tensor(out=out, in0=in0, scalar=scalar, in1=in1, op0=op0, op1=op1), reads, writes)


def x_red(S_, out, in_, op, reads, writes):
    S_.op("dve", lambda e: e.tensor_reduce(out=out, in_=in_, axis=AX.X, op=op), reads, writes)


def x_dma(S_, q, out, in_, reads, writes):
    return S_.dma(q, lambda e: e.dma_start(out=out, in_=in_), reads, writes)


def x_rstd(S_, C, out, in_, n, reads, writes):
    x_act(S_, out, in_, AF.Ln, list(reads) + [C.identb], writes, bias=C.epsc[:, 0:1], scale=1.0 / n)
    x_act(S_, out, out, AF.Exp, writes, writes, scale=-0.5)


def coll(S_, nc, kind, groups, src, dst, reads, writes):
    if not hasattr(S_, "cc_sem"):
        S_.cc_sem = S_.new_sem("ccsem")
        S_.cc_val = 0
    sem = S_.cc_sem
    deps = S_._deps(reads, writes)
    if S_.cc_val > 0:
        deps.append((sem, S_.cc_val, "dma"))
    S_.cc_val += 1
    waits = S_._waits("pool", deps)
    ev = (sem, S_.cc_val, "dma")
    S_.streams["pool"].append((waits, lambda e: e.collective_compute(kind, ALU.bypass, replica_groups=groups, ins=[src], outs=[dst]), sem, 1))
    S_._mark(ev, reads, writes)


def stage_branch(nc, S_, C, I):
    with contextlib.ExitStack() as st:
        getf, getb = C.getf, C.getb
        sb = lambda name, shape, dt: _sb(nc, st, "b_" + name, shape, dt)
        hoT = sb("hoT", [128, 32, 512], BF16); hoTb = Buf()
        hT2 = sb("hT2", [128, 32, 512], BF16); hT2b = Buf()
        yT = sb("yT", [128, 32, 512], BF16); yTb = Buf()
        W = [sb("W%d" % i, [128, 96, 128], BF16) for i in range(2)]; Wb = [Buf(), Buf()]
        gt = [sb("gt%d" % i, [128, 1024], BF16) for i in range(4)]; gtb = [Buf() for _ in range(4)]
        gidx = sb("gidx", [128, 8, 4], I32); gidxb = Buf()
        sg = [sb("sg%d" % i, [128, 512], F32) for i in range(4)]; sgb = [Buf() for _ in range(4)]
        S_.barrier()
        x_dma(S_, "sp", gidx[:], I["gidx"], [], [gidxb])
        wc = 0
        for tt in range(2):
            x_dma(S_, "sp", hT2[:, :, :].rearrange("p a b -> p (a b)"), C.hT2[tt, :, :], [C.dramb], [hT2b])
            for ts in range(4):
                sub = tt * 4 + ts
                for jp in range(4):
                    g = gt[jp]
                    S_.dma("pool", lambda e, g=g, sub=sub, jp=jp: e.indirect_dma_start(
                        out=g[:, :], out_offset=None, in_=C.cc1_out[:, :],
                        in_offset=bass.IndirectOffsetOnAxis(ap=gidx[:, sub, jp:jp + 1], axis=0),
                        bounds_check=NCORE * S - 1, oob_is_err=False), reads=[gidxb, C.dramb], writes=[gtb[jp]])
                    pt, pb = getb()
                    for cbk in range(8):
                        x_tr(S_, C, pt[:, cbk * 128:(cbk + 1) * 128], g[:, cbk * 128:(cbk + 1) * 128], [gtb[jp]], [pb])
                    x_cp(S_, "act", hoT[:, jp * 4:jp * 4 + 4, ts * 128:(ts + 1) * 128],
                         pt[:, 0:512].rearrange("p (a b) -> p a b", b=128), [pb], [hoTb])
                    x_cp(S_, "dve", hoT[:, 16 + jp * 4:16 + jp * 4 + 4, ts * 128:(ts + 1) * 128],
                         pt[:, 512:1024].rearrange("p (a b) -> p a b", b=128), [pb], [hoTb])
            for dcn in range(32):
                k = wc % 2
                wc += 1
                x_dma(S_, "pool", W[k][:], I["w2"][dcn], [], [Wb[k]])
                ps = []
                for (i0, n, src, srcb) in ((0, 16, hoT, hoTb), (16, 16, hoT, hoTb), (32, 32, hT2, hT2b), (64, 32, hT2, hT2b)):
                    pp, ppb = getf()
                    for q in range(n):
                        kk = (i0 + q) if i0 < 32 else q
                        x_mm(S_, pp[:, :], W[k][:, i0 + q, :], src[:, kk, :], q == 0, q == n - 1, [Wb[k], srcb], [ppb])
                    ps.append((pp, ppb))
                (pym, pymb), (pyh, pyhb), (pgm, pgmb), (pgh, pghb) = ps
                x_act(S_, sg[0][:], pgm[:, :], AF.Sigmoid, [pgmb], [sgb[0]])
                x_act(S_, sg[1][:], pgh[:, :], AF.Sigmoid, [pghb], [sgb[1]])
                x_tt(S_, "dve", sg[2][:], pym[:, :], sg[0][:], ALU.mult, [pymb, sgb[0]], [sgb[2]])
                x_tt(S_, "dve", sg[3][:], pyh[:, :], sg[1][:], ALU.mult, [pyhb, sgb[1]], [sgb[3]])
                x_tt(S_, "dve", yT[:, dcn, :], sg[2][:], sg[3][:], ALU.add, [sgb[2], sgb[3]], [yTb])
            x_dma(S_, "sp", C.yT[tt, :, :], yT[:, :, :].rearrange("p a b -> p (a b)"), [yTb], [C.dramb])
        S_.barrier()
        S_.emit()


def stage_outproj(nc, S_, C, I):
    with contextlib.ExitStack() as st:
        getf, getb = C.getf, C.getb
        sb = lambda name, shape, dt: _sb(nc, st, "o_" + name, shape, dt)
        yT = sb("yT", [128, 32, 512], BF16); yTb = Buf()
        x2 = sb("x2", [128, 4, D], F32); x2b = [Buf() for _ in range(4)]
        W = [sb("W%d" % i, [128, 32, 256], BF16) for i in range(2)]; Wb = [Buf(), Buf()]
        xn = sb("xn", [128, D], BF16); xnb = Buf()
        h2T = sb("h2T", [128, 32, 128], BF16); h2Tb = Buf()
        nw = sb("nw", [128, D], F32); nwb = Buf()
        wr = sb("wr", [128, 32, 72], BF16); wrb = Buf()
        rb = sb("rb", [128, 72], F32); io8 = sb("io8", [128, 8], F32)
        lg = sb("lg", [128, 72], F32); lgb = Buf()
        t64 = sb("t64", [128, 64], F32); t8 = sb("t8", [128, 6, 8], F32); sm = sb("sm", [128, 16], F32)
        info = sb("info", [128, 16], F32); infob = Buf()
        cb_ = Buf()
        S_.barrier()
        x_dma(S_, "sp", nw[:], I["nw2"], [], [nwb])
        x_dma(S_, "pool", wr[:], I["wr"], [], [wrb])
        x_dma(S_, "sp", rb[:], I["rb"], [], [cb_])
        x_dma(S_, "sp", io8[:], I["io8"], [], [cb_])
        S_.op("dve", lambda e: e.memset(info[:], 0.0), writes=[infob])
        wc = 0
        s_ = lambda i: sm[:, i:i + 1]
        for tt in range(2):
            x_dma(S_, "sp", yT[:, :, :].rearrange("p a b -> p (a b)"), C.yT[tt, :, :], [C.dramb], [yTb])
            for ts in range(4):
                r0 = (tt * 4 + ts) * 128
                x_dma(S_, "sp", x2[:, ts, :], I["xt"][r0:r0 + 128, :], [], [x2b[ts]])
            for ct in range(16):
                k = wc % 2
                wc += 1
                x_dma(S_, "pool", W[k][:], I["w3"][ct], [], [Wb[k]])
                for ts in range(4):
                    pp, ppb = getf()
                    for kc in range(32):
                        x_mm(S_, pp[:, 0:256], yT[:, kc, ts * 128:(ts + 1) * 128], W[k][:, kc, :], kc == 0, kc == 31, [yTb, Wb[k]], [ppb])
                    x_tt(S_, "dve", x2[:, ts, ct * 256:(ct + 1) * 256], x2[:, ts, ct * 256:(ct + 1) * 256], pp[:, 0:256], ALU.add,
                         [ppb, x2b[ts]], [x2b[ts]])
            for ts in range(4):
                r0 = (tt * 4 + ts) * 128
                x_dma(S_, "sp", C.cc2_in[r0:r0 + 128, 0:D], x2[:, ts, :], [x2b[ts]], [C.dramb])
                x_act(S_, xn[:], x2[:, ts, :], AF.Square, [x2b[ts]], [xnb, cb_], accum_out=s_(0))
                x_rstd(S_, C, s_(1), s_(0), D, [cb_], [cb_])
                x_stt(S_, "dve", xn[:], x2[:, ts, :], s_(1), nw[:], ALU.mult, ALU.mult, [x2b[ts], cb_, nwb], [xnb])
                for g in range(4):
                    pt, pb = getb()
                    for q in range(8):
                        kc = g * 8 + q
                        x_tr(S_, C, pt[:, q * 128:(q + 1) * 128], xn[:, kc * 128:(kc + 1) * 128], [xnb], [pb])
                    x_cp(S_, "act" if g % 2 == 0 else "dve", h2T[:, g * 8:(g + 1) * 8, :], pt[:, :].rearrange("p (a b) -> p a b", b=128), [pb], [h2Tb])
                pr, prb = getf()
                for kc in range(32):
                    x_mm(S_, pr[:, 0:72], h2T[:, kc, :], wr[:, kc, :], kc == 0, kc == 31, [h2Tb, wrb], [prb])
                R_, W_ = [lgb, cb_], [cb_]
                x_tt(S_, "dve", lg[:], pr[:, 0:72], rb[:], ALU.add, [prb, cb_], [lgb])
                x_red(S_, s_(2), lg[:, 0:8], ALU.max, R_, W_)
                x_ts(S_, "dve", t8[:, 0, :], lg[:, 0:8], s_(2), None, ALU.is_equal, None, R_, W_)
                x_tt(S_, "dve", t8[:, 1, :], t8[:, 0, :], io8[:], ALU.mult, R_, W_)
                x_red(S_, info[:, 0:1], t8[:, 1, :], ALU.add, R_, [infob, cb_])
                x_ts(S_, "dve", s_(3), s_(2), -1.0, None, ALU.mult, None, R_, W_)
                x_act(S_, t8[:, 1, :], lg[:, 0:8], AF.Exp, R_, W_, bias=s_(3), scale=1.0, accum_out=s_(4))
                S_.op("dve", lambda e: e.reciprocal(out=sm[:, 5:6], in_=sm[:, 4:5]), R_, W_)
                el3 = lg[:, 8:72].rearrange("p (g e) -> p g e", e=8)
                oh3 = t8[:, 0, :].rearrange("p (g o) -> p g o", o=1).to_broadcast([128, 8, 8])
                x_tt(S_, "dve", t64[:].rearrange("p (g e) -> p g e", e=8), el3, oh3, ALU.mult, R_, W_)
                x_red(S_, t8[:, 2, :], t64[:].rearrange("p (g e) -> p e g", e=8), ALU.add, R_, W_)
                x_red(S_, s_(6), t8[:, 2, :], ALU.max, R_, W_)
                x_ts(S_, "dve", t8[:, 3, :], t8[:, 2, :], s_(6), None, ALU.is_equal, None, R_, W_)
                x_stt(S_, "dve", t8[:, 4, :], t8[:, 3, :], -1e30, t8[:, 2, :], ALU.mult, ALU.add, R_, W_)
                x_red(S_, s_(7), t8[:, 4, :], ALU.max, R_, W_)
                x_ts(S_, "dve", t8[:, 5, :], t8[:, 4, :], s_(7), None, ALU.is_equal, None, R_, W_)
                x_tt(S_, "dve", s_(8), s_(7), s_(6), ALU.subtract, R_, W_)
                x_act(S_, s_(9), s_(8), AF.Exp, R_, W_)
                x_ts(S_, "dve", s_(10), s_(9), 1.0, None, ALU.add, None, R_, W_)
                S_.op("dve", lambda e: e.reciprocal(out=sm[:, 10:11], in_=sm[:, 10:11]), R_, W_)
                x_tt(S_, "dve", s_(11), s_(10), s_(5), ALU.mult, R_, W_)
                x_tt(S_, "dve", s_(12), s_(11), s_(9), ALU.mult, R_, W_)
                x_ts(S_, "dve", t8[:, 1, :], t8[:, 3, :], s_(11), None, ALU.mult, None, R_, W_)
                x_stt(S_, "dve", info[:, 1:9], t8[:, 5, :], s_(12), t8[:, 1, :], ALU.mult, ALU.add, R_, [infob, cb_])
                x_dma(S_, "sp", C.cc2_in[r0:r0 + 128, D:D + 16], info[:], [infob], [C.dramb, infob])
        S_.barrier()
        S_.emit()


def stage_moe(nc, S_, C, I):
    NB = CAP // 128
    with contextlib.ExitStack() as st:
        getf, getb = C.getf, C.getb
        sb = lambda name, shape, dt: _sb(nc, st, "e_" + name, shape, dt)
        acc = sb("acc", [128, 4, XW], F32); accb = [Buf() for _ in range(4)]
        XgT = sb("XgT", [128, 32, 512], BF16); XgTb = Buf()
        xn = sb("xn", [128, D], BF16); xnb = Buf()
        aT = sb("aT", [128, 6, 512], BF16); aTb = Buf()
        Wt = [sb("Wt%d" % i, [128, 64, 128], BF16) for i in range(2)]; Wtb = [Buf(), Buf()]
        Wd = [sb("Wd%d" % i, [128, 6, 512], BF16) for i in range(2)]; Wdb = [Buf(), Buf()]
        nw = sb("nw", [128, D], F32); nwb = Buf()
        sg = [sb("sg%d" % i, [128, 512], F32) for i in range(2)]; sgb = [Buf(), Buf()]
        inf = sb("inf", [128, 64, 16], F32); cb_ = Buf()
        wk = sb("wk", [128, 6, 64], F32)
        idx = sb("idx", [128, NB], I32); idxb = Buf()
        gid = sb("gid", [128, 2], F32); Ls = sb("Ls", [128, 128], F32)
        sm = sb("sm", [128, 8], F32)
        s_ = lambda i: sm[:, i:i + 1]
        S_.barrier()
        iotar = sb("iotar", [128, CAP], F32)
        tokhl = sb("tokhl", [128, 64, 4], BF16)
        oh = [sb("oh%d" % i, [128, CAP], BF16) for i in range(2)]; ohb = [Buf(), Buf()]
        res = sb("res", [128, NB, 4], F32)
        x_dma(S_, "sp", inf[:], C.cc2_out[:, D:D + 16].rearrange("(p i) c -> p i c", i=64), [C.dramb], [cb_])
        x_dma(S_, "sp", gid[:, 0:1], I["gid"], [], [cb_])
        x_dma(S_, "sp", Ls[:], I["Ls"], [], [cb_])
        x_dma(S_, "sp", iotar[:], I["iotar"], [], [cb_])
        x_dma(S_, "pool", tokhl[:], I["tokhl"], [], [cb_])
        S_.op("dve", lambda e: e.memset(wk[:, 0, :], 1.0), writes=[cb_])
        S_.op("dve", lambda e: e.memset(acc[:], 0.0), writes=accb)
        R_, W_ = [cb_], [cb_]
        x_ts(S_, "dve", wk[:, 1, :], inf[:, :, 0], gid[:, 0:1], None, ALU.is_equal, None, R_, W_)
        S_.op("dve", lambda e: e.tensor_tensor_scan(out=wk[:, 2, :], data0=wk[:, 0, :], data1=wk[:, 1, :], initial=0.0,
                                                    op0=ALU.mult, op1=ALU.add), R_, W_)
        pp, ppb = getf()
        x_mm(S_, pp[:, 0:1], Ls[:], wk[:, 2, 63:64], True, True, R_, [ppb])
        x_cp(S_, "act", s_(0), pp[:, 0:1], [ppb], W_)
        x_ts(S_, "dve", wk[:, 3, :], wk[:, 2, :], s_(0), None, ALU.add, None, R_, W_)
        x_tt(S_, "dve", wk[:, 3, :], wk[:, 3, :], wk[:, 1, :], ALU.mult, R_, W_)
        x_ts(S_, "dve", wk[:, 3, :], wk[:, 3, :], -1.0, None, ALU.add, None, R_, W_)
        identF = sb("identF", [128, 128], F32)
        rowsT = sb("rowsT", [4, CAP], F32)
        x_dma(S_, "sp", identF[:], I["ident"], [], [cb_])
        pcs = [getf() for _ in range(CAP // 512)]
        for i in range(64):
            k = i % 2
            x_ts(S_, "dve", oh[k][:], iotar[:], wk[:, 3, i:i + 1], None, ALU.is_equal, None, R_, [ohb[k]])
            for c3, (pc, pcb) in enumerate(pcs):
                x_mm(S_, pc[0:4, :], tokhl[:, i, :], oh[k][:, c3 * 512:(c3 + 1) * 512], i == 0, i == 63, [ohb[k], cb_], [pcb])
        for c3, (pc, pcb) in enumerate(pcs):
            x_cp(S_, "act", rowsT[:, c3 * 512:(c3 + 1) * 512], pc[0:4, :], [pcb], W_)
        pt2, pt2b = getf()
        for rb_ in range(NB):
            S_.op("pe", lambda e, rb_=rb_: e.transpose(out=pt2[:, rb_ * 4:rb_ * 4 + 4], in_=rowsT[0:4, rb_ * 128:(rb_ + 1) * 128],
                                                       identity=identF[0:4, 0:4]), R_, [pt2b])
        x_cp(S_, "act", res[:], pt2[:, 0:NB * 4].rearrange("p (a b) -> p a b", b=4), [pt2b], W_)
        x_stt(S_, "dve", res[:, :, 3], res[:, :, 0], 64.0, res[:, :, 1], ALU.mult, ALU.add, R_, W_)
        x_ts(S_, "dve", res[:, :, 3], res[:, :, 3], -100000.0, None, ALU.add, None, R_, W_)
        x_tt(S_, "dve", res[:, :, 3], res[:, :, 3], res[:, :, 2], ALU.mult, R_, W_)
        x_ts(S_, "dve", res[:, :, 3], res[:, :, 3], 100000.0, None, ALU.add, None, R_, W_)
        x_cp(S_, "dve", idx[:], res[:, :, 3], R_, [idxb])
        x_dma(S_, "sp", C.oidx, idx[:], [idxb], [C.dramb])
        wtc = 0
        wdc = 0
        for tt in range(CAP // 512):
            x_dma(S_, "sp", nw[:], I["nw2"], [], [nwb])
            for ts in range(4):
                blk = tt * 4 + ts
                S_.dma("pool", lambda e, ts=ts, blk=blk: e.indirect_dma_start(
                    out=acc[:, ts, :], out_offset=None, in_=C.cc2_out[:, :],
                    in_offset=bass.IndirectOffsetOnAxis(ap=idx[:, blk:blk + 1], axis=0),
                    bounds_check=NCORE * 1024 - 1, oob_is_err=False), reads=[idxb, C.dramb], writes=[accb[ts]])
                x_act(S_, xn[:], acc[:, ts, 0:D], AF.Square, [accb[ts]], [xnb, cb_], accum_out=s_(1))
                x_rstd(S_, C, s_(2), s_(1), D, [cb_], [cb_])
                x_stt(S_, "dve", xn[:], acc[:, ts, 0:D], s_(2), nw[:], ALU.mult, ALU.mult, [accb[ts], cb_, nwb], [xnb])
                for g in range(4):
                    pt, pb = getb()
                    for q in range(8):
                        kc = g * 8 + q
                        x_tr(S_, C, pt[:, q * 128:(q + 1) * 128], xn[:, kc * 128:(kc + 1) * 128], [xnb], [pb])
                    x_cp(S_, "act" if g % 2 == 0 else "dve", XgT[:, g * 8:(g + 1) * 8, ts * 128:(ts + 1) * 128],
                         pt[:, :].rearrange("p (a b) -> p a b", b=128), [pb], [XgTb])
            for ex in range(8):
                for fc in range(6):
                    k = wtc % 2
                    wtc += 1
                    x_dma(S_, "pool", Wt[k][:], I["wgu"][ex * 6 + fc], [], [Wtb[k]])
                    pg, pgb = getf()
                    for kc in range(32):
                        x_mm(S_, pg[:, :], Wt[k][:, kc, :], XgT[:, kc, :], kc == 0, kc == 31, [Wtb[k], XgTb], [pgb])
                    pu, pub = getf()
                    for kc in range(32):
                        x_mm(S_, pu[:, :], Wt[k][:, 32 + kc, :], XgT[:, kc, :], kc == 0, kc == 31, [Wtb[k], XgTb], [pub])
                    x_act(S_, sg[0][:], pg[:, :], AF.Sigmoid, [pgb], [sgb[0]])
                    x_tt(S_, "dve", sg[1][:], pg[:, :], sg[0][:], ALU.mult, [pgb, sgb[0]], [sgb[1]])
                    x_tt(S_, "dve", aT[:, fc, :], pu[:, :], sg[1][:], ALU.mult, [pub, sgb[1]], [aTb])
                for ct in range(8):
                    k = wdc % 2
                    wdc += 1
                    x_dma(S_, "pool", Wd[k][:], I["wdn"][ex * 8 + ct], [], [Wdb[k]])
                    for ts in range(4):
                        po, pob = getf()
                        for fc in range(6):
                            x_mm(S_, po[:, :], aT[:, fc, ts * 128:(ts + 1) * 128], Wd[k][:, fc, :], fc == 0, fc == 5, [aTb, Wdb[k]], [pob])
                        x_stt(S_, "dve", acc[:, ts, ct * 512:(ct + 1) * 512], po[:, :], acc[:, ts, D + 1 + ex:D + 2 + ex],
                              acc[:, ts, ct * 512:(ct + 1) * 512], ALU.mult, ALU.add, [pob, accb[ts]], [accb[ts]])
            x_dma(S_, "sp", nw[:], I["nwf"], [], [nwb])
            for ts in range(4):
                blk = tt * 4 + ts
                x_act(S_, xn[:], acc[:, ts, 0:D], AF.Square, [accb[ts]], [xnb, cb_], accum_out=s_(1))
                x_rstd(S_, C, s_(2), s_(1), D, [cb_], [cb_])
                x_stt(S_, "dve", acc[:, ts, 0:D], acc[:, ts, 0:D], s_(2), nw[:], ALU.mult, ALU.mult, [accb[ts], cb_, nwb], [accb[ts]])
                x_dma(S_, "sp", C.orow[blk * 128:(blk + 1) * 128, :], acc[:, ts, 0:D], [accb[ts]], [C.dramb])
        S_.barrier()
        S_.emit()


def build_full(upto=3, debug=False, mode="fused"):
    nc = bass.Bass("TRN2", target_bir_lowering=False)
    I = {}

    def inp(name, shape, dt=F32):
        I[name] = nc.dram_tensor(name, list(shape), dt, kind="ExternalInput").ap()

    fused = mode == "fused"
    do1 = fused or mode == "L1"
    do2 = (fused and upto >= 2) or mode == "L2"
    do3 = (fused and upto >= 3) or mode == "L3"
    inp("ident", [128, 128])
    if do1:
        inp("maskUT", [128, 128]); inp("rst", [128, 512])
        inp("xb", [S, D]); inp("w1", [16, 128, 32, 256]); inp("wg", [128, 32, 4])
        inp("cw", [128, 8, 4]); inp("cb", [128, 8]); inp("lb", [128, 4, 2]); inp("gb", [1, 8])
        inp("mnw", [128, 512]); inp("hnw", [128, 512])
    if do1 or do2:
        inp("nw1", [128, D])
    if do2:
        inp("xt", [1024, D]); inp("gidx", [128, 8, 4], I32); inp("w2", [32, 128, 96, 128]); inp("w3", [16, 128, 32, 256])
        inp("wr", [128, 32, 72]); inp("rb", [128, 72]); inp("io8", [128, 8])
    if do2 or do3:
        inp("nw2", [128, D])
    if do3:
        inp("gid", [128, 1]); inp("Ls", [128, 128]); inp("iotar", [128, CAP]); inp("tokhl", [128, 64, 4])
        inp("wgu", [48, 128, 64, 128]); inp("wdn", [64, 128, 6, 512]); inp("nwf", [128, D])
    C = Ctx()
    dk = lambda lvl: "ExternalOutput" if (debug and upto == lvl) else "Internal"
    C.dramb = Buf("dram")
    if do1:
        C.hT1 = nc.dram_tensor("hT1", [8, 128, 32 * 512], BF16).ap()
        C.cc1_in = nc.dram_tensor("cc1_in", [S, 1024], BF16, kind="ExternalOutput" if mode == "L1" else dk(1)).ap()
    if do2:
        if fused:
            C.cc1_out = nc.dram_tensor("cc1_out", [NCORE * S, 1024], BF16).ap()
        else:
            C.cc1_out = nc.dram_tensor("cc1_out", [4 * S, 1024], BF16, kind="ExternalInput").ap()
        C.hT2 = nc.dram_tensor("hT2", [2, 128, 32 * 512], BF16).ap()
        C.yT = nc.dram_tensor("yT", [2, 128, 32 * 512], BF16).ap()
        C.cc2_in = nc.dram_tensor("cc2_in", [1024, XW], F32, kind="ExternalOutput" if mode == "L2" else dk(2)).ap()
    if do3:
        if fused:
            C.cc2_out = nc.dram_tensor("cc2_out", [NCORE * 1024, XW], F32).ap()
        else:
            C.cc2_out = nc.dram_tensor("cc2_out", [NCORE * 1024, XW], F32, kind="ExternalInput").ap()
        C.oidx = nc.dram_tensor("oidx", [128, CAP // 128], I32, kind="ExternalOutput").ap()
        C.orow = nc.dram_tensor("orow", [CAP, D], F32, kind="ExternalOutput").ap()
    with contextlib.ExitStack() as stack:
        S_ = Sched(nc, stack)
        C.getf, C.getb = _psum_pool(nc, stack, S_)
        C.ident = _sb(nc, stack, "ident", [128, 128], BF16)
        C.identb = Buf("ident")
        S_.dma("pool", lambda e: e.dma_start(out=C.ident[:], in_=I["ident"]), writes=[C.identb])
        C.epsc = _sb(nc, stack, "epsc", [128, 2], F32)
        S_.op("dve", lambda e: e.memset(C.epsc[:], EPS), writes=[C.identb])
        if do1:
            stage_norm_T(nc, S_, C, I["xb"], I["nw1"], C.hT1, S)
            stage_mixer(nc, S_, C, I)
        if do2:
            if fused:
                S_.barrier()
                for k in range(S // CH1):
                    coll(S_, nc, "AllGather", [list(range(NCORE))], C.cc1_in[k * CH1:(k + 1) * CH1, :],
                         C.cc1_out[k * NCORE * CH1:(k + 1) * NCORE * CH1, :], [C.dramb], [C.dramb])
            stage_norm_T(nc, S_, C, I["xt"], I["nw1"], C.hT2, 1024)
            import os as _os
            lim = int(_os.environ.get("K_LIM", "9"))
            if lim >= 1:
                stage_branch(nc, S_, C, I)
            if lim >= 2:
                stage_outproj(nc, S_, C, I)
        if do3:
            if fused:
                S_.barrier()
                for k in range(1024 // CH2):
                    coll(S_, nc, "AllGather", [list(range(NCORE))], C.cc2_in[k * CH2:(k + 1) * CH2, :],
                         C.cc2_out[k * NCORE * CH2:(k + 1) * NCORE * CH2, :], [C.dramb], [C.dramb])
            stage_moe(nc, S_, C, I)
        S_.barrier()
        S_.emit()
    return nc


def prep_p2(c, inputs, shared, fused=False):
    seg = c % 4
    gidx = np.zeros((128, 8, 4), np.int32)
    for sub in range(8):
        for jp in range(4):
            t = seg * 1024 + sub * 128 + np.arange(128)
            if fused:
                rank = (c // 4) * 4 + jp
                gidx[:, sub, jp] = (t // CH1) * (NCORE * CH1) + rank * CH1 + (t % CH1)
            else:
                gidx[:, sub, jp] = jp * S + t
    xt = inputs["x"].reshape(-1, D)[c * 1024:(c + 1) * 1024]
    d = {"xt": np.ascontiguousarray(xt, dtype=np.float32), "gidx": gidx}
    d.update(shared)
    return d


def prep_p2_shared(inputs):
    w_in = inputs["w_in"][0]
    g0 = 8 * 2048 + 16
    wbm = inputs["w_branch_m"][0]
    wbh = inputs["w_branch_h"][0]
    w2 = np.empty((32, 128, 96, 128), np.float32)
    for dcn in range(32):
        cs = slice(dcn * 128, (dcn + 1) * 128)
        w2[dcn, :, 0:16, :] = wbm[:, cs].reshape(16, 128, 128).transpose(1, 0, 2)
        w2[dcn, :, 16:32, :] = wbh[:, cs].reshape(16, 128, 128).transpose(1, 0, 2)
        w2[dcn, :, 32:64, :] = w_in[:, g0 + dcn * 128:g0 + (dcn + 1) * 128].reshape(32, 128, 128).transpose(1, 0, 2)
        w2[dcn, :, 64:96, :] = w_in[:, g0 + D + dcn * 128:g0 + D + (dcn + 1) * 128].reshape(32, 128, 128).transpose(1, 0, 2)
    wo = inputs["w_out"][0]
    w3 = np.ascontiguousarray(wo.reshape(32, 128, 16, 256).transpose(2, 1, 0, 3))
    wrc = np.concatenate([inputs["router_group_w"][0], inputs["router_expert_w"][0]], 1)
    wr = _wtile(wrc)
    rbv = np.concatenate([inputs["router_group_b"][0], inputs["router_expert_b"][0]])
    return {"w2": w2, "w3": w3, "nw2": np.ascontiguousarray(np.broadcast_to(inputs["norm_ffn_w"][0], (128, D)), dtype=np.float32),
            "wr": np.ascontiguousarray(wr, dtype=np.float32), "rb": np.ascontiguousarray(np.broadcast_to(rbv, (128, 72)), dtype=np.float32),
            "io8": np.ascontiguousarray(np.broadcast_to(np.arange(8, dtype=np.float32), (128, 8)))}


def _tokhl():
    t = np.zeros((128, 64, 4), np.float32)
    t[:, :, 0] = np.arange(128)[:, None]
    t[:, :, 1] = np.arange(64)[None, :]
    t[:, :, 2] = 1.0
    return t


def prep_p3(c, inputs):
    wg_, wu_, wd_ = inputs["w_gate"][0], inputs["w_up"][0], inputs["w_down"][0]
    wgu = np.empty((48, 128, 64, 128), np.float32)
    wdn = np.empty((64, 128, 6, 512), np.float32)
    for ex in range(8):
        e = c * 8 + ex
        a = np.asarray(wg_[e]).reshape(32, 128, 6, 128)
        b = np.asarray(wu_[e]).reshape(32, 128, 6, 128)
        wgu[ex * 6:(ex + 1) * 6, :, 0:32, :] = a.transpose(2, 1, 0, 3)
        wgu[ex * 6:(ex + 1) * 6, :, 32:64, :] = b.transpose(2, 1, 0, 3)
        dd = np.asarray(wd_[e]).reshape(6, 128, 8, 512)
        wdn[ex * 8:(ex + 1) * 8] = dd.transpose(2, 1, 0, 3)
    s = np.arange(128)
    return {"gid": np.full((128, 1), float(c), np.float32), "Ls": (s[:, None] < s[None, :]).astype(np.float32),
            "iotar": np.ascontiguousarray(np.broadcast_to(np.arange(CAP, dtype=np.float32), (128, CAP))), "tokhl": _tokhl(), "wgu": wgu, "wdn": wdn,
            "nwf": np.ascontiguousarray(np.broadcast_to(inputs["norm_final_w"], (128, D)), dtype=np.float32)}


_NC_CACHE = {}


def _get_nc(mode):
    if mode not in _NC_CACHE:
        _NC_CACHE[mode] = build_full(3, mode=mode)
    return _NC_CACHE[mode]


def _pick(d, nc_inputs):
    return {k: d[k] for k in nc_inputs}


FUSED = False


def _kernel_fused(inputs):
    com = _prep_common()
    cores = list(range(NCORE))
    shared = prep_p2_shared(inputs)
    maps = []
    for c in cores:
        d = prep_p1(c, inputs)
        d.update(com)
        d.update(prep_p2(c, inputs, shared, fused=True))
        d.update(prep_p3(c, inputs))
        maps.append(d)
    res = run_bass_kernel_spmd(_get_nc("fused"), maps, core_ids=cores).results
    out = np.zeros((NCORE * 1024, D), np.float32)
    for c in cores:
        idx = np.asarray(res[c]["oidx"]).T.reshape(-1)
        rows = np.asarray(res[c]["orow"])
        ok = (idx >= 0) & (idx < NCORE * 1024)
        f = idx[ok]
        tok = ((f % (NCORE * CH2)) // CH2) * 1024 + (f // (NCORE * CH2)) * CH2 + (f % CH2)
        out[tok] = rows[ok]
    return out.reshape(2, S, D)


def kernel(**inputs):
    inputs = {k: np.asarray(v) for k, v in inputs.items()}
    if FUSED:
        return _kernel_fused(inputs)
    com = _prep_common()
    cores = list(range(NCORE))
    names1 = ["ident", "maskUT", "rst", "xb", "w1", "wg", "cw", "cb", "lb", "gb", "mnw", "hnw", "nw1"]
    maps = []
    for c in cores:
        d = prep_p1(c, inputs)
        d.update(com)
        maps.append(_pick(d, names1))
    r1 = run_bass_kernel_spmd(_get_nc("L1"), maps, core_ids=cores).results
    ho = [np.asarray(r["cc1_in"]) for r in r1]
    del maps
    shared = prep_p2_shared(inputs)
    nw1 = np.ascontiguousarray(np.broadcast_to(inputs["norm_mix_w"][0], (128, D)), dtype=np.float32)
    names2 = ["ident", "nw1", "xt", "gidx", "w2", "w3", "wr", "rb", "io8", "nw2", "cc1_out"]
    gath = [np.concatenate(ho[0:4], 0), np.concatenate(ho[4:8], 0)]
    maps = []
    for c in cores:
        d = prep_p2(c, inputs, shared)
        d.update({"ident": com["ident"], "nw1": nw1, "cc1_out": gath[c // 4]})
        maps.append(_pick(d, names2))
    r2 = run_bass_kernel_spmd(_get_nc("L2"), maps, core_ids=cores).results
    x2all = np.concatenate([np.asarray(r["cc2_in"]) for r in r2], 0)
    del maps
    names3 = ["ident", "nw2", "gid", "Ls", "iotar", "tokhl", "wgu", "wdn", "nwf", "cc2_out"]
    maps = []
    for c in cores:
        d = prep_p3(c, inputs)
        d.update({"ident": com["ident"], "nw2": shared["nw2"], "cc2_out": x2all})
        maps.append(_pick(d, names3))
    r3 = run_bass_kernel_spmd(_get_nc("L3"), maps, core_ids=cores).results
    out = np.zeros((NCORE * 1024, D), np.float32)
    for c in cores:
        idx = np.asarray(r3[c]["oidx"]).T.reshape(-1)
        rows = np.asarray(r3[c]["orow"])
        ok = (idx >= 0) & (idx < NCORE * 1024)
        out[idx[ok]] = rows[ok]
    return out.reshape(2, S, D)
```

```python
import contextlib
import numpy as np
import concourse.bass as bass
import concourse.mybir as mybir
from concourse.bass_utils import run_bass_kernel_spmd

F32, BF16, I32 = mybir.dt.float32, mybir.dt.bfloat16, mybir.dt.int32
AF = mybir.ActivationFunctionType
ALU = mybir.AluOpType
AX = mybir.AxisListType

D = 4096
S = 4096
NCORE = 8
EPS = 1e-6
CAP = 1536
XW = D + 16
ENGS = ("pe", "act", "dve", "pool", "sp")
ROT = 30000
NDMA = 6
CH1 = 256
CH2 = 32


class Buf:
    __slots__ = ("name", "w", "r")

    def __init__(self, name=""):
        self.name = name
        self.w = None
        self.r = {}


class Sched:
    def __init__(self, nc, stack):
        self.nc = nc
        self.stack = stack
        self.streams = {e: [] for e in ENGS}
        self.cnt = {e: 0 for e in ENGS}
        self.prog = {e: [] for e in ENGS}
        self.seen = {e: {} for e in ENGS}
        self.dma_sems = {e: [] for e in ENGS}
        self.dma_val = {}
        self.dma_rr = {e: 0 for e in ENGS}
        self.nsem = 0

    def new_sem(self, name):
        self.nsem += 1
        return self.stack.enter_context(self.nc.semaphore(name))

    def _prog_event(self, e):
        idx = self.cnt[e]
        self.cnt[e] += 1
        r = idx // ROT
        while len(self.prog[e]) <= r:
            self.prog[e].append(self.new_sem("p_%s_%d" % (e, len(self.prog[e]))))
        return (self.prog[e][r], idx % ROT + 1, e)

    def _waits(self, e, deps):
        best = {}
        for (sem, val, src) in deps:
            if src == "pe" and e == "pe":
                continue
            if self.seen[e].get(sem, 0) >= val:
                continue
            if sem not in best or best[sem] < val:
                best[sem] = val
        out = []
        for sem, val in best.items():
            self.seen[e][sem] = val
            out.append((sem, val))
        return out

    def _deps(self, reads, writes):
        deps = []
        for b in reads:
            if b.w is not None:
                deps.append(b.w)
        for b in writes:
            if b.w is not None:
                deps.append(b.w)
            deps.extend(b.r.values())
        return deps

    def _mark(self, ev, reads, writes):
        for b in reads:
            old = b.r.get(ev[0])
            if old is None or old[1] < ev[1]:
                b.r[ev[0]] = ev
        for b in writes:
            b.w = ev
            b.r = {}

    def op(self, e, fn, reads=(), writes=()):
        waits = self._waits(e, self._deps(reads, writes))
        ev = self._prog_event(e)
        self.streams[e].append((waits, fn, ev[0], 1))
        self._mark(ev, reads, writes)
        return ev

    def dma(self, q, fn, reads=(), writes=(), inc=16):
        deps = self._deps(reads, writes)
        sems = self.dma_sems[q]
        i = self.dma_rr[q] % NDMA
        self.dma_rr[q] += 1
        if len(sems) <= i:
            s = self.new_sem("d_%s_%d" % (q, i))
            sems.append(s)
            self.dma_val[s] = 0
        sem = sems[i]
        prev = self.dma_val[sem]
        if prev > 0:
            deps.append((sem, prev, "dma"))
        val = prev + inc
        self.dma_val[sem] = val
        waits = self._waits(q, deps)
        ev = (sem, val, "dma")
        self.streams[q].append((waits, fn, sem, inc))
        self._mark(ev, reads, writes)
        return ev

    def barrier(self):
        evs = []
        for e in ENGS:
            c = self.cnt[e]
            if c > 0:
                r = (c - 1) // ROT
                evs.append((self.prog[e][r], (c - 1) % ROT + 1, "x"))
        for s, v in self.dma_val.items():
            if v > 0:
                evs.append((s, v, "dma"))
        if getattr(self, "cc_val", 0) > 0:
            evs.append((self.cc_sem, self.cc_val, "dma"))
        for e in ENGS:
            waits = self._waits(e, evs)
            if waits:
                self.streams[e].append((waits, None, None, 0))

    def emit(self):
        nc = self.nc
        streams = self.streams
        self.streams = {e: [] for e in ENGS}

        def run(eng, lst):
            for (waits, fn, sem, inc) in lst:
                for (s, v) in waits:
                    eng.wait_ge(s, v)
                if fn is not None:
                    fn(eng).then_inc(sem, inc)

        with nc.Block() as block:
            block.tensor(lambda eng: run(eng, streams["pe"]))
            block.scalar(lambda eng: run(eng, streams["act"]))
            block.vector(lambda eng: run(eng, streams["dve"]))
            block.gpsimd(lambda eng: run(eng, streams["pool"]))
            block.sync(lambda eng: run(eng, streams["sp"]))


class Ctx:
    pass


def _psum_pool(nc, stack, S_):
    banks = []
    for i in range(6):
        t = stack.enter_context(nc.psum_tensor("psf%d" % i, [128, 512], F32))
        banks.append((t, Buf("psf%d" % i)))
    bb = []
    for i in range(2):
        t = stack.enter_context(nc.psum_tensor("psb%d" % i, [128, 1024], BF16))
        bb.append((t, Buf("psb%d" % i)))
    st = {"f": 0, "b": 0}

    def getf():
        st["f"] += 1
        return banks[st["f"] % 6]

    def getb():
        st["b"] += 1
        return bb[st["b"] % 2]

    return getf, getb


_SBN = [0]


def _sb(nc, stack, name, shape, dt):
    _SBN[0] += 1
    return stack.enter_context(nc.sbuf_tensor("s%d_%s" % (_SBN[0], name), list(shape), dt))


def stage_norm_T(nc, S_, C, src, nw_dram, dst, ntok):
    with contextlib.ExitStack() as st:
        getf, getb = C.getf, C.getb
        xs = [_sb(nc, st, "n_xs%d" % i, [128, D], F32) for i in range(2)]
        xsb = [Buf() for _ in range(2)]
        xn = [_sb(nc, st, "n_xn%d" % i, [128, D], BF16) for i in range(2)]
        xnb = [Buf() for _ in range(2)]
        nw = _sb(nc, st, "n_nw", [128, D], F32)
        nwb = Buf()
        hT = [_sb(nc, st, "n_hT%d" % i, [128, 32, 512], BF16) for i in range(2)]
        hTb = [Buf() for _ in range(2)]
        ss = _sb(nc, st, "n_ss", [128, 8], F32)
        ssb = [Buf() for _ in range(4)]
        S_.barrier()
        S_.dma("sp", lambda e: e.dma_start(out=nw[:], in_=nw_dram), writes=[nwb])
        def front(ti):
            tt, ts = ti // 4, ti % 4
            k = ti % 2
            S_.dma("sp", lambda e, k=k, ti=ti: e.dma_start(out=xs[k][:], in_=src[ti * 128:(ti + 1) * 128, :]),
                   writes=[xsb[k]])
            sb_ = ssb[ti % 4]
            c0 = (ti % 4) * 2
            S_.op("act", lambda e, k=k, c0=c0: e.activation(out=xn[k][:], in_=xs[k][:], func=AF.Square,
                                                            accum_out=ss[:, c0:c0 + 1]),
                  reads=[xsb[k]], writes=[xnb[k], sb_])
            S_.op("act", lambda e, c0=c0: e.activation(out=ss[:, c0 + 1:c0 + 2], in_=ss[:, c0:c0 + 1], func=AF.Ln,
                                                       bias=C.epsc[:, 0:1], scale=1.0 / D), reads=[sb_, C.identb], writes=[sb_])
            S_.op("act", lambda e, c0=c0: e.activation(out=ss[:, c0 + 1:c0 + 2], in_=ss[:, c0 + 1:c0 + 2], func=AF.Exp,
                                                       scale=-0.5), reads=[sb_], writes=[sb_])
            S_.op("dve", lambda e, k=k, c0=c0: e.scalar_tensor_tensor(out=xn[k][:], in0=xs[k][:],
                                                                      scalar=ss[:, c0 + 1:c0 + 2], in1=nw[:],
                                                                      op0=ALU.mult, op1=ALU.mult),
                  reads=[xsb[k], sb_, nwb], writes=[xnb[k]])

        def back(ti):
            tt, ts = ti // 4, ti % 4
            k = ti % 2
            hb = (tt % 2)
            for g in range(4):
                pt, pb = getb()
                for q in range(8):
                    kc = g * 8 + q
                    S_.op("pe", lambda e, pt=pt, q=q, kc=kc, k=k: e.transpose(
                        out=pt[:, q * 128:(q + 1) * 128], in_=xn[k][:, kc * 128:(kc + 1) * 128], identity=C.ident[:]),
                        reads=[xnb[k], C.identb], writes=[pb])
                eng = "act" if g % 2 == 0 else "dve"
                if eng == "act":
                    S_.op("act", lambda e, pt=pt, g=g, hb=hb, ts=ts: e.copy(
                        out=hT[hb][:, g * 8:(g + 1) * 8, ts * 128:(ts + 1) * 128],
                        in_=pt[:, :].rearrange("p (a b) -> p a b", b=128)), reads=[pb], writes=[hTb[hb]])
                else:
                    S_.op("dve", lambda e, pt=pt, g=g, hb=hb, ts=ts: e.tensor_copy(
                        out=hT[hb][:, g * 8:(g + 1) * 8, ts * 128:(ts + 1) * 128],
                        in_=pt[:, :].rearrange("p (a b) -> p a b", b=128)), reads=[pb], writes=[hTb[hb]])
            if ts == 3:
                S_.dma("sp", lambda e, hb=hb, tt=tt: e.dma_start(
                    out=dst[tt, :, :], in_=hT[hb][:, :, :].rearrange("p a b -> p (a b)")),
                    reads=[hTb[hb]], writes=[C.dramb])

        nsub = ntok // 128
        front(0)
        for ti in range(nsub):
            if ti + 1 < nsub:
                front(ti + 1)
            back(ti)
        S_.barrier()
        S_.emit()


def stage_mixer(nc, S_, C, I, ntiles=8, dbg=None):
    with contextlib.ExitStack() as st:
        getf, getb = C.getf, C.getb
        sb = lambda name, shape, dt: _sb(nc, st, "m_" + name, shape, dt)
        hT = sb("hT", [128, 32, 512], BF16); hTb = Buf()
        W = [sb("W%d" % i, [128, 32, 256], BF16) for i in range(3)]; Wb = [Buf(), Buf(), Buf()]
        wg = sb("wg", [128, 32, 4], BF16); wgb = Buf()
        FA = [sb("FA%d" % i, [128, 2, 512], BF16) for i in range(2)]; FAb = [Buf(), Buf()]
        FB = [sb("FB%d" % i, [128, 2, 512], BF16) for i in range(2)]; FBb = [Buf(), Buf()]
        FBf = [sb("FBf%d" % i, [128, 2, 512], F32) for i in range(2)]
        TA = [sb("TA%d" % i, [128, 4, 258], BF16) for i in range(2)]; TAb = [Buf(), Buf()]
        TB = [sb("TB%d" % i, [128, 4, 256], BF16) for i in range(2)]; TBb = [Buf(), Buf()]
        hist = sb("hist", [128, 8, 4], F32); histb = [Buf() for _ in range(8)]
        pre = [sb("pre%d" % i, [128, 516], F32) for i in range(2)]; preb = [Buf(), Buf()]
        acc = [sb("acc%d" % i, [128, 512], F32) for i in range(2)]; accb = [Buf(), Buf()]
        sg = [sb("sg%d" % i, [128, 512], F32) for i in range(2)]; sgb = [Buf(), Buf()]
        ex = [sb("ex%d" % i, [128, 512], F32) for i in range(3)]; exb = [Buf() for _ in range(3)]
        eg = sb("eg", [128, 2, 8], F32); egb = Buf()
        qe0 = sb("qe0", [128, 2, 512], BF16); qe1 = sb("qe1", [128, 2, 512], BF16)
        ke = sb("ke", [128, 2, 512], BF16); kg = sb("kg", [128, 2, 512], BF16)
        qeb, keb, kgb = Buf(), Buf(), Buf()
        kgT = sb("kgT", [128, 4, 256], BF16); kgTb = Buf()
        AT = [sb("AT%d" % i, [128, 128], BF16) for i in range(2)]; ATb = [Buf(), Buf()]
        Cst = sb("Cst", [128, 8, 257], F32)
        Cb = sb("Cb", [128, 8, 2, 258], BF16)
        Cstb = [Buf() for _ in range(8)]; Cbb = [[Buf(), Buf()] for _ in range(8)]
        numS = [sb("numS%d" % i, [128, 257], F32) for i in range(2)]; numSb = [Buf(), Buf()]
        ytmp = [sb("ytmp%d" % i, [128, 256], F32) for i in range(2)]; ytmpb = [Buf(), Buf()]
        sm = sb("sm", [128, 16], F32); smb = [Buf(), Buf()]
        outst = [sb("outst%d" % i, [128, 4, 256], BF16) for i in range(2)]; outstb = [Buf(), Buf()]
        maskUT = sb("maskUT", [128, 128], F32); rst = sb("rst", [128, 512], F32)
        mnw = sb("mnw", [128, 512], F32); hnw = sb("hnw", [128, 512], F32)
        cw = sb("cw", [128, 8, 4], F32); cbias = sb("cbias", [128, 8], F32)
        lbt = sb("lbt", [128, 4, 4], F32)
        gbt = sb("gbt", [1, 8], F32)
        rows = sb("rows", [1, 6, 512], F32); rowsb = [Buf() for _ in range(6)]
        ones1 = sb("ones1", [1, 128], F32)
        constb = Buf()

        S_.barrier()
        S_.dma("sp", lambda e: e.dma_start(out=maskUT[:], in_=I["maskUT"]), writes=[constb])
        S_.dma("sp", lambda e: e.dma_start(out=rst[:], in_=I["rst"]), writes=[constb])
        S_.dma("sp", lambda e: e.dma_start(out=mnw[:], in_=I["mnw"]), writes=[constb])
        S_.dma("sp", lambda e: e.dma_start(out=hnw[:], in_=I["hnw"]), writes=[constb])
        S_.dma("sp", lambda e: e.dma_start(out=cw[:], in_=I["cw"]), writes=[constb])
        S_.dma("sp", lambda e: e.dma_start(out=cbias[:], in_=I["cb"]), writes=[constb])
        S_.dma("sp", lambda e: e.dma_start(out=lbt[:, :, 0:2], in_=I["lb"]), writes=[constb])
        S_.dma("sp", lambda e: e.dma_start(out=gbt[:], in_=I["gb"]), writes=[constb])
        S_.dma("pool", lambda e: e.dma_start(out=wg[:], in_=I["wg"]), writes=[wgb])
        S_.op("dve", lambda e: e.memset(ones1[:], 1.0), writes=[constb])
        S_.op("dve", lambda e: e.memset(hist[:], 0.0), writes=histb)
        S_.op("dve", lambda e: e.memset(Cst[:], 0.0), writes=Cstb)
        S_.op("dve", lambda e: e.memset(Cb[:], 0.0), writes=[b for p in Cbb for b in p])
        S_.op("dve", lambda e: e.memset(qe0[:], 0.0), writes=[qeb])
        S_.op("dve", lambda e: e.memset(qe1[:], 0.0), writes=[qeb])
        for i in range(2):
            S_.op("dve", lambda e, i=i: e.memset(TA[i][:], 1.0), writes=[TAb[i]])
        S_.op("dve", lambda e: e.tensor_tensor(out=lbt[:, :, 2], in0=lbt[:, :, 0], in1=lbt[:, :, 1], op=ALU.subtract),
              reads=[constb], writes=[constb])
        S_.op("act", lambda e: e.activation(out=lbt[:, :, 2], in_=lbt[:, :, 2], func=AF.Sigmoid),
              reads=[constb], writes=[constb])
        S_.op("dve", lambda e: e.tensor_scalar(out=lbt[:, :, 3], in0=lbt[:, :, 2], scalar1=-1.0, scalar2=1.0,
                                               op0=ALU.mult, op1=ALU.add), reads=[constb], writes=[constb])
        S_.op("dve", lambda e: e.tensor_scalar(out=gbt[:, 4:6], in0=gbt[:, 2:4], scalar1=-1.0, scalar2=None,
                                               op0=ALU.mult), reads=[constb], writes=[constb])

        wcount = [0]

        def load_w(t):
            k = wcount[0] % 3
            wcount[0] += 1
            S_.dma("pool", lambda e, k=k, t=t: e.dma_start(out=W[k][:], in_=I["w1"][t]), writes=[Wb[k]])
            return k

        cnt = {"acc": 0, "ex": 0, "num": 0, "AT": 0}

        def gla(tt, u, FAq, FBk, FBk_b, TAv, TAv_b, nd, dv, hsel, sidx, ebuf, eamb, egba, egap, post):
            qv = lambda t_: t_.rearrange("p (a c l) -> p a c l", c=2, l=64)
            for dc in range(nd):
                S_.op("dve", lambda e, dc=dc: e.tensor_tensor(out=qv(qe0[:, dc, :])[:, :, 0, :], in0=qv(FAq[:, dc, :])[:, :, 0, :],
                                                             in1=qv(ebuf)[:, :, 0, :], op=ALU.mult),
                      reads=[FAb[u], exb[0]], writes=[qeb])
                S_.op("dve", lambda e, dc=dc: e.tensor_tensor(out=qv(qe1[:, dc, :])[:, :, 1, :], in0=qv(FAq[:, dc, :])[:, :, 1, :],
                                                             in1=qv(ebuf)[:, :, 1, :], op=ALU.mult),
                      reads=[FAb[u], exb[0]], writes=[qeb])
                S_.op("dve", lambda e, dc=dc: e.tensor_tensor(out=ke[:, dc, :], in0=FBk[:, dc, :], in1=eamb, op=ALU.mult),
                      reads=[FBk_b, exb[1]], writes=[keb])
                S_.op("dve", lambda e, dc=dc: e.tensor_tensor(out=kg[:, dc, :], in0=FBk[:, dc, :], in1=egba, op=ALU.mult),
                      reads=[FBk_b, exb[2]], writes=[kgb])
            for ts in range(4):
                pt, pb = getb()
                for dc in range(nd):
                    S_.op("pe", lambda e, pt=pt, dc=dc, ts=ts: e.transpose(out=pt[:, dc * 128:(dc + 1) * 128],
                                                                          in_=kg[:, dc, ts * 128:(ts + 1) * 128],
                                                                          identity=C.ident[:]),
                          reads=[kgb, C.identb], writes=[pb])
                S_.op("act", lambda e, pt=pt, ts=ts: e.copy(out=kgT[:, ts, 0:nd * 128], in_=pt[:, 0:nd * 128]),
                      reads=[pb], writes=[kgTb])
            for ts in range(4):
                tok = slice(ts * 128, (ts + 1) * 128)
                pa, pab = getf()
                n_mm = 2 * nd
                i_mm = 0
                for dc in range(nd):
                    for qq in (qe0, qe1):
                        S_.op("pe", lambda e, pa=pa, dc=dc, qq=qq, i_mm=i_mm, tok=tok: e.matmul(
                            pa[:, 0:128], lhsT=ke[:, dc, tok], rhs=qq[:, dc, tok], start=(i_mm == 0), stop=(i_mm == n_mm - 1)),
                            reads=[keb, qeb], writes=[pab])
                        i_mm += 1
                ai = cnt["AT"] % 2
                cnt["AT"] += 1
                S_.op("dve", lambda e, pa=pa, ai=ai: e.tensor_tensor(out=AT[ai][:], in0=pa[:, 0:128], in1=maskUT[:], op=ALU.mult),
                      reads=[pab, constb], writes=[ATb[ai]])
                v = TAv(ts)
                for c in range(2):
                    rowsl = slice(c * 64, (c + 1) * 64)
                    if c == 1:
                        po, pob = getf()
                        S_.op("pe", lambda e, po=po, ai=ai, v=v: e.matmul(po[:, 0:dv], lhsT=AT[ai][:], rhs=v, start=True, stop=False),
                              reads=[ATb[ai], TAv_b], writes=[pob])
                        for dc in range(nd):
                            si = sidx[dc]
                            S_.op("pe", lambda e, po=po, dc=dc, si=si, tok=tok: e.matmul(po[:, 0:dv], lhsT=qe0[:, dc, tok], rhs=Cb[:, si, 0, 0:dv],
                                                                               start=False, stop=False),
                                  reads=[qeb, Cbb[si][0]], writes=[pob])
                            S_.op("pe", lambda e, po=po, dc=dc, si=si, tok=tok: e.matmul(po[:, 0:dv], lhsT=qe1[:, dc, tok], rhs=Cb[:, si, 1, 0:dv],
                                                                               start=False, stop=(dc == nd - 1)),
                                  reads=[qeb, Cbb[si][1]], writes=[pob])
                        post(ts, po, pob)
                    for dc in range(nd):
                        si = sidx[dc]
                        pu, pub = getf()
                        S_.op("pe", lambda e, pu=pu, dc=dc, rowsl=rowsl, v=v, ts=ts: e.matmul(
                            pu[:, 0:dv], lhsT=kgT[rowsl, ts, dc * 128:(dc + 1) * 128], rhs=v[rowsl, :], start=True, stop=True),
                            reads=[kgTb, TAv_b], writes=[pub])
                        ch = ts * 2 + c
                        S_.op("dve", lambda e, pu=pu, si=si, ch=ch, dc=dc: e.scalar_tensor_tensor(
                            out=Cst[:, si, 0:dv], in0=Cst[:, si, 0:dv], scalar=egap(dc)[:, ch:ch + 1], in1=pu[:, 0:dv],
                            op0=ALU.mult, op1=ALU.add), reads=[pub, egb, Cstb[si]], writes=[Cstb[si]])
                        tgt = 1 if c == 0 else 0
                        S_.op("act", lambda e, si=si, tgt=tgt: e.copy(out=Cb[:, si, tgt, 0:dv], in_=Cst[:, si, 0:dv]),
                              reads=[Cstb[si]], writes=[Cbb[si][tgt]])

        def small(i):
            return sm[:, i:i + 1]

        for tt in range(ntiles):
            S_.dma("sp", lambda e, tt=tt: e.dma_start(out=hT[:, :, :].rearrange("p a b -> p (a b)"), in_=C.hT1[tt, :, :]),
                   reads=[C.dramb], writes=[hTb])
            for gi in range(4):
                pg, pgb = getf()
                for kc in range(32):
                    S_.op("pe", lambda e, pg=pg, gi=gi, kc=kc: e.matmul(pg[0:1, :], lhsT=wg[:, kc, gi:gi + 1], rhs=hT[:, kc, :],
                                                                       start=(kc == 0), stop=(kc == 31)),
                          reads=[wgb, hTb], writes=[pgb])
                h = gi % 2
                if gi < 2:
                    S_.op("act", lambda e, pg=pg, h=h: e.activation(out=rows[:, h, :], in_=pg[0:1, :], func=AF.Identity,
                                                                    bias=gbt[:, h:h + 1], scale=1.0),
                          reads=[pgb, constb], writes=[rowsb[h]])
                else:
                    S_.op("act", lambda e, pg=pg, h=h: e.activation(out=rows[:, 4, :], in_=pg[0:1, :], func=AF.Exp,
                                                                    bias=gbt[:, 4 + h:5 + h], scale=-1.0),
                          reads=[pgb, constb], writes=[rowsb[4]])
                    S_.op("act", lambda e: e.activation(out=rows[:, 4, :], in_=rows[:, 4, :], func=AF.Ln, bias=1.0, scale=1.0),
                          reads=[rowsb[4]], writes=[rowsb[4]])
                    S_.op("dve", lambda e, h=h: e.tensor_tensor_scan(out=rows[:, 2 + h, :], data0=rst[0:1, :], data1=rows[:, 4, :],
                                                                     initial=0.0, op0=ALU.mult, op1=ALU.subtract),
                          reads=[rowsb[4], constb], writes=[rowsb[2 + h]])
            def inproj(u):
                ub = u % 2
                mls = u < 2
                for part in range(4):
                    k = load_w(u * 4 + part)
                    if part < 2:
                        for cc in range(2):
                            pp, ppb = getf()
                            for kc in range(32):
                                S_.op("pe", lambda e, pp=pp, k=k, cc=cc, kc=kc: e.matmul(
                                    pp[:, :], lhsT=W[k][:, kc, cc * 128:(cc + 1) * 128], rhs=hT[:, kc, :],
                                    start=(kc == 0), stop=(kc == 31)), reads=[Wb[k], hTb], writes=[ppb])
                            if mls:
                                ci = u * 4 + part * 2 + cc
                                cwi = part * 4 + u * 2 + cc
                                pk = cnt["acc"] % 2
                                cnt["acc"] += 1
                                S_.op("act", lambda e, pp=pp, pk=pk: e.copy(out=pre[pk][:, 3:515], in_=pp[:, :]),
                                      reads=[ppb], writes=[preb[pk]])
                                S_.op("dve", lambda e, pk=pk, cwi=cwi: e.tensor_copy(out=pre[pk][:, 0:3], in_=hist[:, cwi, 0:3]),
                                      reads=[histb[cwi]], writes=[preb[pk]])
                                S_.op("dve", lambda e, pk=pk, cwi=cwi: e.tensor_scalar(
                                    out=acc[pk][:], in0=pre[pk][:, 3:515], scalar1=cw[:, cwi, 3:4], scalar2=cbias[:, cwi:cwi + 1],
                                    op0=ALU.mult, op1=ALU.add), reads=[preb[pk], constb], writes=[accb[pk]])
                                for tap in range(3):
                                    S_.op("dve", lambda e, pk=pk, cwi=cwi, tap=tap: e.scalar_tensor_tensor(
                                        out=acc[pk][:], in0=pre[pk][:, tap:tap + 512], scalar=cw[:, cwi, tap:tap + 1], in1=acc[pk][:],
                                        op0=ALU.mult, op1=ALU.add), reads=[preb[pk], constb, accb[pk]], writes=[accb[pk]])
                                S_.op("dve", lambda e, pk=pk, cwi=cwi: e.tensor_copy(out=hist[:, cwi, 0:3], in_=pre[pk][:, 512:515]),
                                      reads=[preb[pk]], writes=[histb[cwi]])
                                S_.op("act", lambda e, pk=pk: e.activation(out=sg[pk][:], in_=acc[pk][:], func=AF.Sigmoid),
                                      reads=[accb[pk]], writes=[sgb[pk]])
                                dstt = FA[ub] if part == 0 else FB[ub]
                                dstb = FAb[ub] if part == 0 else FBb[ub]
                                cmul = 1.0 if part == 0 else 1.0 / 16.0
                                S_.op("dve", lambda e, pk=pk, dstt=dstt, cc=cc, cmul=cmul: e.scalar_tensor_tensor(
                                    out=dstt[:, cc, :], in0=acc[pk][:], scalar=cmul, in1=sg[pk][:], op0=ALU.mult, op1=ALU.mult),
                                    reads=[accb[pk], sgb[pk]], writes=[dstb])
                            else:
                                hh = (u - 2) * 2 + cc
                                pk = cnt["acc"] % 2
                                cnt["acc"] += 1
                                if part == 0:
                                    S_.op("act", lambda e, pp=pp, pk=pk: e.activation(out=sg[pk][:], in_=pp[:, :], func=AF.Sigmoid),
                                          reads=[ppb], writes=[sgb[pk]])
                                    S_.op("dve", lambda e, pp=pp, pk=pk, cc=cc, ub=ub: e.tensor_tensor(
                                        out=FA[ub][:, cc, :], in0=pp[:, :], in1=sg[pk][:], op=ALU.mult),
                                        reads=[ppb, sgb[pk]], writes=[FAb[ub]])
                                else:
                                    S_.op("act", lambda e, pp=pp, pk=pk: e.activation(out=sg[pk][:], in_=pp[:, :], func=AF.Sigmoid),
                                          reads=[ppb], writes=[sgb[pk]])
                                    S_.op("dve", lambda e, pk=pk, hh=hh: e.tensor_scalar(
                                        out=acc[pk][:], in0=sg[pk][:], scalar1=lbt[:, hh, 3:4], scalar2=lbt[:, hh, 2:3],
                                        op0=ALU.mult, op1=ALU.add), reads=[sgb[pk], constb], writes=[accb[pk]])
                                    S_.op("dve", lambda e, pk=pk, cc=cc, ub=ub: e.tensor_scalar(
                                        out=FB[ub][:, cc, :], in0=acc[pk][:], scalar1=-1.0, scalar2=1.0, op0=ALU.mult, op1=ALU.add),
                                        reads=[accb[pk]], writes=[FBb[ub]])
                                    S_.op("act", lambda e, pk=pk: e.activation(out=sg[pk][:], in_=acc[pk][:], func=AF.Ln),
                                          reads=[accb[pk]], writes=[sgb[pk]])
                                    S_.op("dve", lambda e, pk=pk, cc=cc, ub=ub: e.tensor_tensor_scan(
                                        out=FBf[ub][:, cc, :], data0=rst[:], data1=sg[pk][:], initial=0.0, op0=ALU.mult, op1=ALU.add),
                                        reads=[sgb[pk], constb], writes=[FBb[ub]])
                    else:
                        for ts in range(4):
                            pp, ppb = getf()
                            for kc in range(32):
                                S_.op("pe", lambda e, pp=pp, k=k, ts=ts, kc=kc: e.matmul(
                                    pp[:, 0:256], lhsT=hT[:, kc, ts * 128:(ts + 1) * 128], rhs=W[k][:, kc, :],
                                    start=(kc == 0), stop=(kc == 31)), reads=[Wb[k], hTb], writes=[ppb])
                            if part == 2:
                                S_.op("act", lambda e, pp=pp, ts=ts, ub=ub: e.copy(out=TA[ub][:, ts, 0:256], in_=pp[:, 0:256]),
                                      reads=[ppb], writes=[TAb[ub]])
                            else:
                                S_.op("act", lambda e, pp=pp, ts=ts, ub=ub: e.activation(out=TB[ub][:, ts, :], in_=pp[:, 0:256], func=AF.Sigmoid),
                                      reads=[ppb], writes=[TBb[ub]])
            def recur(u):
                ub = u % 2
                mls = u < 2
                ob = outstb[ub]
                if mls:
                    h = u
                    S_.op("dve", lambda e, h=h: e.tensor_tensor(out=rows[:, 5, :], in0=rows[:, h, :], in1=rows[:, 2 + h, :], op=ALU.subtract),
                          reads=[rowsb[h], rowsb[2 + h]], writes=[rowsb[5]])
                    r3 = lambda ap: ap.rearrange("p (c l) -> p c l", l=64)
                    S_.op("dve", lambda e, h=h: e.tensor_tensor(
                        out=r3(rows[:, 4, :]), in0=r3(rows[:, 5, :]), in1=r3(rows[:, 2 + h, :])[:, :, 63:64].to_broadcast([1, 8, 64]),
                        op=ALU.add), reads=[rowsb[5], rowsb[2 + h]], writes=[rowsb[4]])
                    for (ri, xi, rb_) in ((2 + h, 0, rowsb[2 + h]), (5, 1, rowsb[5]), (4, 2, rowsb[4])):
                        pbc, pbcb = getf()
                        S_.op("pe", lambda e, pbc=pbc, ri=ri: e.matmul(pbc[:, :], lhsT=ones1[:, :], rhs=rows[:, ri, :], start=True, stop=True),
                              reads=[rb_, constb], writes=[pbcb])
                        S_.op("act", lambda e, pbc=pbc, xi=xi: e.activation(out=ex[xi][:], in_=pbc[:, :], func=AF.Exp),
                              reads=[pbcb], writes=[exb[xi]])
                    S_.op("dve", lambda e: e.tensor_copy(out=eg[:, 0, :], in_=ex[0][:].rearrange("p (c l) -> p c l", l=64)[:, :, 63]),
                          reads=[exb[0]], writes=[egb])

                    def post_m(ts, po, pob, h=h, ub=ub, ob=ob):
                        ni = cnt["num"] % 2
                        cnt["num"] += 1
                        o0 = ni * 8
                        S_.op("act", lambda e: e.copy(out=numS[ni][:], in_=po[:, 0:257]), reads=[pob], writes=[numSb[ni]])
                        S_.op("act", lambda e: e.activation(out=small(o0), in_=numS[ni][:, 256:257], func=AF.Abs),
                              reads=[numSb[ni]], writes=[smb[ni]])
                        S_.op("dve", lambda e: e.tensor_scalar(out=small(o0), in0=small(o0), scalar1=1.0, scalar2=None,
                                                               op0=ALU.max), reads=[smb[ni]], writes=[smb[ni]])
                        S_.op("dve", lambda e: e.reciprocal(out=small(o0 + 1), in_=small(o0)), reads=[smb[ni]], writes=[smb[ni]])
                        S_.op("act", lambda e: e.activation(out=ytmp[ni][:], in_=numS[ni][:, 0:256], func=AF.Square,
                                                            accum_out=small(o0 + 2)), reads=[numSb[ni]], writes=[ytmpb[ni], smb[ni]])
                        S_.op("dve", lambda e: e.tensor_tensor(out=small(o0 + 3), in0=small(o0 + 1), in1=small(o0 + 1), op=ALU.mult),
                              reads=[smb[ni]], writes=[smb[ni]])
                        S_.op("dve", lambda e: e.tensor_tensor(out=small(o0 + 3), in0=small(o0 + 3), in1=small(o0 + 2), op=ALU.mult),
                              reads=[smb[ni]], writes=[smb[ni]])
                        S_.op("act", lambda e: e.activation(out=small(o0 + 3), in_=small(o0 + 3), func=AF.Ln, bias=C.epsc[:, 0:1],
                                                            scale=1.0 / 256), reads=[smb[ni], C.identb], writes=[smb[ni]])
                        S_.op("act", lambda e: e.activation(out=small(o0 + 3), in_=small(o0 + 3), func=AF.Exp, scale=-0.5),
                              reads=[smb[ni]], writes=[smb[ni]])
                        S_.op("dve", lambda e: e.tensor_tensor(out=small(o0 + 3), in0=small(o0 + 3), in1=small(o0 + 1), op=ALU.mult),
                              reads=[smb[ni]], writes=[smb[ni]])
                        S_.op("dve", lambda e: e.scalar_tensor_tensor(out=ytmp[ni][:], in0=numS[ni][:, 0:256], scalar=small(o0 + 3),
                                                                      in1=mnw[:, h * 256:(h + 1) * 256], op0=ALU.mult, op1=ALU.mult),
                              reads=[numSb[ni], smb[ni], constb], writes=[ytmpb[ni]])
                        S_.op("dve", lambda e: e.tensor_tensor(out=outst[ub][:, ts, :], in0=ytmp[ni][:], in1=TB[ub][:, ts, :], op=ALU.mult),
                              reads=[ytmpb[ni], TBb[ub]], writes=[ob])

                    gla(tt, ub, FA[ub], FB[ub], FBb[ub], lambda ts: TA[ub][:, ts, 0:257], TAb[ub], 2, 257, h, [h * 2, h * 2 + 1],
                        ex[0][:], ex[1][:], ex[2][:], lambda dc: eg[:, 0, :], post_m)
                    col0 = h * 256
                else:
                    for cc in range(2):
                        hh = (u - 2) * 2 + cc
                        bsrc = FBf[ub][:, cc, :]
                        S_.op("act", lambda e, bsrc=bsrc: e.activation(out=ex[0][:], in_=bsrc, func=AF.Exp), reads=[FBb[ub]], writes=[exb[0]])
                        S_.op("dve", lambda e, bsrc=bsrc: e.tensor_scalar(out=ex[1][:], in0=bsrc, scalar1=-1.0, scalar2=80.0,
                                                                          op0=ALU.mult, op1=ALU.min), reads=[FBb[ub]], writes=[exb[1]])
                        S_.op("act", lambda e: e.activation(out=ex[1][:], in_=ex[1][:], func=AF.Exp), reads=[exb[1]], writes=[exb[1]])
                        r3 = lambda ap: ap.rearrange("p (c l) -> p c l", l=64)
                        S_.op("dve", lambda e, bsrc=bsrc: e.tensor_tensor(
                            out=r3(ex[2][:]), in0=r3(bsrc)[:, :, 63:64].to_broadcast([128, 8, 64]), in1=r3(bsrc), op=ALU.subtract),
                            reads=[FBb[ub]], writes=[exb[2]])
                        S_.op("act", lambda e: e.activation(out=ex[2][:], in_=ex[2][:], func=AF.Exp), reads=[exb[2]], writes=[exb[2]])
                        S_.op("dve", lambda e, cc=cc: e.tensor_copy(out=eg[:, cc, :], in_=r3(ex[0][:])[:, :, 63]),
                              reads=[exb[0]], writes=[egb])

                        def post_h(ts, po, pob, hh=hh, cc=cc, ub=ub, ob=ob):
                            ni = cnt["num"] % 2
                            cnt["num"] += 1
                            o0 = ni * 8
                            S_.op("act", lambda e: e.activation(out=ytmp[ni][:, 0:128], in_=po[:, 0:128], func=AF.Square,
                                                                accum_out=small(o0 + 2)), reads=[pob], writes=[ytmpb[ni], smb[ni]])
                            S_.op("act", lambda e: e.activation(out=small(o0 + 3), in_=small(o0 + 2), func=AF.Ln, bias=C.epsc[:, 0:1],
                                                                scale=1.0 / 128), reads=[smb[ni], C.identb], writes=[smb[ni]])
                            S_.op("act", lambda e: e.activation(out=small(o0 + 3), in_=small(o0 + 3), func=AF.Exp, scale=-0.5),
                                  reads=[smb[ni]], writes=[smb[ni]])
                            S_.op("dve", lambda e: e.scalar_tensor_tensor(out=ytmp[ni][:, 0:128], in0=po[:, 0:128], scalar=small(o0 + 3),
                                                                          in1=hnw[:, hh * 128:(hh + 1) * 128], op0=ALU.mult, op1=ALU.mult),
                                  reads=[pob, smb[ni], constb], writes=[ytmpb[ni]])
                            S_.op("dve", lambda e: e.tensor_tensor(out=outst[ub][:, ts, cc * 128:(cc + 1) * 128], in0=ytmp[ni][:, 0:128],
                                                                    in1=TB[ub][:, ts, cc * 128:(cc + 1) * 128], op=ALU.mult),
                                  reads=[ytmpb[ni], TBb[ub]], writes=[ob])

                        gla(tt, ub, FA[ub][:, cc:cc + 1, :], FB[ub][:, cc:cc + 1, :], FBb[ub],
                            lambda ts, cc=cc: TA[ub][:, ts, cc * 128:(cc + 1) * 128], TAb[ub], 1, 128, hh, [4 + hh],
                            ex[0][:], ex[1][:], ex[2][:], lambda dc, cc=cc: eg[:, cc, :], post_h)
                    col0 = 512 + (u - 2) * 256
                S_.dma("sp", lambda e, ub=ub, tt=tt, col0=col0: e.dma_start(
                    out=C.cc1_in[tt * 512:(tt + 1) * 512, col0:col0 + 256].rearrange("(a p) c -> p a c", p=128), in_=outst[ub][:]),
                    reads=[ob], writes=[C.dramb])

            inproj(0)
            for u in range(4):
                if u < 3:
                    inproj(u + 1)
                recur(u)
        if dbg is not None:
            S_.barrier()
            for nm, t in (("FA1", FA[1]), ("FB1", FB[1]), ("FBf1", FBf[1]), ("TA1", TA[1]), ("TB1", TB[1]), ("ex0", ex[0]), ("ex1", ex[1]),
                          ("ex2", ex[2]), ("kgT", kgT), ("Cst", Cst), ("qe0", qe0), ("qe1", qe1), ("ke", ke), ("kg", kg), ("hT", hT), ("W0", W[0]), ("W1", W[1])):
                shp = list(t.shape)
                d_ = nc.dram_tensor("dbg_" + nm, shp, t.dtype, kind="ExternalOutput").ap()
                S_.dma("sp", lambda e, d_=d_, t=t: e.dma_start(out=d_, in_=t[:]))
        S_.barrier()
        S_.emit()


def build(stages=("p1",), debug=False, ntiles=8):
    nc = bass.Bass("TRN2", target_bir_lowering=False)
    I = {}

    def inp(name, shape, dt=F32):
        I[name] = nc.dram_tensor(name, list(shape), dt, kind="ExternalInput").ap()

    inp("ident", [128, 128]); inp("maskUT", [128, 128]); inp("rst", [128, 512])
    inp("xb", [S, D]); inp("nw1", [128, D]); inp("w1", [16, 128, 32, 256]); inp("wg", [128, 32, 4])
    inp("cw", [128, 8, 4]); inp("cb", [128, 8]); inp("lb", [128, 4, 2]); inp("gb", [1, 8])
    inp("mnw", [128, 512]); inp("hnw", [128, 512])
    C = Ctx()
    dbg_kind = "ExternalOutput" if debug else "Internal"
    C.hT1_t = nc.dram_tensor("hT1", [8, 128, 32 * 512], BF16, kind=dbg_kind)
    C.hT1 = C.hT1_t.ap()
    C.cc1_t = nc.dram_tensor("cc1_in", [S, 1024], BF16, kind=dbg_kind)
    C.cc1_in = C.cc1_t.ap()
    C.dramb = Buf("dram")
    with contextlib.ExitStack() as stack:
        S_ = Sched(nc, stack)
        C.getf, C.getb = _psum_pool(nc, stack, S_)
        C.ident = _sb(nc, stack, "ident", [128, 128], BF16)
        C.identb = Buf("ident")
        S_.dma("pool", lambda e: e.dma_start(out=C.ident[:], in_=I["ident"]), writes=[C.identb])
        C.epsc = _sb(nc, stack, "epsc", [128, 2], F32)
        S_.op("dve", lambda e: e.memset(C.epsc[:], EPS), writes=[C.identb])
        stage_norm_T(nc, S_, C, I["xb"], I["nw1"], C.hT1, ntiles * 512)
        stage_mixer(nc, S_, C, I, ntiles, True if debug else None)
        if debug:
            fin = _sb(nc, stack, "fin", [128, 8], F32)
            S_.barrier()
            S_.emit()
    return nc


def _prep_common():
    ident = np.eye(128, dtype=np.float32)
    s = np.arange(128)
    maskUT = ((s[:, None] // 64 == s[None, :] // 64) & (s[:, None] <= s[None, :])).astype(np.float32)
    rst = np.ones((128, 512), np.float32)
    rst[:, ::64] = 0.0
    return {"ident": ident, "maskUT": maskUT, "rst": rst}


def _wtile(wcols):
    return np.ascontiguousarray(wcols.reshape(32, 128, wcols.shape[1]).transpose(1, 0, 2))


def prep_p1(c, inputs):
    b, j = c // 4, c % 4
    w_in = inputs["w_in"][0]
    MW = 2048
    off = {"qm": 0, "km": MW, "vm": 2 * MW, "om": 3 * MW, "ipre": 4 * MW, "fpre": 4 * MW + 8,
           "qh": 4 * MW + 16, "fh": 5 * MW + 16, "ih": 6 * MW + 16, "gh": 7 * MW + 16,
           "gm": 8 * MW + 16, "ghh": 8 * MW + 16 + D}
    tiles = []
    for u in range(2):
        hd = 2 * j + u
        for nm in ("qm", "km", "vm", "om"):
            tiles.append(_wtile(w_in[:, off[nm] + hd * 256: off[nm] + (hd + 1) * 256]))
    for u in range(2):
        h0 = 4 * j + 2 * u
        for nm in ("qh", "fh", "ih", "gh"):
            tiles.append(_wtile(w_in[:, off[nm] + h0 * 128: off[nm] + (h0 + 2) * 128]))
    w1 = np.stack(tiles, 0)
    gcols = np.stack([w_in[:, off["ipre"] + 2 * j], w_in[:, off["ipre"] + 2 * j + 1],
                      w_in[:, off["fpre"] + 2 * j], w_in[:, off["fpre"] + 2 * j + 1]], 1)
    wg = _wtile(gcols)
    conv_w = inputs["conv_qk_w"][0]
    conv_b = inputs["conv_qk_b"][0]
    cw = np.zeros((128, 8, 4), np.float32)
    cb = np.zeros((128, 8), np.float32)
    for part in range(2):
        for u in range(2):
            for cc in range(2):
                ch0 = part * 2048 + (2 * j + u) * 256 + cc * 128
                idx = part * 4 + u * 2 + cc
                cw[:, idx, :] = conv_w[:, ch0:ch0 + 128].T
                cb[:, idx] = conv_b[ch0:ch0 + 128]
    lbr = inputs["hgrn_lb"]
    lb = np.zeros((128, 4, 2), np.float32)
    for hh in range(4):
        ch0 = (4 * j + hh) * 128
        lb[:, hh, 0] = lbr[0, ch0:ch0 + 128]
        lb[:, hh, 1] = lbr[1, ch0:ch0 + 128]
    gbv = inputs["gate_if_b"][0]
    gb = np.zeros((1, 8), np.float32)
    gb[0, 0:2] = gbv[2 * j:2 * j + 2]
    gb[0, 2:4] = gbv[8 + 2 * j:8 + 2 * j + 2]
    mnw = np.broadcast_to(inputs["mlstm_norm_w"][0][j * 512:(j + 1) * 512], (128, 512))
    hnw = np.broadcast_to(inputs["hgrn_norm_w"][0][j * 512:(j + 1) * 512], (128, 512))
    d = {"xb": inputs["x"][b], "nw1": np.broadcast_to(inputs["norm_mix_w"][0], (128, D)), "w1": w1, "wg": wg,
         "cw": cw, "cb": cb, "lb": lb, "gb": gb, "mnw": mnw, "hnw": hnw}
    return {k: np.ascontiguousarray(v, dtype=np.float32) for k, v in d.items()}


def x_mm(S_, out, lhsT, rhs, start, stop, reads, writes):
    S_.op("pe", lambda e: e.matmul(out, lhsT=lhsT, rhs=rhs, start=start, stop=stop), reads, writes)


def x_tr(S_, C, out, in_, reads, writes):
    S_.op("pe", lambda e: e.transpose(out=out, in_=in_, identity=C.ident[:]), list(reads) + [C.identb], writes)


def x_act(S_, out, in_, func, reads, writes, **kw):
    S_.op("act", lambda e: e.activation(out=out, in_=in_, func=func, **kw), reads, writes)


def x_cp(S_, eng, out, in_, reads, writes):
    if eng == "act":
        S_.op("act", lambda e: e.copy(out=out, in_=in_), reads, writes)
    else:
        S_.op(eng, lambda e: e.tensor_copy(out=out, in_=in_), reads, writes)


def x_tt(S_, eng, out, in0, in1, op, reads, writes):
    S_.op(eng, lambda e: e.tensor_tensor(out=out, in0=in0, in1=in1, op=op), reads, writes)


def x_ts(S_, eng, out, in0, s1, s2, op0, op1, reads, writes):
    if op1 is None:
        S_.op(eng, lambda e: e.tensor_scalar(out=out, in0=in0, scalar1=s1, scalar2=None, op0=op0), reads, writes)
    else:
        S_.op(eng, lambda e: e.tensor_scalar(out=out, in0=in0, scalar1=s1, scalar2=s2, op0=op0, op1=op1), reads, writes)


def x_stt(S_, eng, out, in0, scalar, in1, op0, op1, reads, writes):
    S_.op(eng, lambda e: e.scalar_tensor_tensor(out=out, in0=in0, scalar=scalar, in1=in1, op0=op0, op1=op1), reads, writes)


def x_red(S_, out, in_, op, reads, writes):
    S_.op("dve", lambda e: e.tensor_reduce(out=out, in_=in_, axis=AX.X, op=op), reads, writes)


def x_dma(S_, q, out, in_, reads, writes):
    return S_.dma(q, lambda e: e.dma_start(out=out, in_=in_), reads, writes)


def x_rstd(S_, C, out, in_, n, reads, writes):
    x_act(S_, out, in_, AF.Ln, list(reads) + [C.identb], writes, bias=C.epsc[:, 0:1], scale=1.0 / n)
    x_act(S_, out, out, AF.Exp, writes, writes, scale=-0.5)


def coll(S_, nc, kind, groups, src, dst, reads, writes):
    if not hasattr(S_, "cc_sem"):
        S_.cc_sem = S_.new_sem("ccsem")
        S_.cc_val = 0
    sem = S_.cc_sem
    deps = S_._deps(reads, writes)
    if S_.cc_val > 0:
        deps.append((sem, S_.cc_val, "dma"))
    S_.cc_val += 1
    waits = S_._waits("pool", deps)
    ev = (sem, S_.cc_val, "dma")
    S_.streams["pool"].append((waits, lambda e: e.collective_compute(kind, ALU.bypass, replica_groups=groups, ins=[src], outs=[dst]), sem, 1))
    S_._mark(ev, reads, writes)


def stage_branch(nc, S_, C, I):
    with contextlib.ExitStack() as st:
        getf, getb = C.getf, C.getb
        sb = lambda name, shape, dt: _sb(nc, st, "b_" + name, shape, dt)
        hoT = sb("hoT", [128, 32, 512], BF16); hoTb = Buf()
        hT2 = sb("hT2", [128, 32, 512], BF16); hT2b = Buf()
        yT = sb("yT", [128, 32, 512], BF16); yTb = Buf()
        W = [sb("W%d" % i, [128, 96, 128], BF16) for i in range(2)]; Wb = [Buf(), Buf()]
        gt = [sb("gt%d" % i, [128, 1024], BF16) for i in range(4)]; gtb = [Buf() for _ in range(4)]
        gidx = sb("gidx", [128, 8, 4], I32); gidxb = Buf()
        sg = [sb("sg%d" % i, [128, 512], F32) for i in range(4)]; sgb = [Buf() for _ in range(4)]
        S_.barrier()
        x_dma(S_, "sp", gidx[:], I["gidx"], [], [gidxb])
        wc = 0
        for tt in range(2):
            x_dma(S_, "sp", hT2[:, :, :].rearrange("p a b -> p (a b)"), C.hT2[tt, :, :], [C.dramb], [hT2b])
            for ts in range(4):
                sub = tt * 4 + ts
                for jp in range(4):
                    g = gt[jp]
                    S_.dma("pool", lambda e, g=g, sub=sub, jp=jp: e.indirect_dma_start(
                        out=g[:, :], out_offset=None, in_=C.cc1_out[:, :],
                        in_offset=bass.IndirectOffsetOnAxis(ap=gidx[:, sub, jp:jp + 1], axis=0),
                        bounds_check=NCORE * S - 1, oob_is_err=False), reads=[gidxb, C.dramb], writes=[gtb[jp]])
                    pt, pb = getb()
                    for cbk in range(8):
                        x_tr(S_, C, pt[:, cbk * 128:(cbk + 1) * 128], g[:, cbk * 128:(cbk + 1) * 128], [gtb[jp]], [pb])
                    x_cp(S_, "act", hoT[:, jp * 4:jp * 4 + 4, ts * 128:(ts + 1) * 128],
                         pt[:, 0:512].rearrange("p (a b) -> p a b", b=128), [pb], [hoTb])
                    x_cp(S_, "dve", hoT[:, 16 + jp * 4:16 + jp * 4 + 4, ts * 128:(ts + 1) * 128],
                         pt[:, 512:1024].rearrange("p (a b) -> p a b", b=128), [pb], [hoTb])
            for dcn in range(32):
                k = wc % 2
                wc += 1
                x_dma(S_, "pool", W[k][:], I["w2"][dcn], [], [Wb[k]])
                ps = []
                for (i0, n, src, srcb) in ((0, 16, hoT, hoTb), (16, 16, hoT, hoTb), (32, 32, hT2, hT2b), (64, 32, hT2, hT2b)):
                    pp, ppb = getf()
                    for q in range(n):
                        kk = (i0 + q) if i0 < 32 else q
                        x_mm(S_, pp[:, :], W[k][:, i0 + q, :], src[:, kk, :], q == 0, q == n - 1, [Wb[k], srcb], [ppb])
                    ps.append((pp, ppb))
                (pym, pymb), (pyh, pyhb), (pgm, pgmb), (pgh, pghb) = ps
                x_act(S_, sg[0][:], pgm[:, :], AF.Sigmoid, [pgmb], [sgb[0]])
                x_act(S_, sg[1][:], pgh[:, :], AF.Sigmoid, [pghb], [sgb[1]])
                x_tt(S_, "dve", sg[2][:], pym[:, :], sg[0][:], ALU.mult, [pymb, sgb[0]], [sgb[2]])
                x_tt(S_, "dve", sg[3][:], pyh[:, :], sg[1][:], ALU.mult, [pyhb, sgb[1]], [sgb[3]])
                x_tt(S_, "dve", yT[:, dcn, :], sg[2][:], sg[3][:], ALU.add, [sgb[2], sgb[3]], [yTb])
            x_dma(S_, "sp", C.yT[tt, :, :], yT[:, :, :].rearrange("p a b -> p (a b)"), [yTb], [C.dramb])
        S_.barrier()
        S_.emit()


def stage_outproj(nc, S_, C, I):
    with contextlib.ExitStack() as st:
        getf, getb = C.getf, C.getb
        sb = lambda name, shape, dt: _sb(nc, st, "o_" + name, shape, dt)
        yT = sb("yT", [128, 32, 512], BF16); yTb = Buf()
        x2 = sb("x2", [128, 4, D], F32); x2b = [Buf() for _ in range(4)]
        W = [sb("W%d" % i, [128, 32, 256], BF16) for i in range(2)]; Wb = [Buf(), Buf()]
        xn = sb("xn", [128, D], BF16); xnb = Buf()
        h2T = sb("h2T", [128, 32, 128], BF16); h2Tb = Buf()
        nw = sb("nw", [128, D], F32); nwb = Buf()
        wr = sb("wr", [128, 32, 72], BF16); wrb = Buf()
        rb = sb("rb", [128, 72], F32); io8 = sb("io8", [128, 8], F32)
        lg = sb("lg", [128, 72], F32); lgb = Buf()
        t64 = sb("t64", [128, 64], F32); t8 = sb("t8", [128, 6, 8], F32); sm = sb("sm", [128, 16], F32)
        info = sb("info", [128, 16], F32); infob = Buf()
        cb_ = Buf()
        S_.barrier()
        x_dma(S_, "sp", nw[:], I["nw2"], [], [nwb])
        x_dma(S_, "pool", wr[:], I["wr"], [], [wrb])
        x_dma(S_, "sp", rb[:], I["rb"], [], [cb_])
        x_dma(S_, "sp", io8[:], I["io8"], [], [cb_])
        S_.op("dve", lambda e: e.memset(info[:], 0.0), writes=[infob])
        wc = 0
        s_ = lambda i: sm[:, i:i + 1]
        for tt in range(2):
            x_dma(S_, "sp", yT[:, :, :].rearrange("p a b -> p (a b)"), C.yT[tt, :, :], [C.dramb], [yTb])
            for ts in range(4):
                r0 = (tt * 4 + ts) * 128
                x_dma(S_, "sp", x2[:, ts, :], I["xt"][r0:r0 + 128, :], [], [x2b[ts]])
            for ct in range(16):
                k = wc % 2
                wc += 1
                x_dma(S_, "pool", W[k][:], I["w3"][ct], [], [Wb[k]])
                for ts in range(4):
                    pp, ppb = getf()
                    for kc in range(32):
                        x_mm(S_, pp[:, 0:256], yT[:, kc, ts * 128:(ts + 1) * 128], W[k][:, kc, :], kc == 0, kc == 31, [yTb, Wb[k]], [ppb])
                    x_tt(S_, "dve", x2[:, ts, ct * 256:(ct + 1) * 256], x2[:, ts, ct * 256:(ct + 1) * 256], pp[:, 0:256], ALU.add,
                         [ppb, x2b[ts]], [x2b[ts]])
            for ts in range(4):
                r0 = (tt * 4 + ts) * 128
                x_dma(S_, "sp", C.cc2_in[r0:r0 + 128, 0:D], x2[:, ts, :], [x2b[ts]], [C.dramb])
                x_act(S_, xn[:], x2[:, ts, :], AF.Square, [x2b[ts]], [xnb, cb_], accum_out=s_(0))
                x_rstd(S_, C, s_(1), s_(0), D, [cb_], [cb_])
                x_stt(S_, "dve", xn[:], x2[:, ts, :], s_(1), nw[:], ALU.mult, ALU.mult, [x2b[ts], cb_, nwb], [xnb])
                for g in range(4):
                    pt, pb = getb()
                    for q in range(8):
                        kc = g * 8 + q
                        x_tr(S_, C, pt[:, q * 128:(q + 1) * 128], xn[:, kc * 128:(kc + 1) * 128], [xnb], [pb])
                    x_cp(S_, "act" if g % 2 == 0 else "dve", h2T[:, g * 8:(g + 1) * 8, :], pt[:, :].rearrange("p (a b) -> p a b", b=128), [pb], [h2Tb])
                pr, prb = getf()
                for kc in range(32):
                    x_mm(S_, pr[:, 0:72], h2T[:, kc, :], wr[:, kc, :], kc == 0, kc == 31, [h2Tb, wrb], [prb])
                R_, W_ = [lgb, cb_], [cb_]
                x_tt(S_, "dve", lg[:], pr[:, 0:72], rb[:], ALU.add, [prb, cb_], [lgb])
                x_red(S_, s_(2), lg[:, 0:8], ALU.max, R_, W_)
                x_ts(S_, "dve", t8[:, 0, :], lg[:, 0:8], s_(2), None, ALU.is_equal, None, R_, W_)
                x_tt(S_, "dve", t8[:, 1, :], t8[:, 0, :], io8[:], ALU.mult, R_, W_)
                x_red(S_, info[:, 0:1], t8[:, 1, :], ALU.add, R_, [infob, cb_])
                x_ts(S_, "dve", s_(3), s_(2), -1.0, None, ALU.mult, None, R_, W_)
                x_act(S_, t8[:, 1, :], lg[:, 0:8], AF.Exp, R_, W_, bias=s_(3), scale=1.0, accum_out=s_(4))
                S_.op("dve", lambda e: e.reciprocal(out=sm[:, 5:6], in_=sm[:, 4:5]), R_, W_)
                el3 = lg[:, 8:72].rearrange("p (g e) -> p g e", e=8)
                oh3 = t8[:, 0, :].rearrange("p (g o) -> p g o", o=1).to_broadcast([128, 8, 8])
                x_tt(S_, "dve", t64[:].rearrange("p (g e) -> p g e", e=8), el3, oh3, ALU.mult, R_, W_)
                x_red(S_, t8[:, 2, :], t64[:].rearrange("p (g e) -> p e g", e=8), ALU.add, R_, W_)
                x_red(S_, s_(6), t8[:, 2, :], ALU.max, R_, W_)
                x_ts(S_, "dve", t8[:, 3, :], t8[:, 2, :], s_(6), None, ALU.is_equal, None, R_, W_)
                x_stt(S_, "dve", t8[:, 4, :], t8[:, 3, :], -1e30, t8[:, 2, :], ALU.mult, ALU.add, R_, W_)
                x_red(S_, s_(7), t8[:, 4, :], ALU.max, R_, W_)
                x_ts(S_, "dve", t8[:, 5, :], t8[:, 4, :], s_(7), None, ALU.is_equal, None, R_, W_)
                x_tt(S_, "dve", s_(8), s_(7), s_(6), ALU.subtract, R_, W_)
                x_act(S_, s_(9), s_(8), AF.Exp, R_, W_)
                x_ts(S_, "dve", s_(10), s_(9), 1.0, None, ALU.add, None, R_, W_)
                S_.op("dve", lambda e: e.reciprocal(out=sm[:, 10:11], in_=sm[:, 10:11]), R_, W_)
                x_tt(S_, "dve", s_(11), s_(10), s_(5), ALU.mult, R_, W_)
                x_tt(S_, "dve", s_(12), s_(11), s_(9), ALU.mult, R_, W_)
                x_ts(S_, "dve", t8[:, 1, :], t8[:, 3, :], s_(11), None, ALU.mult, None, R_, W_)
                x_stt(S_, "dve", info[:, 1:9], t8[:, 5, :], s_(12), t8[:, 1, :], ALU.mult, ALU.add, R_, [infob, cb_])
                x_dma(S_, "sp", C.cc2_in[r0:r0 + 128, D:D + 16], info[:], [infob], [C.dramb, infob])
        S_.barrier()
        S_.emit()


def stage_moe(nc, S_, C, I):
    NB = CAP // 128
    with contextlib.ExitStack() as st:
        getf, getb = C.getf, C.getb
        sb = lambda name, shape, dt: _sb(nc, st, "e_" + name, shape, dt)
        acc = sb("acc", [128, 4, XW], F32); accb = [Buf() for _ in range(4)]
        XgT = sb("XgT", [128, 32, 512], BF16); XgTb = Buf()
        xn = sb("xn", [128, D], BF16); xnb = Buf()
        aT = sb("aT", [128, 6, 512], BF16); aTb = Buf()
        Wt = [sb("Wt%d" % i, [128, 64, 128], BF16) for i in range(2)]; Wtb = [Buf(), Buf()]
        Wd = [sb("Wd%d" % i, [128, 6, 512], BF16) for i in range(2)]; Wdb = [Buf(), Buf()]
        nw = sb("nw", [128, D], F32); nwb = Buf()
        sg = [sb("sg%d" % i, [128, 512], F32) for i in range(2)]; sgb = [Buf(), Buf()]
        inf = sb("inf", [128, 64, 16], F32); cb_ = Buf()
        wk = sb("wk", [128, 6, 64], F32)
        idx = sb("idx", [128, NB], I32); idxb = Buf()
        gid = sb("gid", [128, 2], F32); Ls = sb("Ls", [128, 128], F32)
        sm = sb("sm", [128, 8], F32)
        s_ = lambda i: sm[:, i:i + 1]
        S_.barrier()
        iotar = sb("iotar", [128, CAP], F32)
        tokhl = sb("tokhl", [128, 64, 4], BF16)
        oh = [sb("oh%d" % i, [128, CAP], BF16) for i in range(2)]; ohb = [Buf(), Buf()]
        res = sb("res", [128, NB, 4], F32)
        x_dma(S_, "sp", inf[:], C.cc2_out[:, D:D + 16].rearrange("(p i) c -> p i c", i=64), [C.dramb], [cb_])
        x_dma(S_, "sp", gid[:, 0:1], I["gid"], [], [cb_])
        x_dma(S_, "sp", Ls[:], I["Ls"], [], [cb_])
        x_dma(S_, "sp", iotar[:], I["iotar"], [], [cb_])
        x_dma(S_, "pool", tokhl[:], I["tokhl"], [], [cb_])
        S_.op("dve", lambda e: e.memset(wk[:, 0, :], 1.0), writes=[cb_])
        S_.op("dve", lambda e: e.memset(acc[:], 0.0), writes=accb)
        R_, W_ = [cb_], [cb_]
        x_ts(S_, "dve", wk[:, 1, :], inf[:, :, 0], gid[:, 0:1], None, ALU.is_equal, None, R_, W_)
        S_.op("dve", lambda e: e.tensor_tensor_scan(out=wk[:, 2, :], data0=wk[:, 0, :], data1=wk[:, 1, :], initial=0.0,
                                                    op0=ALU.mult, op1=ALU.add), R_, W_)
        pp, ppb = getf()
        x_mm(S_, pp[:, 0:1], Ls[:], wk[:, 2, 63:64], True, True, R_, [ppb])
        x_cp(S_, "act", s_(0), pp[:, 0:1], [ppb], W_)
        x_ts(S_, "dve", wk[:, 3, :], wk[:, 2, :], s_(0), None, ALU.add, None, R_, W_)
        x_tt(S_, "dve", wk[:, 3, :], wk[:, 3, :], wk[:, 1, :], ALU.mult, R_, W_)
        x_ts(S_, "dve", wk[:, 3, :], wk[:, 3, :], -1.0, None, ALU.add, None, R_, W_)
        identF = sb("identF", [128, 128], F32)
        rowsT = sb("rowsT", [4, CAP], F32)
        x_dma(S_, "sp", identF[:], I["ident"], [], [cb_])
        pcs = [getf() for _ in range(CAP // 512)]
        for i in range(64):
            k = i % 2
            x_ts(S_, "dve", oh[k][:], iotar[:], wk[:, 3, i:i + 1], None, ALU.is_equal, None, R_, [ohb[k]])
            for c3, (pc, pcb) in enumerate(pcs):
                x_mm(S_, pc[0:4, :], tokhl[:, i, :], oh[k][:, c3 * 512:(c3 + 1) * 512], i == 0, i == 63, [ohb[k], cb_], [pcb])
        for c3, (pc, pcb) in enumerate(pcs):
            x_cp(S_, "act", rowsT[:, c3 * 512:(c3 + 1) * 512], pc[0:4, :], [pcb], W_)
        pt2, pt2b = getf()
        for rb_ in range(NB):
            S_.op("pe", lambda e, rb_=rb_: e.transpose(out=pt2[:, rb_ * 4:rb_ * 4 + 4], in_=rowsT[0:4, rb_ * 128:(rb_ + 1) * 128],
                                                       identity=identF[0:4, 0:4]), R_, [pt2b])
        x_cp(S_, "act", res[:], pt2[:, 0:NB * 4].rearrange("p (a b) -> p a b", b=4), [pt2b], W_)
        x_stt(S_, "dve", res[:, :, 3], res[:, :, 0], 64.0, res[:, :, 1], ALU.mult, ALU.add, R_, W_)
        x_ts(S_, "dve", res[:, :, 3], res[:, :, 3], -100000.0, None, ALU.add, None, R_, W_)
        x_tt(S_, "dve", res[:, :, 3], res[:, :, 3], res[:, :, 2], ALU.mult, R_, W_)
        x_ts(S_, "dve", res[:, :, 3], res[:, :, 3], 100000.0, None, ALU.add, None, R_, W_)
        x_cp(S_, "dve", idx[:], res[:, :, 3], R_, [idxb])
        x_dma(S_, "sp", C.oidx, idx[:], [idxb], [C.dramb])
        wtc = 0
        wdc = 0
        for tt in range(CAP // 512):
            x_dma(S_, "sp", nw[:], I["nw2"], [], [nwb])
            for ts in range(4):
                blk = tt * 4 + ts
                S_.dma("pool", lambda e, ts=ts, blk=blk: e.indirect_dma_start(
                    out=acc[:, ts, :], out_offset=None, in_=C.cc2_out[:, :],
                    in_offset=bass.IndirectOffsetOnAxis(ap=idx[:, blk:blk + 1], axis=0),
                    bounds_check=NCORE * 1024 - 1, oob_is_err=False), reads=[idxb, C.dramb], writes=[accb[ts]])
                x_act(S_, xn[:], acc[:, ts, 0:D], AF.Square, [accb[ts]], [xnb, cb_], accum_out=s_(1))
                x_rstd(S_, C, s_(2), s_(1), D, [cb_], [cb_])
                x_stt(S_, "dve", xn[:], acc[:, ts, 0:D], s_(2), nw[:], ALU.mult, ALU.mult, [accb[ts], cb_, nwb], [xnb])
                for g in range(4):
                    pt, pb = getb()
                    for q in range(8):
                        kc = g * 8 + q
                        x_tr(S_, C, pt[:, q * 128:(q + 1) * 128], xn[:, kc * 128:(kc + 1) * 128], [xnb], [pb])
                    x_cp(S_, "act" if g % 2 == 0 else "dve", XgT[:, g * 8:(g + 1) * 8, ts * 128:(ts + 1) * 128],
                         pt[:, :].rearrange("p (a b) -> p a b", b=128), [pb], [XgTb])
            for ex in range(8):
                for fc in range(6):
                    k = wtc % 2
                    wtc += 1
                    x_dma(S_, "pool", Wt[k][:], I["wgu"][ex * 6 + fc], [], [Wtb[k]])
                    pg, pgb = getf()
                    for kc in range(32):
                        x_mm(S_, pg[:, :], Wt[k][:, kc, :], XgT[:, kc, :], kc == 0, kc == 31, [Wtb[k], XgTb], [pgb])
                    pu, pub = getf()
                    for kc in range(32):
                        x_mm(S_, pu[:, :], Wt[k][:, 32 + kc, :], XgT[:, kc, :], kc == 0, kc == 31, [Wtb[k], XgTb], [pub])
                    x_act(S_, sg[0][:], pg[:, :], AF.Sigmoid, [pgb], [sgb[0]])
                    x_tt(S_, "dve", sg[1][:], pg[:, :], sg[0][:], ALU.mult, [pgb, sgb[0]], [sgb[1]])
                    x_tt(S_, "dve", aT[:, fc, :], pu[:, :], sg[1][:], ALU.mult, [pub, sgb[1]], [aTb])
                for ct in range(8):
                    k = wdc % 2
                    wdc += 1
                    x_dma(S_, "pool", Wd[k][:], I["wdn"][ex * 8 + ct], [], [Wdb[k]])
                    for ts in range(4):
                        po, pob = getf()
                        for fc in range(6):
                            x_mm(S_, po[:, :], aT[:, fc, ts * 128:(ts + 1) * 128], Wd[k][:, fc, :], fc == 0, fc == 5, [aTb, Wdb[k]], [pob])
                        x_stt(S_, "dve", acc[:, ts, ct * 512:(ct + 1) * 512], po[:, :], acc[:, ts, D + 1 + ex:D + 2 + ex],
                              acc[:, ts, ct * 512:(ct + 1) * 512], ALU.mult, ALU.add, [pob, accb[ts]], [accb[ts]])
            x_dma(S_, "sp", nw[:], I["nwf"], [], [nwb])
            for ts in range(4):
                blk = tt * 4 + ts
                x_act(S_, xn[:], acc[:, ts, 0:D], AF.Square, [accb[ts]], [xnb, cb_], accum_out=s_(1))
                x_rstd(S_, C, s_(2), s_(1), D, [cb_], [cb_])
                x_stt(S_, "dve", acc[:, ts, 0:D], acc[:, ts, 0:D], s_(2), nw[:], ALU.mult, ALU.mult, [accb[ts], cb_, nwb], [accb[ts]])
                x_dma(S_, "sp", C.orow[blk * 128:(blk + 1) * 128, :], acc[:, ts, 0:D], [accb[ts]], [C.dramb])
        S_.barrier()
        S_.emit()


def build_full(upto=3, debug=False, mode="fused"):
    nc = bass.Bass("TRN2", target_bir_lowering=False)
    I = {}

    def inp(name, shape, dt=F32):
        I[name] = nc.dram_tensor(name, list(shape), dt, kind="ExternalInput").ap()

    fused = mode == "fused"
    do1 = fused or mode == "L1"
    do2 = (fused and upto >= 2) or mode == "L2"
    do3 = (fused and upto >= 3) or mode == "L3"
    inp("ident", [128, 128])
    if do1:
        inp("maskUT", [128, 128]); inp("rst", [128, 512])
        inp("xb", [S, D]); inp("w1", [16, 128, 32, 256]); inp("wg", [128, 32, 4])
        inp("cw", [128, 8, 4]); inp("cb", [128, 8]); inp("lb", [128, 4, 2]); inp("gb", [1, 8])
        inp("mnw", [128, 512]); inp("hnw", [128, 512])
    if do1 or do2:
        inp("nw1", [128, D])
    if do2:
        inp("xt", [1024, D]); inp("gidx", [128, 8, 4], I32); inp("w2", [32, 128, 96, 128]); inp("w3", [16, 128, 32, 256])
        inp("wr", [128, 32, 72]); inp("rb", [128, 72]); inp("io8", [128, 8])
    if do2 or do3:
        inp("nw2", [128, D])
    if do3:
        inp("gid", [128, 1]); inp("Ls", [128, 128]); inp("iotar", [128, CAP]); inp("tokhl", [128, 64, 4])
        inp("wgu", [48, 128, 64, 128]); inp("wdn", [64, 128, 6, 512]); inp("nwf", [128, D])
    C = Ctx()
    dk = lambda lvl: "ExternalOutput" if (debug and upto == lvl) else "Internal"
    C.dramb = Buf("dram")
    if do1:
        C.hT1 = nc.dram_tensor("hT1", [8, 128, 32 * 512], BF16).ap()
        C.cc1_in = nc.dram_tensor("cc1_in", [S, 1024], BF16, kind="ExternalOutput" if mode == "L1" else dk(1)).ap()
    if do2:
        if fused:
            C.cc1_out = nc.dram_tensor("cc1_out", [NCORE * S, 1024], BF16).ap()
        else:
            C.cc1_out = nc.dram_tensor("cc1_out", [4 * S, 1024], BF16, kind="ExternalInput").ap()
        C.hT2 = nc.dram_tensor("hT2", [2, 128, 32 * 512], BF16).ap()
        C.yT = nc.dram_tensor("yT", [2, 128, 32 * 512], BF16).ap()
        C.cc2_in = nc.dram_tensor("cc2_in", [1024, XW], F32, kind="ExternalOutput" if mode == "L2" else dk(2)).ap()
    if do3:
        if fused:
            C.cc2_out = nc.dram_tensor("cc2_out", [NCORE * 1024, XW], F32).ap()
        else:
            C.cc2_out = nc.dram_tensor("cc2_out", [NCORE * 1024, XW], F32, kind="ExternalInput").ap()
        C.oidx = nc.dram_tensor("oidx", [128, CAP // 128], I32, kind="ExternalOutput").ap()
        C.orow = nc.dram_tensor("orow", [CAP, D], F32, kind="ExternalOutput").ap()
    with contextlib.ExitStack() as stack:
        S_ = Sched(nc, stack)
        C.getf, C.getb = _psum_pool(nc, stack, S_)
        C.ident = _sb(nc, stack, "ident", [128, 128], BF16)
        C.identb = Buf("ident")
        S_.dma("pool", lambda e: e.dma_start(out=C.ident[:], in_=I["ident"]), writes=[C.identb])
        C.epsc = _sb(nc, stack, "epsc", [128, 2], F32)
        S_.op("dve", lambda e: e.memset(C.epsc[:], EPS), writes=[C.identb])
        if do1:
            stage_norm_T(nc, S_, C, I["xb"], I["nw1"], C.hT1, S)
            stage_mixer(nc, S_, C, I)
        if do2:
            if fused:
                S_.barrier()
                for k in range(S // CH1):
                    coll(S_, nc, "AllGather", [list(range(NCORE))], C.cc1_in[k * CH1:(k + 1) * CH1, :],
                         C.cc1_out[k * NCORE * CH1:(k + 1) * NCORE * CH1, :], [C.dramb], [C.dramb])
            stage_norm_T(nc, S_, C, I["xt"], I["nw1"], C.hT2, 1024)
            import os as _os
            lim = int(_os.environ.get("K_LIM", "9"))
            if lim >= 1:
                stage_branch(nc, S_, C, I)
            if lim >= 2:
                stage_outproj(nc, S_, C, I)
        if do3:
            if fused:
                S_.barrier()
                for k in range(1024 // CH2):
                    coll(S_, nc, "AllGather", [list(range(NCORE))], C.cc2_in[k * CH2:(k + 1) * CH2, :],
                         C.cc2_out[k * NCORE * CH2:(k + 1) * NCORE * CH2, :], [C.dramb], [C.dramb])
            stage_moe(nc, S_, C, I)
        S_.barrier()
        S_.emit()
    return nc


def prep_p2(c, inputs, shared, fused=False):
    seg = c % 4
    gidx = np.zeros((128, 8, 4), np.int32)
    for sub in range(8):
        for jp in range(4):
            t = seg * 1024 + sub * 128 + np.arange(128)
            if fused:
                rank = (c // 4) * 4 + jp
                gidx[:, sub, jp] = (t // CH1) * (NCORE * CH1) + rank * CH1 + (t % CH1)
            else:
                gidx[:, sub, jp] = jp * S + t
    xt = inputs["x"].reshape(-1, D)[c * 1024:(c + 1) * 1024]
    d = {"xt": np.ascontiguousarray(xt, dtype=np.float32), "gidx": gidx}
    d.update(shared)
    return d


def prep_p2_shared(inputs):
    w_in = inputs["w_in"][0]
    g0 = 8 * 2048 + 16
    wbm = inputs["w_branch_m"][0]
    wbh = inputs["w_branch_h"][0]
    w2 = np.empty((32, 128, 96, 128), np.float32)
    for dcn in range(32):
        cs = slice(dcn * 128, (dcn + 1) * 128)
        w2[dcn, :, 0:16, :] = wbm[:, cs].reshape(16, 128, 128).transpose(1, 0, 2)
        w2[dcn, :, 16:32, :] = wbh[:, cs].reshape(16, 128, 128).transpose(1, 0, 2)
        w2[dcn, :, 32:64, :] = w_in[:, g0 + dcn * 128:g0 + (dcn + 1) * 128].reshape(32, 128, 128).transpose(1, 0, 2)
        w2[dcn, :, 64:96, :] = w_in[:, g0 + D + dcn * 128:g0 + D + (dcn + 1) * 128].reshape(32, 128, 128).transpose(1, 0, 2)
    wo = inputs["w_out"][0]
    w3 = np.ascontiguousarray(wo.reshape(32, 128, 16, 256).transpose(2, 1, 0, 3))
    wrc = np.concatenate([inputs["router_group_w"][0], inputs["router_expert_w"][0]], 1)
    wr = _wtile(wrc)
    rbv = np.concatenate([inputs["router_group_b"][0], inputs["router_expert_b"][0]])
    return {"w2": w2, "w3": w3, "nw2": np.ascontiguousarray(np.broadcast_to(inputs["norm_ffn_w"][0], (128, D)), dtype=np.float32),
            "wr": np.ascontiguousarray(wr, dtype=np.float32), "rb": np.ascontiguousarray(np.broadcast_to(rbv, (128, 72)), dtype=np.float32),
            "io8": np.ascontiguousarray(np.broadcast_to(np.arange(8, dtype=np.float32), (128, 8)))}


def _tokhl():
    t = np.zeros((128, 64, 4), np.float32)
    t[:, :, 0] = np.arange(128)[:, None]
    t[:, :, 1] = np.arange(64)[None, :]
    t[:, :, 2] = 1.0
    return t


def prep_p3(c, inputs):
    wg_, wu_, wd_ = inputs["w_gate"][0], inputs["w_up"][0], inputs["w_down"][0]
    wgu = np.empty((48, 128, 64, 128), np.float32)
    wdn = np.empty((64, 128, 6, 512), np.float32)
    for ex in range(8):
        e = c * 8 + ex
        a = np.asarray(wg_[e]).reshape(32, 128, 6, 128)
        b = np.asarray(wu_[e]).reshape(32, 128, 6, 128)
        wgu[ex * 6:(ex + 1) * 6, :, 0:32, :] = a.transpose(2, 1, 0, 3)
        wgu[ex * 6:(ex + 1) * 6, :, 32:64, :] = b.transpose(2, 1, 0, 3)
        dd = np.asarray(wd_[e]).reshape(6, 128, 8, 512)
        wdn[ex * 8:(ex + 1) * 8] = dd.transpose(2, 1, 0, 3)
    s = np.arange(128)
    return {"gid": np.full((128, 1), float(c), np.float32), "Ls": (s[:, None] < s[None, :]).astype(np.float32),
            "iotar": np.ascontiguousarray(np.broadcast_to(np.arange(CAP, dtype=np.float32), (128, CAP))), "tokhl": _tokhl(), "wgu": wgu, "wdn": wdn,
            "nwf": np.ascontiguousarray(np.broadcast_to(inputs["norm_final_w"], (128, D)), dtype=np.float32)}


_NC_CACHE = {}


def _get_nc(mode):
    if mode not in _NC_CACHE:
        _NC_CACHE[mode] = build_full(3, mode=mode)
    return _NC_CACHE[mode]


def _pick(d, nc_inputs):
    return {k: d[k] for k in nc_inputs}


FUSED = False


def _kernel_fused(inputs):
    com = _prep_common()
    cores = list(range(NCORE))
    shared = prep_p2_shared(inputs)
    maps = []
    for c in cores:
        d = prep_p1(c, inputs)
        d.update(com)
        d.update(prep_p2(c, inputs, shared, fused=True))
        d.update(prep_p3(c, inputs))
        maps.append(d)
    res = run_bass_kernel_spmd(_get_nc("fused"), maps, core_ids=cores).results
    out = np.zeros((NCORE * 1024, D), np.float32)
    for c in cores:
        idx = np.asarray(res[c]["oidx"]).T.reshape(-1)
        rows = np.asarray(res[c]["orow"])
        ok = (idx >= 0) & (idx < NCORE * 1024)
        f = idx[ok]
        tok = ((f % (NCORE * CH2)) // CH2) * 1024 + (f // (NCORE * CH2)) * CH2 + (f % CH2)
        out[tok] = rows[ok]
    return out.reshape(2, S, D)


def kernel(**inputs):
    inputs = {k: np.asarray(v) for k, v in inputs.items()}
    if FUSED:
        return _kernel_fused(inputs)
    com = _prep_common()
    cores = list(range(NCORE))
    names1 = ["ident", "maskUT", "rst", "xb", "w1", "wg", "cw", "cb", "lb", "gb", "mnw", "hnw", "nw1"]
    maps = []
    for c in cores:
        d = prep_p1(c, inputs)
        d.update(com)
        maps.append(_pick(d, names1))
    r1 = run_bass_kernel_spmd(_get_nc("L1"), maps, core_ids=cores).results
    ho = [np.asarray(r["cc1_in"]) for r in r1]
    del maps
    shared = prep_p2_shared(inputs)
    nw1 = np.ascontiguousarray(np.broadcast_to(inputs["norm_mix_w"][0], (128, D)), dtype=np.float32)
    names2 = ["ident", "nw1", "xt", "gidx", "w2", "w3", "wr", "rb", "io8", "nw2", "cc1_out"]
    gath = [np.concatenate(ho[0:4], 0), np.concatenate(ho[4:8], 0)]
    maps = []
    for c in cores:
        d = prep_p2(c, inputs, shared)
        d.update({"ident": com["ident"], "nw1": nw1, "cc1_out": gath[c // 4]})
        maps.append(_pick(d, names2))
    r2 = run_bass_kernel_spmd(_get_nc("L2"), maps, core_ids=cores).results
    x2all = np.concatenate([np.asarray(r["cc2_in"]) for r in r2], 0)
    del maps
    names3 = ["ident", "nw2", "gid", "Ls", "iotar", "tokhl", "wgu", "wdn", "nwf", "cc2_out"]
    maps = []
    for c in cores:
        d = prep_p3(c, inputs)
        d.update({"ident": com["ident"], "nw2": shared["nw2"], "cc2_out": x2all})
        maps.append(_pick(d, names3))
    r3 = run_bass_kernel_spmd(_get_nc("L3"), maps, core_ids=cores).results
    out = np.zeros((NCORE * 1024, D), np.float32)
    for c in cores:
        idx = np.asarray(r3[c]["oidx"]).T.reshape(-1)
        rows = np.asarray(r3[c]["orow"])
        ok = (idx >= 0) & (idx < NCORE * 1024)
        out[idx[ok]] = rows[ok]
    return out.reshape(2, S, D)
```
